# Optimizing a Trainium2 kernel written in Bass

```python
import math
import jax, jax.numpy as jnp
from jax import lax
import numpy as np

D_MODEL = 1024
BATCH = 8
SEQ = 2048
DEPTH = 4

GRID_W = 64
CTX_LEN = 256
EPS = 1e-6
N_MOD = 6
DA_HEADS = 4
DA_HEAD_DIM = 64
DA_WIDTH = DA_HEADS * 2 * DA_HEAD_DIM
ROPE_BASE = 10000.0
Q_BLOCK = 128
NA_HEADS = 8
NA_HEAD_DIM = 64
NA_WIDTH = NA_HEADS * NA_HEAD_DIM
NA_KH_MAX = 8
NA_KW = 16
FT_GROUPS = 4
FT_GROUP_DIM = 128
FT_WIDTH = FT_GROUPS * FT_GROUP_DIM
BRANCH_WIDTH = 512
N_BRANCHES = 3
IN_SPLITS = [DA_WIDTH, DA_WIDTH, DA_WIDTH, NA_WIDTH, NA_WIDTH, NA_WIDTH, FT_WIDTH, N_BRANCHES * D_MODEL]
IN_COLS = sum(IN_SPLITS)
N_EXPERTS = 32
TOP_K = 4
D_FF = D_MODEL
SWIGLU_LIMIT = 7.0
SWIGLU_ALPHA = 1.702
MOE_BLOCK = 256

kernel_name = "hybrid_diffusion_parallel_mixer_moe"


def rmsnorm(x, g):
    xf = x.astype(jnp.float32)
    xf = xf * lax.rsqrt(jnp.mean(xf * xf, axis=-1, keepdims=True) + EPS)
    return (xf * g.astype(jnp.float32)).astype(x.dtype)


def axial_rope_tables(n):
    t = jnp.arange(n)
    row = (t // GRID_W).astype(jnp.float32)
    col = (t % GRID_W).astype(jnp.float32)
    quarter = DA_HEAD_DIM // 4
    freqs = ROPE_BASE ** (-jnp.arange(quarter, dtype=jnp.float32) / quarter)
    ang = jnp.stack([row[:, None] * freqs, col[:, None] * freqs], axis=1)
    return jnp.cos(ang), jnp.sin(ang)


def apply_axial_rope(x, cos, sin):
    xs = x.astype(jnp.float32).reshape(x.shape[:-1] + (2, 2, x.shape[-1] // 4))
    x1, x2 = xs[..., 0, :], xs[..., 1, :]
    c = cos[None, :, None, None]
    s = sin[None, :, None, None]
    out = jnp.stack([x1 * c - x2 * s, x1 * s + x2 * c], axis=-2)
    return out.reshape(x.shape).astype(x.dtype)


def diff_attend(q, k, v, lam, subln_g, lam_init):
    s = jnp.einsum('bqhmd,bkhmd->bhmqk', q, k).astype(jnp.float32) * (DA_HEAD_DIM ** -0.5)
    p = jax.nn.softmax(s, axis=-1)
    a = p[:, :, 0] - lam * p[:, :, 1]
    o = jnp.einsum('bhqk,bkhe->bqhe', a.astype(v.dtype), v)
    o = rmsnorm(o, subln_g) * (1.0 - lam_init)
    return o.reshape(o.shape[0], o.shape[1], DA_WIDTH)


def diff_attention_latent(q, k, v, lam, subln_g, lam_init):
    B, S = q.shape[0], q.shape[1]
    nb = S // Q_BLOCK
    qb = q.reshape(B, nb, Q_BLOCK, DA_HEADS, 2, DA_HEAD_DIM).swapaxes(0, 1)
    ob = lax.map(lambda qi: diff_attend(qi, k, v, lam, subln_g, lam_init), qb)
    return ob.swapaxes(0, 1).reshape(B, S, DA_WIDTH)


def neighbourhood_attention(q, k, v, k_ctx, v_ctx, rpb):
    B, S, H, d = q.shape
    rows = S // GRID_W
    kh = min(NA_KH_MAX, rows)
    r = jnp.arange(rows)
    row_start = jnp.clip(r - kh // 2, 0, rows - kh)
    key_rows = row_start[:, None] + jnp.arange(kh)[None, :]
    col = jnp.arange(GRID_W)
    col_start = jnp.clip(col - NA_KW // 2, 0, GRID_W - NA_KW)
    col_mask = (col[None, :] >= col_start[:, None]) & (col[None, :] < col_start[:, None] + NA_KW)
    qg = q.reshape(B, rows, GRID_W, H, d)
    kg = k.reshape(B, rows, GRID_W, H, d)[:, key_rows]
    vg = v.reshape(B, rows, GRID_W, H, d)[:, key_rows]
    scale = d ** -0.5
    s_nb = jnp.einsum('brqhd,brjkhd->brhqjk', qg, kg).astype(jnp.float32) * scale
    dr = key_rows - r[:, None] + NA_KH_MAX - 1
    dc = jnp.clip(col[None, :] - col[:, None], 1 - NA_KW, NA_KW - 1) + NA_KW - 1
    bias = rpb.astype(jnp.float32)[:, dr[:, :, None, None], dc[None, None]]
    s_nb = s_nb + bias.transpose(1, 0, 3, 2, 4)[None]
    s_nb = jnp.where(col_mask[:, None, :], s_nb, -jnp.inf)
    s_ctx = jnp.einsum('brqhd,bkhd->brhqk', qg, k_ctx).astype(jnp.float32) * scale
    n_nb = kh * GRID_W
    s_all = jnp.concatenate([s_nb.reshape(B, rows, H, GRID_W, n_nb), s_ctx], axis=-1)
    p = jax.nn.softmax(s_all, axis=-1).astype(v.dtype)
    p_nb = p[..., :n_nb].reshape(B, rows, H, GRID_W, kh, GRID_W)
    p_ctx = p[..., n_nb:]
    o = (jnp.einsum('brhqjk,brjkhd->brqhd', p_nb, vg)
         + jnp.einsum('brhqk,bkhd->brqhd', p_ctx, v_ctx))
    return o.reshape(B, S, H * d)


def context_attention(q, k, v):
    s = jnp.einsum('bqhd,bkhd->bhqk', q, k).astype(jnp.float32) * (q.shape[-1] ** -0.5)
    p = jax.nn.softmax(s, axis=-1).astype(v.dtype)
    o = jnp.einsum('bhqk,bkhd->bqhd', p, v)
    return o.reshape(q.shape[0], q.shape[1], -1)


def fourier_mix(u):
    B, n, _ = u.shape
    ug = u.astype(jnp.float32).reshape(B, n, FT_GROUPS, FT_GROUP_DIM)
    f = jnp.fft.fftn(ug, axes=(1, 3), norm='ortho').real
    return f.reshape(B, n, FT_WIDTH).astype(u.dtype)


def merge_branches(oa, ob, oc, gate_logits, w_branch_l, w_out_l):
    o = jnp.stack([oa, ob, oc], axis=2)
    proj = jnp.einsum('bnie,ied->bnid', o, w_branch_l)
    g = jax.nn.sigmoid(gate_logits.reshape(gate_logits.shape[:-1] + (N_BRANCHES, D_MODEL)))
    return jnp.sum(g * proj, axis=2) @ w_out_l


def moe_ffn(h, w_router, b_router, w_gate_up, b_gate_up, w_down, b_down):
    lead = h.shape[:-1]
    hf = h.reshape(-1, D_MODEL)
    T = hf.shape[0]
    logits = (hf @ w_router + b_router).astype(jnp.float32)
    top_val, top_idx = lax.top_k(logits, TOP_K)
    top_w = jax.nn.softmax(top_val, axis=-1).astype(h.dtype)
    n_assign = T * TOP_K
    flat_e = top_idx.reshape(-1)
    order = jnp.argsort(flat_e)
    sorted_e = flat_e[order]
    sorted_tok = (order // TOP_K).astype(jnp.int32)
    sorted_w = top_w.reshape(-1)[order]
    counts = jnp.bincount(flat_e, length=N_EXPERTS)
    padded = (counts + MOE_BLOCK - 1) // MOE_BLOCK * MOE_BLOCK
    pad_end = jnp.cumsum(padded)
    pad_start = pad_end - padded
    grp_start = jnp.cumsum(counts) - counts
    slot = pad_start[sorted_e] + jnp.arange(n_assign) - grp_start[sorted_e]
    n_blocks = -(-n_assign // MOE_BLOCK) + N_EXPERTS
    n_slots = n_blocks * MOE_BLOCK
    slot_tok = jnp.full((n_slots,), T, jnp.int32).at[slot].set(sorted_tok)
    slot_w = jnp.zeros((n_slots,), h.dtype).at[slot].set(sorted_w)
    block_e = jnp.minimum(jnp.searchsorted(pad_end, jnp.arange(n_blocks) * MOE_BLOCK, side='right'),
                          N_EXPERTS - 1)
    h_pad = jnp.concatenate([hf, jnp.zeros((1, D_MODEL), h.dtype)], axis=0)
    xb = h_pad[slot_tok].reshape(n_blocks, MOE_BLOCK, D_MODEL)

    def expert_block(args):
        xe, e = args
        gu = xe @ w_gate_up[e] + b_gate_up[e]
        gate = jnp.minimum(gu[:, :D_FF], SWIGLU_LIMIT)
        up = jnp.clip(gu[:, D_FF:], -SWIGLU_LIMIT, SWIGLU_LIMIT)
        act = gate * jax.nn.sigmoid(SWIGLU_ALPHA * gate) * (up + 1.0)
        return act @ w_down[e] + b_down[e]

    yb = lax.map(expert_block, (xb, block_e)).reshape(n_slots, D_MODEL)
    out = jnp.zeros((T + 1, D_MODEL), h.dtype).at[slot_tok].add(yb * slot_w[:, None])
    return out[:T].reshape(lead + (D_MODEL,))


def setup_inputs(seed: int = 0) -> dict:
    key = jax.random.key(seed)
    ks = jax.random.split(key, 24)
    f32 = jnp.float32
    nrm = lambda k, shape, s: (jax.random.normal(k, shape, f32) * s)
    D = D_MODEL
    return {
        'x': nrm(ks[0], (BATCH, SEQ, D), 1.0),
        'c': nrm(ks[1], (BATCH, D), 1.0),
        'ctx': nrm(ks[2], (BATCH, CTX_LEN, D), 1.0),
        'c_ctx': nrm(ks[3], (D,), 1.0),
        'w_mod': nrm(ks[4], (DEPTH, D, N_MOD * D), 0.5 * D ** -0.5),
        'b_mod': nrm(ks[5], (DEPTH, N_MOD * D), 0.02),
        'norm_mix_g': 1.0 + nrm(ks[6], (DEPTH, D), 0.02),
        'norm_ffn_g': 1.0 + nrm(ks[7], (DEPTH, D), 0.02),
        'w_in': nrm(ks[8], (DEPTH, D, IN_COLS), D ** -0.5),
        'da_lambda': nrm(ks[9], (DEPTH, 4, DA_HEAD_DIM), 0.1),
        'da_subln_g': 1.0 + nrm(ks[10], (DEPTH, 2 * DA_HEAD_DIM), 0.02),
        'na_rpb': nrm(ks[11], (DEPTH, NA_HEADS, 2 * NA_KH_MAX - 1, 2 * NA_KW - 1), 0.1),
        'w_branch': nrm(ks[12], (DEPTH, N_BRANCHES, BRANCH_WIDTH, D), BRANCH_WIDTH ** -0.5),
        'w_out': nrm(ks[13], (DEPTH, D, D), D ** -0.5),
        'w_router': nrm(ks[14], (DEPTH, D, N_EXPERTS), D ** -0.5),
        'b_router': nrm(ks[15], (DEPTH, N_EXPERTS), 0.01),
        'w_gate_up': nrm(ks[16], (DEPTH, N_EXPERTS, D, 2 * D_FF), D ** -0.5),
        'b_gate_up': nrm(ks[17], (DEPTH, N_EXPERTS, 2 * D_FF), 0.02),
        'w_down': nrm(ks[18], (DEPTH, N_EXPERTS, D_FF, D), D_FF ** -0.5),
        'b_down': nrm(ks[19], (DEPTH, N_EXPERTS, D), 0.02),
        'final_g': 1.0 + nrm(ks[20], (D,), 0.02),
    }


def reference(x, c, ctx, c_ctx, w_mod, b_mod, norm_mix_g, norm_ffn_g, w_in, da_lambda,
              da_subln_g, na_rpb, w_branch, w_out, w_router, b_router, w_gate_up,
              b_gate_up, w_down, b_down, final_g):
    B, S, _ = x.shape
    C = ctx.shape[1]
    split_pts = list(np.cumsum(IN_SPLITS)[:-1])
    cos, sin = axial_rope_tables(S)
    silu_c = jax.nn.silu(c)
    silu_cc = jax.nn.silu(c_ctx)[None]
    y = ctx
    for l in range(DEPTH):
        last = l == DEPTH - 1
        lam_init = 0.8 - 0.6 * math.exp(-0.3 * l)
        lq1, lk1, lq2, lk2 = [da_lambda[l, i].astype(jnp.float32) for i in range(4)]
        lam = jnp.exp(jnp.sum(lq1 * lk1)) - jnp.exp(jnp.sum(lq2 * lk2)) + lam_init
        sh1x, sc1x, g1x, sh2x, sc2x, g2x = [m[:, None] for m in jnp.split(silu_c @ w_mod[l] + b_mod[l], N_MOD, axis=-1)]
        sh1y, sc1y, g1y, sh2y, sc2y, g2y = [m[:, None] for m in jnp.split(silu_cc @ w_mod[l] + b_mod[l], N_MOD, axis=-1)]

        hx = rmsnorm(x, norm_mix_g[l]) * (1.0 + sc1x) + sh1x
        hy = rmsnorm(y, norm_mix_g[l]) * (1.0 + sc1y) + sh1y
        qa_x, ka_x, va_x, qn_x, kn_x, vn_x, f_x, gt_x = jnp.split(hx @ w_in[l], split_pts, axis=-1)
        qa_y, ka_y, va_y, qn_y, kn_y, vn_y, f_y, gt_y = jnp.split(hy @ w_in[l], split_pts, axis=-1)

        qa_x = apply_axial_rope(qa_x.reshape(B, S, DA_HEADS, 2, DA_HEAD_DIM), cos, sin)
        ka_x = apply_axial_rope(ka_x.reshape(B, S, DA_HEADS, 2, DA_HEAD_DIM), cos, sin)
        ka_y = ka_y.reshape(B, C, DA_HEADS, 2, DA_HEAD_DIM)
        va_x = va_x.reshape(B, S, DA_HEADS, 2 * DA_HEAD_DIM)
        va_y = va_y.reshape(B, C, DA_HEADS, 2 * DA_HEAD_DIM)
        k_cat = jnp.concatenate([ka_x, ka_y], axis=1)
        v_cat = jnp.concatenate([va_x, va_y], axis=1)
        oa_x = diff_attention_latent(qa_x, k_cat, v_cat, lam, da_subln_g[l], lam_init)

        kn_y = kn_y.reshape(B, C, NA_HEADS, NA_HEAD_DIM)
        vn_y = vn_y.reshape(B, C, NA_HEADS, NA_HEAD_DIM)
        ob_x = neighbourhood_attention(qn_x.reshape(B, S, NA_HEADS, NA_HEAD_DIM),
                                       kn_x.reshape(B, S, NA_HEADS, NA_HEAD_DIM),
                                       vn_x.reshape(B, S, NA_HEADS, NA_HEAD_DIM),
                                       kn_y, vn_y, na_rpb[l])

        oc_x = fourier_mix(f_x)
        mix_x = merge_branches(oa_x, ob_x, oc_x, gt_x, w_branch[l], w_out[l])

        if not last:
            oa_y = diff_attend(qa_y.reshape(B, C, DA_HEADS, 2, DA_HEAD_DIM), ka_y, va_y,
                               lam, da_subln_g[l], lam_init)
            ob_y = context_attention(qn_y.reshape(B, C, NA_HEADS, NA_HEAD_DIM), kn_y, vn_y)
            oc_y = fourier_mix(f_y)
            mix_y = merge_branches(oa_y, ob_y, oc_y, gt_y, w_branch[l], w_out[l])
            y = y + g1y * mix_y
            hy2 = rmsnorm(y, norm_ffn_g[l]) * (1.0 + sc2y) + sh2y
            y = y + g2y * moe_ffn(hy2, w_router[l], b_router[l], w_gate_up[l], b_gate_up[l],
                                  w_down[l], b_down[l])

        x = x + g1x * mix_x
        hx2 = rmsnorm(x, norm_ffn_g[l]) * (1.0 + sc2x) + sh2x
        x = x + g2x * moe_ffn(hx2, w_router[l], b_router[l], w_gate_up[l], b_gate_up[l],
                              w_down[l], b_down[l])
    return rmsnorm(x, final_g)
```

```python
import math
import os
import numpy as np
BDBG = int(os.environ.get('BDBG', '0'))
import ml_dtypes
from contextlib import ExitStack
import concourse.bass as bass
import concourse.mybir as mybir
from concourse.bass_utils import run_bass_kernel_spmd

F32 = mybir.dt.float32
BF16 = mybir.dt.bfloat16
I32 = mybir.dt.int32
AF = mybir.ActivationFunctionType
ALU = mybir.AluOpType
AX = mybir.AxisListType

EPOCH = 12000
COMPUTE = ('pe', 'act', 'dve', 'pool')
SAME_ENGINE_SYNC = ('act', 'dve', 'pool')


class Buf:
    __slots__ = ('name', 'w', 'r', 'dsem')

    def __init__(self, name):
        self.name = name
        self.w = None
        self.r = {}
        self.dsem = None


class DSem:
    __slots__ = ('sem', 'cnt', 'idx')

    def __init__(self, sem, idx):
        self.sem = sem
        self.cnt = 0
        self.idx = idx


class V:
    __slots__ = ('ap', 'bufs', 'dram')

    def __init__(self, ap, bufs, dram=False):
        self.ap = ap
        self.bufs = bufs
        self.dram = dram


class TT:
    def __init__(self, P, handle, name, shape, slot_axis=None, is_dram=False, nslots=None):
        self.P = P
        self.h = handle
        self.name = name
        self.shape = list(shape)
        self.slot_axis = slot_axis
        self.is_dram = is_dram
        if nslots is not None:
            self.bufs = [Buf("%s.%d" % (name, i)) for i in range(nslots)]
        elif slot_axis is None:
            self.bufs = [Buf(name)]
        else:
            self.bufs = [Buf("%s.%d" % (name, i)) for i in range(shape[slot_axis])]

    def base(self):
        return self.h.ap() if self.is_dram else self.h

    def __getitem__(self, idx):
        if not isinstance(idx, tuple):
            idx = (idx,)
        ap = self.base()[idx]
        if self.slot_axis is None:
            return V(ap, self.bufs, self.is_dram)
        if self.slot_axis < len(idx):
            s = idx[self.slot_axis]
            if isinstance(s, int):
                return V(ap, [self.bufs[s]], self.is_dram)
            if isinstance(s, slice):
                return V(ap, self.bufs[s], self.is_dram)
        return V(ap, self.bufs, self.is_dram)

    def view(self, ap, slots=None):
        if slots is None:
            return V(ap, self.bufs, self.is_dram)
        return V(ap, [self.bufs[s] for s in slots], self.is_dram)


class Prog:
    def __init__(self, nc, es, n_dsem=56):
        self.nc = nc
        self.es = es
        self.eng = {'pe': nc.tensor, 'act': nc.scalar, 'dve': nc.vector, 'pool': nc.gpsimd, 'sp': nc.sync}
        self.cnt = {e: 0 for e in COMPUTE}
        self.esems = {e: [] for e in COMPUTE}
        self.seen_e = {e: {c: 0 for c in COMPUTE} for e in self.eng}
        self.seen_d = {e: {} for e in self.eng}
        self.dpool = []
        for i in range(n_dsem):
            self.dpool.append(DSem(es.enter_context(nc.semaphore("dsem%d" % i)), i))
        self.dfree = list(self.dpool)
        self.dused = []
        self.n_instr = 0
        self.n_wait = 0
        self.psum = []
        self.psum_i = 0

    def sbuf(self, es, name, shape, dtype, slot_axis=None):
        self.uid = getattr(self, 'uid', 0) + 1
        name = "s%d_%s" % (self.uid, name)
        h = es.enter_context(self.nc.sbuf_tensor(name, list(shape), dtype))
        return TT(self, h, name, shape, slot_axis)

    def dram(self, name, shape, dtype, kind="Internal", slot_axis=None, nslots=None):
        h = self.nc.dram_tensor(name, list(shape), dtype, kind=kind)
        return TT(self, h, name, shape, slot_axis, is_dram=True, nslots=nslots)

    def init_psum(self, es, n=8):
        for i in range(n):
            h = es.enter_context(self.nc.psum_tensor("psb%d" % i, [128, 512], F32))
            self.psum.append(TT(self, h, "psb%d" % i, [128, 512]))

    def bank(self):
        t = self.psum[self.psum_i % len(self.psum)]
        self.psum_i += 1
        return t

    def _esem(self, e, epoch):
        while len(self.esems[e]) <= epoch:
            self.esems[e].append(self.es.enter_context(self.nc.semaphore("es_%s_%d" % (e, len(self.esems[e])))))
        return self.esems[e][epoch]

    def _wait(self, e, t):
        if t is None:
            return
        if t[0] == 'e':
            _, c, n = t
            if c == e and e not in SAME_ENGINE_SYNC:
                return
            if self.seen_e[e][c] >= n:
                return
            self.seen_e[e][c] = n
            epoch = (n - 1) // EPOCH
            self.eng[e].wait_ge(self._esem(c, epoch), n - epoch * EPOCH)
            self.n_wait += 1
        else:
            _, ds, n = t
            if self.seen_d[e].get(ds.idx, 0) >= n:
                return
            self.seen_d[e][ds.idx] = n
            self.eng[e].wait_ge(ds.sem, n)
            self.n_wait += 1

    @staticmethod
    def _tkey(t):
        return (t[0], t[1] if t[0] == 'e' else t[1].idx)

    def _deps(self, e, rbufs, wbufs):
        for b in rbufs:
            self._wait(e, b.w)
        for b in wbufs:
            self._wait(e, b.w)
            for t in list(b.r.values()):
                self._wait(e, t)

    def _record(self, t, rbufs, wbufs):
        k = self._tkey(t)
        for b in rbufs:
            b.r[k] = t
        for b in wbufs:
            b.w = t
            b.r = {}

    def op(self, e, fn, reads=(), writes=()):
        rbufs = [b for v in reads for b in v.bufs]
        wbufs = [b for v in writes for b in v.bufs]
        self._deps(e, rbufs, wbufs)
        ins = fn(self.eng[e])
        self.cnt[e] += 1
        n = self.cnt[e]
        ep = (n - 1) // EPOCH
        ins.then_inc(self._esem(e, ep), 1)
        self.seen_e[e][e] = max(self.seen_e[e][e], 0)
        self._record(('e', e, n), rbufs, wbufs)
        self.n_instr += 1
        return ins

    def _dsem_for(self, buf):
        if buf.dsem is None:
            if not self.dfree:
                raise RuntimeError("out of DMA semaphores")
            buf.dsem = self.dfree.pop()
            self.dused.append(buf)
        return buf.dsem

    def release_dsems(self, tts):
        for tt in tts:
            for b in tt.bufs:
                if b.dsem is not None:
                    self.dfree.append(b.dsem)
                    b.dsem = None
                    if b in self.dused:
                        self.dused.remove(b)

    def dma(self, q, out, in_, semv=None, first=True, **kw):
        if semv is None:
            semv = in_ if out.dram else out
        sb = semv.bufs[0]
        ds = self._dsem_for(sb)
        rbufs = list(in_.bufs)
        wbufs = list(out.bufs)
        if first:
            self._deps(q, rbufs, wbufs)
            if ds.cnt:
                self._wait(q, ('d', ds, ds.cnt))
        ins = self.eng[q].dma_start(out=out.ap, in_=in_.ap, **kw)
        ds.cnt += 16
        ins.then_inc(ds.sem, 16)
        self._record(('d', ds, ds.cnt), rbufs, wbufs)
        self.n_instr += 1
        return ins

    def barrier(self, engines=None):
        engines = engines or list(self.eng.keys())
        for e in engines:
            for c in COMPUTE:
                if self.cnt[c]:
                    self._wait(e, ('e', c, self.cnt[c]))
            for ds in self.dpool:
                if ds.cnt:
                    self._wait(e, ('d', ds, ds.cnt))

    def mm(self, out, lhsT, rhs, start=True, stop=True, sgc=False):
        if sgc:
            return self.op('pe', lambda e: e.matmul(out.ap, lhsT.ap, rhs.ap, start=start, stop=stop, skip_group_check=True),
                           reads=[lhsT, rhs], writes=[out])
        return self.op('pe', lambda e: e.matmul(out.ap, lhsT.ap, rhs.ap, start=start, stop=stop),
                       reads=[lhsT, rhs], writes=[out])

    def act(self, out, in_, func, bias=None, scale=None, eng='act'):
        kw = {}
        reads = [in_]
        if bias is not None:
            if isinstance(bias, V):
                kw['bias'] = bias.ap
                reads.append(bias)
            else:
                kw['bias'] = bias
        if scale is not None:
            if isinstance(scale, V):
                kw['scale'] = scale.ap
                reads.append(scale)
            else:
                kw['scale'] = scale
        return self.op(eng, lambda e: e.activation(out=out.ap, in_=in_.ap, func=func, **kw),
                       reads=reads, writes=[out])

    def tt(self, out, in0, in1, op, eng='dve'):
        return self.op(eng, lambda e: e.tensor_tensor(out=out.ap, in0=in0.ap, in1=in1.ap, op=op),
                       reads=[in0, in1], writes=[out])

    def ts(self, out, in0, s1, s2, op0, op1=None, eng='dve'):
        reads = [in0]
        a1 = s1
        a2 = s2
        if isinstance(s1, V):
            a1 = s1.ap
            reads.append(s1)
        if isinstance(s2, V):
            a2 = s2.ap
            reads.append(s2)
        if op1 is None:
            return self.op(eng, lambda e: e.tensor_scalar(out=out.ap, in0=in0.ap, scalar1=a1, scalar2=None, op0=op0),
                           reads=reads, writes=[out])
        return self.op(eng, lambda e: e.tensor_scalar(out=out.ap, in0=in0.ap, scalar1=a1, scalar2=a2, op0=op0, op1=op1),
                       reads=reads, writes=[out])

    def stt(self, out, in0, s, in1, op0, op1, eng='dve'):
        reads = [in0, in1]
        a = s
        if isinstance(s, V):
            a = s.ap
            reads.append(s)
        return self.op(eng, lambda e: e.scalar_tensor_tensor(out=out.ap, in0=in0.ap, scalar=a, in1=in1.ap, op0=op0, op1=op1),
                       reads=reads, writes=[out])

    def copy(self, out, in_, eng='dve'):
        if eng == 'act':
            return self.act(out, in_, AF.Identity)
        return self.op(eng, lambda e: e.tensor_copy(out=out.ap, in_=in_.ap), reads=[in_], writes=[out])

    def memset(self, out, val, eng='dve'):
        return self.op(eng, lambda e: e.memset(out.ap, val), reads=[], writes=[out])


D = 1024
NT = 2304
NL = 2048
CH = [(0, 512, 0), (512, 512, 0), (1024, 512, 0), (1536, 512, 0), (2048, 256, 1)]
EPS = 1e-6
NEG = -30000.0


class Ctx:
    pass


def build(L=4, stop=None, dbg=False, NE=32):
    nc = bass.Bass("TRN2", target_bir_lowering=False)
    es = ExitStack()
    P = Prog(nc, es)
    P.init_psum(es)
    C = Ctx()
    C.P = P
    C.nc = nc
    C.L = L
    C.NE = NE

    def din(name, shape, dt=F32):
        return P.dram(name, shape, dt, kind="ExternalInput")

    C.xT = din("xT", [D, NT])
    C.cc = din("cc", [128, 8, 2])
    C.w_mod = din("w_mod", [4, D, 6144])
    C.b_modT = din("b_modT", [4, 128, 48])
    C.gmix = din("gmix", [128, 4, 8])
    C.gffn = din("gffn", [128, 4, 8])
    C.gfin = din("gfin", [128, 8])
    C.w_in = din("w_in", [4, D, 6656])
    C.w_rot = din("w_rot", [4, D, 1024])
    C.ropeC = din("ropeC", [128, NL])
    C.ropeS = din("ropeS", [128, NL])
    C.da_lam = din("da_lam", [4, 256])
    C.subln = din("subln", [128, 4])
    C.naT = din("naT", [4, 16, 128, 512])
    C.w_branch = din("w_branch", [4, 3, 512, D])
    C.w_out = din("w_out", [4, D, D])
    C.w_router = din("w_router", [4, D, 32])
    C.b_router = din("b_router", [4, 32])
    C.w_gate_up = din("w_gate_up", [4, NE, D, 2048])
    C.b_guT = din("b_guT", [4, 128, 32, 16])
    C.w_down = din("w_down", [4, NE, D, D])
    C.b_down = din("b_down", [4, 32, D])
    C.cs128 = din("cs128", [128, 256])
    C.cn = din("cn", [NL, NL])
    C.sn = din("sn", [NL, NL])
    C.c256 = din("c256", [256, 256])
    C.s256 = din("s256", [256, 256])
    C.identd = din("ident", [128, 128])
    C.outT = P.dram("outT", [D, NL], F32, kind="ExternalOutput")
    skind = "ExternalOutput" if dbg else "Internal"
    C.XD = P.dram("XD", [D, NT], F32, kind=skind, nslots=5)
    C.OD = [P.dram("OD%d" % i, [512, NT], BF16, kind=skind, nslots=5) for i in range(3)]
    if dbg:
        C.HD = P.dram("HD", [D, NT], BF16, kind="ExternalOutput", nslots=5)

    def xd(c):
        t0, n, s = CH[c]
        return C.XD.view(C.XD.h.ap()[:, t0:t0 + n].rearrange("(k p) t -> p k t", p=128), slots=[c])
    C.xd = xd

    def od(i, c):
        t0, n, s = CH[c]
        return C.OD[i].view(C.OD[i].h.ap()[:, t0:t0 + n].rearrange("(k p) t -> p k t", p=128), slots=[c])
    C.od = od

    g = es
    C.ones_f = P.sbuf(g, "ones_f", [128, 128], F32)
    C.ones_b = P.sbuf(g, "ones_b", [128, 128], BF16)
    C.zeros_b = P.sbuf(g, "zeros_b", [128, 128], BF16)
    C.ident = P.sbuf(g, "ident", [128, 128], F32)
    C.ident_b = P.sbuf(g, "ident_b", [128, 128], BF16)
    C.sc = P.sbuf(g, "silu_c", [128, 8, 2], F32)
    C.modT = P.sbuf(g, "modT", [128, 2, 48], F32)
    C.A1 = P.sbuf(g, "A1", [128, 2, 8], F32)
    C.A2 = P.sbuf(g, "A2", [128, 2, 8], F32)
    C.gmix_s = P.sbuf(g, "gmix_s", [128, 4, 8], F32)
    C.gffn_s = P.sbuf(g, "gffn_s", [128, 4, 8], F32)
    C.gfin_s = P.sbuf(g, "gfin_s", [128, 8], F32)
    C.subln_s = P.sbuf(g, "subln_s", [128, 4], F32)
    C.lamneg = P.sbuf(g, "lamneg", [128, 1], F32)
    C.sgc = P.sbuf(g, "sgc", [128, 1], F32)

    P.memset(C.ones_f[:, :], 1.0)
    P.memset(C.ones_b[:, :], 1.0)
    P.memset(C.zeros_b[:, :], 0.0)
    P.dma('sp', C.ident[:, :], C.identd[:, :])
    P.copy(C.ident_b[:, :], C.ident[:, :])
    P.dma('sp', C.sc[:, :, :], C.cc[:, :, :])
    P.act(C.sc[:, :, :], C.sc[:, :, :], AF.Silu)
    P.dma('sp', C.gmix_s[:, :, :], C.gmix[:, :, :])
    P.dma('sp', C.gffn_s[:, :, :], C.gffn[:, :, :])
    P.dma('sp', C.gfin_s[:, :], C.gfin[:, :])
    P.dma('sp', C.subln_s[:, :], C.subln[:, :])
    for c in range(5):
        t0, n, s = CH[c]
        P.dma('sp', C.XD.view(C.XD.h.ap()[:, t0:t0 + n], slots=[c]), C.xT.view(C.xT.h.ap()[:, t0:t0 + n]),
              semv=C.XD.view(C.XD.h.ap()[:, t0:t0 + n], slots=[c]))

    def done(tag):
        return stop is not None and stop == tag

    fin = False
    for l in range(L):
        last = (l == 3)
        chunks = [0, 1, 2, 3] if last else [0, 1, 2, 3, 4]
        phase_mod(C, l)
        P.barrier()
        if done("mod%d" % l):
            fin = True
            break
        with ExitStack() as hs:
            C.HT = [P.sbuf(hs, "HT%d" % c, [128, 8, CH[c][1]], BF16) for c in range(5)]
            phase_h(C, l, 1, [0, 1, 2, 3, 4])
            P.barrier()
            if dbg:
                for c in range(5):
                    t0, n, s = CH[c]
                    P.dma('sp', C.HD.view(C.HD.h.ap()[:, t0:t0 + n].rearrange("(k p) t -> p k t", p=128), slots=[c]),
                          C.HT[c][:, :, :])
            for tag, fn in (("a", phase_a), ("b", phase_b), ("c", phase_c), ("out", phase_out)):
                if fin:
                    break
                if done("h%d" % l):
                    fin = True
                    break
                fn(C, l, chunks)
                P.barrier()
                if done("%s%d" % (tag, l)):
                    fin = True
            P.barrier()
            P.release_dsems(C.HT)
        if fin:
            break
        phase_moe(C, l, chunks)
        P.barrier()
        if done("moe%d" % l):
            fin = True
            break
    if stop is None:
        phase_final(C)
    P.barrier()
    C.es = es
    return nc, C


def phase_mod(C, l):
    P = C.P
    lam_init = 0.8 - 0.6 * math.exp(-0.3 * l)
    with ExitStack() as es:
        wm = [P.sbuf(es, "wm%d" % i, [128, 8, 512], F32) for i in range(2)]
        bm = P.sbuf(es, "bm", [128, 48], F32)
        lamt = P.sbuf(es, "lamt", [128, 256], F32)
        pr = P.sbuf(es, "lampr", [128, 128], F32)
        s12 = P.sbuf(es, "lams12", [128, 2], F32)
        e12 = P.sbuf(es, "lame12", [128, 2], F32)
        P.dma('sp', bm[:, :], C.b_modT.view(C.b_modT.h.ap()[l]))
        bank = P.psum[0]
        for blk in range(12):
            w = wm[blk % 2]
            P.dma('sp', w[:, :, :], C.w_mod.view(
                C.w_mod.h.ap()[l][:, blk * 512:(blk + 1) * 512].rearrange("(k p) c -> p k c", p=128)))
            for jj in range(4):
                j = blk * 4 + jj
                for k in range(8):
                    P.mm(bank[:, 2 * j:2 * j + 2], w[:, k, jj * 128:(jj + 1) * 128], C.sc[:, k, :],
                         start=(k == 0), stop=(k == 7))
        for s in range(2):
            P.tt(C.modT[:, s, :], bank[:, s:96:2], bm[:, :], ALU.add)
            P.stt(C.A1[:, s, :], C.modT[:, s, 8:16], 1.0, C.gmix_s[:, l, :], ALU.add, ALU.mult)
            P.stt(C.A2[:, s, :], C.modT[:, s, 32:40], 1.0, C.gffn_s[:, l, :], ALU.add, ALU.mult)
        P.dma('sp', lamt[:, :], C.da_lam.view(C.da_lam.h.ap()[l].partition_broadcast(128)))
        P.tt(pr[:, 0:64], lamt[:, 0:64], lamt[:, 64:128], ALU.mult)
        P.tt(pr[:, 64:128], lamt[:, 128:192], lamt[:, 192:256], ALU.mult)
        P.op('dve', lambda e: e.reduce_sum(out=s12[:, 0:1].ap, in_=pr[:, 0:64].ap, axis=AX.X),
             reads=[pr[:, :]], writes=[s12[:, :]])
        P.op('dve', lambda e: e.reduce_sum(out=s12[:, 1:2].ap, in_=pr[:, 64:128].ap, axis=AX.X),
             reads=[pr[:, :]], writes=[s12[:, :]])
        P.act(e12[:, :], s12[:, :], AF.Exp)
        P.tt(C.lamneg[:, :], e12[:, 1:2], e12[:, 0:1], ALU.subtract)
        P.ts(C.lamneg[:, :], C.lamneg[:, :], -lam_init, None, ALU.add)
        P.ts(C.sgc[:, :], C.subln_s[:, l:l + 1], (1.0 - lam_init), None, ALU.mult)
        P.barrier()
        P.release_dsems(wm + [bm, lamt])


def rstd_from_bank(P, r, bank, n, inv_n):
    P.ts(r[:, 0:n], bank[:, 0:n], inv_n, EPS, ALU.mult, ALU.add)
    P.act(r[:, 0:n], r[:, 0:n], AF.Sqrt)
    P.op('dve', lambda e: e.reciprocal(out=r[:, 0:n].ap, in_=r[:, 0:n].ap), reads=[r[:, 0:n]], writes=[r[:, 0:n]])


def phase_h(C, l, which, chunks):
    P = C.P
    with ExitStack() as es:
        xck = [P.sbuf(es, "hx%d" % i, [128, 8, 512], F32) for i in range(2)] if which == 1 else []
        sqt = [P.sbuf(es, "hsq%d" % i, [128, 512], F32) for i in range(2)]
        rs = [P.sbuf(es, "hrs%d" % i, [128, 512], F32) for i in range(2)]
        tmp = [P.sbuf(es, "htm%d" % i, [128, 512], F32) for i in range(2)]
        allt = xck + sqt + rs + tmp
        if which == 2:
            h2f = P.sbuf(es, "h2f", [128, 8, 512], F32)
            wr = P.sbuf(es, "wr", [128, 8, 32], F32)
            brt = P.sbuf(es, "brt", [128, 32], F32)
            lg = P.sbuf(es, "lg", [128, 32], F32)
            m8 = P.sbuf(es, "m8", [128, 8], F32)
            mask = P.sbuf(es, "rmask", [128, 32], F32)
            negm = P.sbuf(es, "negm", [128, 1], F32)
            ex = P.sbuf(es, "rex", [128, 32], F32)
            den = P.sbuf(es, "rden", [128, 1], F32)
            wt = P.sbuf(es, "rwt", [128, 32], F32)
            allt += [h2f, wr, brt]
            P.dma('sp', wr[:, :, :], C.w_router.view(C.w_router.h.ap()[l].rearrange("(k p) e -> p k e", p=128)))
            P.dma('sp', brt[:, :], C.b_router.view(C.b_router.h.ap()[l].partition_broadcast(128)))
        for ci, c in enumerate(chunks):
            t0, n, s = CH[c]
            if which == 1:
                xc = xck[ci % 2]
                P.dma('sp', xc[:, :, 0:n], C.xd(c))
                xsrc = xc
            else:
                xsrc = C.XT[c]
            bank = P.psum[ci % 2]
            for k in range(8):
                sq = sqt[k % 2]
                P.tt(sq[:, 0:n], xsrc[:, k, 0:n], xsrc[:, k, 0:n], ALU.mult, eng='pool')
                P.mm(bank[:, 0:n], C.ones_f[:, :], sq[:, 0:n], start=(k == 0), stop=(k == 7))
            r = rs[ci % 2]
            rstd_from_bank(P, r, bank, n, 1.0 / D)
            A = C.A1 if which == 1 else C.A2
            boff = 0 if which == 1 else 24
            for k in range(8):
                tm = tmp[k % 2]
                P.stt(tm[:, 0:n], xsrc[:, k, 0:n], A[:, s, k:k + 1], r[:, 0:n], ALU.mult, ALU.mult)
                bv = C.modT[:, s, boff + k:boff + k + 1]
                if which == 1:
                    P.act(C.HT[c][:, k, 0:n], tm[:, 0:n], AF.Identity, bias=bv)
                else:
                    P.act(h2f[:, k, 0:n], tm[:, 0:n], AF.Identity, bias=bv)
                    P.copy(C.H2T[c][:, k, 0:n], h2f[:, k, 0:n], eng='pool')
            if which == 2:
                for tl in range(n // 128):
                    tile = t0 // 128 + tl
                    bankR = P.psum[2 + (tile % 2)]
                    for k in range(8):
                        P.mm(bankR[:, 0:32], h2f[:, k, tl * 128:(tl + 1) * 128], wr[:, k, :],
                             start=(k == 0), stop=(k == 7))
                    P.tt(lg[:, :], bankR[:, 0:32], brt[:, :], ALU.add)
                    P.op('dve', lambda e: e.max(out=m8[:, :].ap, in_=lg[:, :].ap), reads=[lg[:, :]], writes=[m8[:, :]])
                    P.ts(mask[:, :], lg[:, :], m8[:, 3:4], None, ALU.is_ge)
                    P.ts(negm[:, :], m8[:, 0:1], -1.0, None, ALU.mult)
                    P.act(ex[:, :], lg[:, :], AF.Exp, bias=negm[:, 0:1])
                    P.tt(ex[:, :], ex[:, :], mask[:, :], ALU.mult)
                    P.op('dve', lambda e: e.reduce_sum(out=den[:, :].ap, in_=ex[:, :].ap, axis=AX.X),
                         reads=[ex[:, :]], writes=[den[:, :]])
                    P.op('dve', lambda e: e.reciprocal(out=den[:, :].ap, in_=den[:, :].ap),
                         reads=[den[:, :]], writes=[den[:, :]])
                    P.ts(wt[:, :], ex[:, :], den[:, 0:1], None, ALU.mult)
                    bankT = P.psum[4 + (tile % 2)]
                    P.mm(bankT[0:32, 0:128], wt[:, 0:32], C.ident[:, :])
                    P.copy(C.WT[0:32, tile * 128:(tile + 1) * 128], bankT[0:32, 0:128])
        P.barrier()
        P.release_dsems(allt)


def load_w(P, dst, src_tt, ap, q='pool'):
    return P.dma(q, dst, src_tt.view(ap))


def proj_fm(P, bank, W, col0, HTc, n):
    for k in range(8):
        P.mm(bank[:, 0:n], W[:, k, col0:col0 + 128], HTc[:, k, 0:n], start=(k == 0), stop=(k == 7))


def rope_evac(C, out, bank, bankP, t0, n, t1, t2):
    P = C.P
    P.tt(t1[:, 0:n], bank[:, 0:n], C.ropeC_s[:, t0:t0 + n], ALU.mult)
    P.tt(t2[:, 0:n], bankP[:, 0:n], C.ropeS_s[:, t0:t0 + n], ALU.mult)
    P.tt(out, t1[:, 0:n], t2[:, 0:n], ALU.add, eng='pool')


def phase_a(C, l, chunks):
    P = C.P
    win = C.w_in.h.ap()[l]
    wrot = C.w_rot.h.ap()[l]
    kp = "(k p) c -> p k c"
    with ExitStack() as es:
        KT = P.sbuf(es, "KT", [128, 4, NT], BF16, slot_axis=1)
        Vt = P.sbuf(es, "Vt", [128, 18, 512], BF16, slot_axis=1)
        C.ropeC_s = P.sbuf(es, "ropeC_s", [128, NL], F32)
        C.ropeS_s = P.sbuf(es, "ropeS_s", [128, NL], F32)
        t1 = P.sbuf(es, "ropet1", [128, 512], F32)
        t2 = P.sbuf(es, "ropet2", [128, 512], F32)
        P.dma('sp', C.ropeC_s[:, :], C.ropeC[:, :])
        P.dma('sp', C.ropeS_s[:, :], C.ropeS[:, :])
        rel = [KT, Vt, C.ropeC_s, C.ropeS_s]
        with ExitStack() as e1:
            Wk = P.sbuf(e1, "Wk", [128, 8, 512], BF16)
            WkP = P.sbuf(e1, "WkP", [128, 8, 512], BF16)
            Wv = P.sbuf(e1, "Wv", [128, 8, 512], BF16)
            load_w(P, Wk[:, :, :], C.w_in, win[:, 512:1024].rearrange(kp, p=128))
            load_w(P, WkP[:, :, :], C.w_rot, wrot[:, 512:1024].rearrange(kp, p=128))
            load_w(P, Wv[:, :, :], C.w_in, win[:, 1024:1536].rearrange(kp, p=128))
            it = 0
            for c in range(5):
                t0, n, s = CH[c]
                for h in range(4):
                    bk = P.psum[(2 * it) % 8]
                    bp = P.psum[(2 * it + 1) % 8]
                    it += 1
                    proj_fm(P, bk, Wk, h * 128, C.HT[c], n)
                    if s == 0:
                        proj_fm(P, bp, WkP, h * 128, C.HT[c], n)
                        rope_evac(C, KT[:, h, t0:t0 + n], bk, bp, t0, n, t1, t2)
                    else:
                        P.copy(KT[:, h, t0:t0 + n], bk[:, 0:n], eng='act')
                for tl in range(n // 128):
                    tile = t0 // 128 + tl
                    bv = P.psum[(2 * it) % 8]
                    it += 1
                    for k in range(8):
                        P.mm(bv[:, 0:512], C.HT[c][:, k, tl * 128:(tl + 1) * 128], Wv[:, k, :],
                             start=(k == 0), stop=(k == 7))
                    P.copy(Vt[:, tile, :], bv[:, 0:512], eng='act')
            P.barrier()
            P.release_dsems([Wk, WkP, Wv])
        with ExitStack() as e2:
            Wq = P.sbuf(e2, "Wq", [128, 8, 512], BF16)
            WqP = P.sbuf(e2, "WqP", [128, 8, 512], BF16)
            load_w(P, Wq[:, :, :], C.w_in, win[:, 0:512].rearrange(kp, p=128))
            load_w(P, WqP[:, :, :], C.w_rot, wrot[:, 0:512].rearrange(kp, p=128))
            QT = [P.sbuf(e2, "QT%d" % i, [128, 4, 512], BF16) for i in range(2)]
            pT = [P.sbuf(e2, "pT%d" % i, [128, 512], BF16) for i in range(3)]
            rsum = P.sbuf(e2, "a_rsum", [128, 512], F32)
            om = [P.sbuf(e2, "a_om%d" % i, [128, 512], F32) for i in range(2)]
            odt = P.sbuf(e2, "a_od", [128, 512], F32)
            sq = P.sbuf(e2, "a_sq", [128, 512], F32)
            rst = P.sbuf(e2, "a_rst", [128, 512], F32)
            OA = [P.sbuf(e2, "OA%d" % i, [128, 4, 512], BF16) for i in range(2)]
            hm = 0
            for ci, c in enumerate(chunks):
                t0, n, s = CH[c]
                q = QT[ci % 2]
                oa = OA[ci % 2]
                for h in range(4):
                    bk = P.psum[7]
                    bp = P.psum[6]
                    proj_fm(P, bk, Wq, h * 128, C.HT[c], n)
                    if s == 0:
                        proj_fm(P, bp, WqP, h * 128, C.HT[c], n)
                        rope_evac(C, q[:, h, 0:n], bk, bp, t0, n, t1, t2)
                    else:
                        P.copy(q[:, h, 0:n], bk[:, 0:n], eng='act')
                tiles = list(range(18)) if s == 0 else [16, 17]
                for h in range(4):
                    for m in range(2):
                        bO = P.psum[3 + (hm % 2)]
                        bS = P.psum[5]
                        hm += 1
                        pb = slice(m * 64, (m + 1) * 64)

                        def st(i):
                            kt = tiles[i]
                            P.mm(P.psum[i % 3][:, 0:n], KT[pb, h, kt * 128:(kt + 1) * 128], q[pb, h, 0:n])
                        st(0)
                        for i, kt in enumerate(tiles):
                            if i + 1 < len(tiles):
                                st(i + 1)
                            p = pT[i % 3]
                            P.act(p[:, 0:n], P.psum[i % 3][:, 0:n], AF.Exp, scale=0.125)
                            P.mm(bO[:, 0:n], Vt[:, kt, h * 128:(h + 1) * 128], p[:, 0:n],
                                 start=(i == 0), stop=(i == len(tiles) - 1))
                            P.mm(bS[:, 0:n], C.ones_b[:, :], p[:, 0:n],
                                 start=(i == 0), stop=(i == len(tiles) - 1))
                        P.op('dve', lambda e: e.reciprocal(out=rsum[:, 0:n].ap, in_=bS[:, 0:n].ap),
                             reads=[bS[:, 0:n]], writes=[rsum[:, 0:n]])
                        P.tt(om[m][:, 0:n], bO[:, 0:n], rsum[:, 0:n], ALU.mult)
                    P.stt(odt[:, 0:n], om[1][:, 0:n], C.lamneg[:, 0:1], om[0][:, 0:n], ALU.mult, ALU.add)
                    P.tt(sq[:, 0:n], odt[:, 0:n], odt[:, 0:n], ALU.mult, eng='pool')
                    bN = P.psum[6]
                    P.mm(bN[:, 0:n], C.ones_f[:, :], sq[:, 0:n])
                    rstd_from_bank(P, rst, bN, n, 1.0 / 128)
                    P.stt(oa[:, h, 0:n], odt[:, 0:n], C.sgc[:, 0:1], rst[:, 0:n], ALU.mult, ALU.mult)
                P.dma('sp', C.od(0, c), oa[:, :, 0:n])
            P.barrier()
            P.release_dsems([Wq, WqP] + QT + OA)
        P.release_dsems(rel)


def na_tiles(r):
    rs_ = min(max(r - 4, 0), 24)
    out = []
    for a in range(rs_ // 2, (rs_ + 7) // 2 + 1):
        r0, r1 = 2 * a, 2 * a + 1
        v0 = rs_ <= r0 < rs_ + 8
        v1 = rs_ <= r1 < rs_ + 8
        if v0 and v1:
            idx = r0 - r + 7
            assert 0 <= idx <= 13
        elif v1:
            assert r1 - r + 7 == 3
            idx = 14
        else:
            assert v0 and r0 - r + 7 == 10
            idx = 15
        out.append((a, idx))
    return out


def phase_b(C, l, chunks):
    P = C.P
    win = C.w_in.h.ap()[l]
    kp = "(k p) c -> p k c"
    with ExitStack() as es:
        KT = P.sbuf(es, "KN", [128, 4, NT], BF16, slot_axis=1)
        Vt = P.sbuf(es, "VN", [128, 18, 512], BF16, slot_axis=1)
        NAT = P.sbuf(es, "NAT", [128, 16, 512], BF16)
        for i0 in range(0, 16, 4):
            load_w(P, NAT[:, i0:i0 + 4, :], C.naT, C.naT.h.ap()[l][i0:i0 + 4].rearrange("i p c -> p i c"))
        rel = [KT, Vt, NAT]
        with ExitStack() as e1:
            Wk = P.sbuf(e1, "Wkn", [128, 8, 512], BF16)
            Wv = P.sbuf(e1, "Wvn", [128, 8, 512], BF16)
            load_w(P, Wk[:, :, :], C.w_in, win[:, 2048:2560].rearrange(kp, p=128))
            load_w(P, Wv[:, :, :], C.w_in, win[:, 2560:3072].rearrange(kp, p=128))
            it = 0
            for c in range(5):
                t0, n, s = CH[c]
                for h in range(4):
                    bk = P.psum[it % 8]
                    it += 1
                    proj_fm(P, bk, Wk, h * 128, C.HT[c], n)
                    P.copy(KT[:, h, t0:t0 + n], bk[:, 0:n], eng=('act' if h % 2 else 'dve'))
                for tl in range(n // 128):
                    tile = t0 // 128 + tl
                    bv = P.psum[it % 8]
                    it += 1
                    for k in range(8):
                        P.mm(bv[:, 0:512], C.HT[c][:, k, tl * 128:(tl + 1) * 128], Wv[:, k, :],
                             start=(k == 0), stop=(k == 7))
                    P.copy(Vt[:, tile, :], bv[:, 0:512], eng=('act' if tl % 2 else 'dve'))
            P.barrier()
            P.release_dsems([Wk, Wv])
        with ExitStack() as e2:
            Wq = P.sbuf(e2, "Wqn", [128, 8, 512], BF16)
            load_w(P, Wq[:, :, :], C.w_in, win[:, 1536:2048].rearrange(kp, p=128))
            QT = [P.sbuf(e2, "QN%d" % i, [128, 4, 512], BF16) for i in range(2)]
            pT = [P.sbuf(e2, "pN%d" % i, [128, 512], BF16) for i in range(3)]
            rsum = P.sbuf(e2, "b_rsum", [128, 512], F32)
            OB = [P.sbuf(e2, "OB%d" % i, [128, 4, 512], BF16) for i in range(2)]
            rowi = 0
            for ci, c in enumerate(chunks):
                t0, n, s = CH[c]
                q = QT[ci % 2]
                ob = OB[ci % 2]
                for h in range(4):
                    bk = P.psum[7]
                    proj_fm(P, bk, Wq, h * 128, C.HT[c], n)
                    P.act(q[:, h, 0:n], bk[:, 0:n], AF.Identity, scale=0.125)
                for rr in range(n // 64):
                    rq = rr * 64
                    if s == 0:
                        r = t0 // 64 + rr
                        tiles = na_tiles(r) + [(16, None), (17, None)]
                    else:
                        tiles = [(16, None), (17, None)]
                    bO = P.psum[4 + (rowi % 2)]
                    bS = P.psum[6]
                    rowi += 1

                    def st(i):
                        a, idx = tiles[i]
                        for par in range(2):
                            bk_ = P.psum[(i % 2) * 2 + par]
                            pb = slice(par * 64, par * 64 + 64)
                            if idx is not None:
                                P.mm(bk_[:, 0:256], C.ident_b[:, :], NAT[:, idx, par * 256:(par + 1) * 256],
                                     start=True, stop=False, sgc=True)
                            for cc in range(4):
                                P.mm(bk_[:, cc * 64:(cc + 1) * 64], KT[pb, cc, a * 128:(a + 1) * 128],
                                     q[pb, cc, rq:rq + 64], start=(idx is None), stop=True, sgc=True)
                    st(0)
                    nt_ = len(tiles)
                    for i, (a, idx) in enumerate(tiles):
                        if i + 1 < nt_:
                            st(i + 1)
                        p = pT[i % 3]
                        for par in range(2):
                            P.act(p[:, par * 256:(par + 1) * 256], P.psum[(i % 2) * 2 + par][:, 0:256], AF.Exp)
                        if i == 0:
                            P.mm(bO[:, 0:512], C.zeros_b[:, :], p[:, :], start=True, stop=False, sgc=True)
                        for par in range(2):
                            for cc in range(4):
                                co = par * 256 + cc * 64
                                P.mm(bO[:, co:co + 64], Vt[:, a, cc * 128:(cc + 1) * 128],
                                     p[:, co:co + 64], start=False, stop=(i == nt_ - 1), sgc=True)
                        P.mm(bS[:, 0:512], C.ones_b[:, :], p[:, :], start=(i == 0), stop=(i == nt_ - 1))
                    P.op('dve', lambda e: e.reciprocal(out=rsum[:, :].ap, in_=bS[:, 0:512].ap),
                         reads=[bS[:, 0:512]], writes=[rsum[:, :]])
                    for par in range(2):
                        pb = slice(par * 64, par * 64 + 64)
                        o_v = ob.view(ob.h[pb, :, rq:rq + 64])
                        b_v = bO.view(bO.h[pb, par * 256:(par + 1) * 256].rearrange("p (c q) -> p c q", c=4))
                        r_v = rsum.view(rsum.h[pb, par * 256:(par + 1) * 256].rearrange("p (c q) -> p c q", c=4))
                        P.tt(o_v, b_v, r_v, ALU.mult)
                P.dma('sp', C.od(1, c), ob[:, :, 0:n])
            P.barrier()
            P.release_dsems([Wq] + QT + OB)
        P.release_dsems(rel)


def phase_c(C, l, chunks):
    P = C.P
    win = C.w_in.h.ap()[l]
    kp = "(k p) c -> p k c"
    with ExitStack() as es:
        AB = P.sbuf(es, "AB", [128, 18, 4, 256], BF16, slot_axis=1)
        CS = P.sbuf(es, "CS128", [128, 256], BF16)
        Wf = P.sbuf(es, "Wf", [128, 8, 512], BF16)
        fT = [P.sbuf(es, "fT%d" % i, [128, 4, 512], BF16) for i in range(2)]
        CNk = P.sbuf(es, "CNk", [128, 16, 512], BF16)
        SNk = P.sbuf(es, "SNk", [128, 16, 512], BF16)
        OC = [P.sbuf(es, "OC%d" % i, [128, 4, 512], BF16) for i in range(2)]
        load_w(P, CS[:, :], C.cs128, C.cs128.h.ap())
        load_w(P, Wf[:, :, :], C.w_in, win[:, 3072:3584].rearrange(kp, p=128))
        it = 0
        for c in range(5):
            t0, n, s = CH[c]
            if s == 1 and 4 not in chunks:
                continue
            f = fT[c % 2]
            for g_ in range(4):
                bk = P.psum[it % 4]
                it += 1
                proj_fm(P, bk, Wf, g_ * 128, C.HT[c], n)
                P.copy(f[:, g_, 0:n], bk[:, 0:n], eng=('act' if g_ % 2 else 'dve'))
            for tl in range(n // 128):
                tile = t0 // 128 + tl
                for half in range(2):
                    bk = P.psum[4 + (it % 4)]
                    it += 1
                    for gg in range(2):
                        g_ = half * 2 + gg
                        P.mm(bk[:, gg * 256:(gg + 1) * 256], f[:, g_, tl * 128:(tl + 1) * 128], CS[:, :])
                    P.copy(AB.view(AB.h[:, tile, half * 2:half * 2 + 2, :], slots=[tile]),
                           bk.view(bk.h[:, 0:512].rearrange("p (g m) -> p g m", g=2)),
                           eng=('act' if half else 'dve'))
        for kc in range(4):
            for t4 in range(0, 16, 4):
                load_w(P, CNk[:, t4:t4 + 4, :], C.cn,
                       C.cn.h.ap()[t4 * 128:(t4 + 4) * 128, kc * 512:(kc + 1) * 512].rearrange("(t p) c -> p t c", p=128))
                load_w(P, SNk[:, t4:t4 + 4, :], C.sn,
                       C.sn.h.ap()[t4 * 128:(t4 + 4) * 128, kc * 512:(kc + 1) * 512].rearrange("(t p) c -> p t c", p=128))
            oc = OC[kc % 2]
            for g_ in range(4):
                bk = P.psum[g_ % 4]
                for nt_ in range(16):
                    P.mm(bk[:, 0:512], AB[:, nt_, g_, 0:128], CNk[:, nt_, :], start=(nt_ == 0), stop=False)
                    P.mm(bk[:, 0:512], AB[:, nt_, g_, 128:256], SNk[:, nt_, :], start=False, stop=(nt_ == 15))
                P.copy(oc[:, g_, :], bk[:, 0:512], eng=('act' if g_ % 2 else 'dve'))
            P.dma('sp', C.od(2, kc), oc[:, :, :])
        if 4 in chunks:
            P.barrier()
            load_w(P, CNk[:, 0:2, 0:256], C.c256, C.c256.h.ap().rearrange("(t p) c -> p t c", p=128))
            load_w(P, SNk[:, 0:2, 0:256], C.s256, C.s256.h.ap().rearrange("(t p) c -> p t c", p=128))
            oc = OC[0]
            for g_ in range(4):
                bk = P.psum[g_ % 4]
                for nt_ in range(2):
                    P.mm(bk[:, 0:256], AB[:, 16 + nt_, g_, 0:128], CNk[:, nt_, 0:256], start=(nt_ == 0), stop=False)
                    P.mm(bk[:, 0:256], AB[:, 16 + nt_, g_, 128:256], SNk[:, nt_, 0:256], start=False, stop=(nt_ == 1))
                P.copy(oc[:, g_, 0:256], bk[:, 0:256], eng=('act' if g_ % 2 else 'dve'))
            P.dma('sp', C.od(2, 4), oc[:, :, 0:256])
        P.barrier()
        P.release_dsems([AB, CS, Wf, CNk, SNk] + fT + OC)


def phase_out(C, l, chunks):
    P = C.P
    win = C.w_in.h.ap()[l]
    with ExitStack() as es:
        MG = [P.sbuf(es, "MG%d" % c, [128, 8, CH[c][1]], BF16) for c in range(5)]
        Oi = [P.sbuf(es, "Oi%d" % c, [128, 4, CH[c][1]], BF16) for c in range(5)]
        Wb = [P.sbuf(es, "Wb%d" % i, [128, 4, 128], BF16) for i in range(2)]
        Wg = [P.sbuf(es, "Wg%d" % i, [128, 8, 128], BF16) for i in range(2)]
        sg = [P.sbuf(es, "sg%d" % i, [128, 512], F32) for i in range(2)]
        tm = [P.sbuf(es, "otm%d" % i, [128, 512], F32) for i in range(2)]
        it = 0
        for i in range(3):
            for c in chunks:
                t0, n, s = CH[c]
                P.dma('sp', Oi[c][:, :, :], C.od(i, c))
            for dc in range(8):
                wb = Wb[it % 2]
                wg = Wg[it % 2]
                load_w(P, wb[:, :, :], C.w_branch,
                       C.w_branch.h.ap()[l, i][:, dc * 128:(dc + 1) * 128].rearrange("(k p) c -> p k c", p=128))
                gc0 = 3584 + i * 1024 + dc * 128
                load_w(P, wg[:, :, :], C.w_in, win[:, gc0:gc0 + 128].rearrange("(k p) c -> p k c", p=128))
                for c in chunks:
                    t0, n, s = CH[c]
                    bP = P.psum[(2 * it) % 8]
                    bG = P.psum[(2 * it + 1) % 8]
                    it += 1
                    for k in range(4):
                        P.mm(bP[:, 0:n], wb[:, k, :], Oi[c][:, k, 0:n], start=(k == 0), stop=(k == 3))
                    for k in range(8):
                        P.mm(bG[:, 0:n], wg[:, k, :], C.HT[c][:, k, 0:n], start=(k == 0), stop=(k == 7))
                    s_ = sg[it % 2]
                    P.act(s_[:, 0:n], bG[:, 0:n], AF.Sigmoid)
                    if i == 0:
                        P.tt(MG[c][:, dc, 0:n], bP[:, 0:n], s_[:, 0:n], ALU.mult)
                    else:
                        t_ = tm[it % 2]
                        P.tt(t_[:, 0:n], bP[:, 0:n], s_[:, 0:n], ALU.mult)
                        P.tt(MG[c][:, dc, 0:n], MG[c][:, dc, 0:n], t_[:, 0:n], ALU.add, eng='pool')
        P.barrier()
        P.release_dsems(Oi + Wb + Wg)
        with ExitStack() as e2:
            Wo = P.sbuf(e2, "Wo", [128, 8, D], BF16)
            xck = [P.sbuf(e2, "ox%d" % i, [128, 8, 512], F32) for i in range(2)]
            load_w(P, Wo[:, :, :], C.w_out, C.w_out.h.ap()[l].rearrange("(k p) c -> p k c", p=128))
            it = 0
            for ci, c in enumerate(chunks):
                t0, n, s = CH[c]
                xc = xck[ci % 2]
                P.dma('sp', xc[:, :, 0:n], C.xd(c))
                for dc in range(8):
                    bk = P.psum[it % 8]
                    it += 1
                    for k in range(8):
                        P.mm(bk[:, 0:n], Wo[:, k, dc * 128:(dc + 1) * 128], MG[c][:, k, 0:n],
                             start=(k == 0), stop=(k == 7))
                    P.stt(xc[:, dc, 0:n], bk[:, 0:n], C.modT[:, s, 16 + dc:17 + dc], xc[:, dc, 0:n], ALU.mult, ALU.add)
                P.dma('sp', C.xd(c), xc[:, :, 0:n])
            P.barrier()
            P.release_dsems([Wo] + xck)


def phase_moe(C, l, chunks):
    P = C.P
    with ExitStack() as es:
        C.XT = [P.sbuf(es, "XT%d" % c, [128, 8, CH[c][1]], F32) for c in range(5)]
        C.H2T = [P.sbuf(es, "H2T%d" % c, [128, 8, CH[c][1]], BF16) for c in range(5)]
        C.WT = P.sbuf(es, "WT", [32, NT], F32)
        for c in chunks:
            P.dma('sp', C.XT[c][:, :, :], C.xd(c))
        phase_h(C, l, 2, chunks)
        wmk = [P.sbuf(es, "wmk%d" % i, [32, 512], F32) for i in range(2)]
        bdn = P.sbuf(es, "bdn", [32, D], F32)
        bgu = P.sbuf(es, "bgu", [128, 32, 16], F32)
        P.dma('sp', bdn[:, :], C.b_down.view(C.b_down.h.ap()[l]))
        P.dma('sp', bgu[:, :, :], C.b_guT.view(C.b_guT.h.ap()[l]))
        bgu1 = P.sbuf(es, "bgu1", [128, 32, 8], F32)
        P.ts(bgu1[:, :, :], bgu[:, :, 8:16], 1.0, None, ALU.add)
        actT = [P.sbuf(es, "actT%d" % c, [128, 2, CH[c][1]], BF16) for c in range(5)]
        wgu = [P.sbuf(es, "wgu%d" % i, [128, 8, 2, 256], BF16) for i in range(2)]
        wd = [P.sbuf(es, "wd%d" % i, [128, 2, D], BF16) for i in range(2)]
        g1 = [P.sbuf(es, "g1_%d" % i, [128, 512], F32) for i in range(2)]
        sgm = [P.sbuf(es, "sgm%d" % i, [128, 512], F32) for i in range(2)]
        u0 = [P.sbuf(es, "u0_%d" % i, [128, 512], F32) for i in range(2)]
        tq = [P.sbuf(es, "tq%d" % i, [128, 512], F32) for i in range(2)]
        it = 0
        for c in chunks:
            t0, n, s = CH[c]
            for dc in range(8):
                bk = P.psum[it % 2]
                it += 1
                P.mm(bk[:, 0:n], bdn[0:32, dc * 128:(dc + 1) * 128], C.WT[0:32, t0:t0 + n])
                P.stt(C.XT[c][:, dc, 0:n], bk[:, 0:n], C.modT[:, s, 40 + dc:41 + dc], C.XT[c][:, dc, 0:n],
                      ALU.mult, ALU.add)
        wgu_ap = C.w_gate_up.h.ap()
        wd_ap = C.w_down.h.ap()
        qi = 0
        u = 0
        for e in range(C.NE):
            for qq in range(4):
                wg = wgu[qi % 2]
                wdn = wd[qi % 2]
                qi += 1
                c0 = qq * 256
                P.dma('pool', wg[:, :, 0, :], C.w_gate_up.view(
                    wgu_ap[l, e][:, c0:c0 + 256].rearrange("(k p) c -> p k c", p=128)))
                P.dma('pool', wg[:, :, 1, :], C.w_gate_up.view(
                    wgu_ap[l, e][:, 1024 + c0:1024 + c0 + 256].rearrange("(k p) c -> p k c", p=128)), first=False)
                P.dma('pool', wdn[:, :, :], C.w_down.view(
                    wd_ap[l, e][c0:c0 + 256, :].rearrange("(j p) c -> p j c", p=128)))
                for c in chunks:
                    t0, n, s = CH[c]
                    bW = P.psum[6 + (u % 2)]
                    wm_ = wmk[u % 2]
                    P.ts(wm_[0:32, 0:n], C.WT[0:32, t0:t0 + n], C.ident[0:32, e:e + 1], None, ALU.mult)
                    P.mm(bW[:, 0:n], C.ones_f[0:32, :], wm_[0:32, 0:n])
                    for j in range(2):
                        ffc = qq * 2 + j
                        bG = P.psum[(2 * u) % 4]
                        bU = P.psum[(2 * u + 1) % 4]
                        for k in range(8):
                            P.mm(bG[:, 0:n], wg[:, k, 0, j * 128:(j + 1) * 128], C.H2T[c][:, k, 0:n],
                                 start=(k == 0), stop=(k == 7))
                        for k in range(8):
                            P.mm(bU[:, 0:n], wg[:, k, 1, j * 128:(j + 1) * 128], C.H2T[c][:, k, 0:n],
                                 start=(k == 0), stop=(k == 7))
                        g_ = g1[u % 2]
                        s_ = sgm[u % 2]
                        u_ = u0[u % 2]
                        t_ = tq[u % 2]
                        P.ts(g_[:, 0:n], bG[:, 0:n], bgu[:, e, ffc:ffc + 1], 7.0, ALU.add, ALU.min)
                        P.act(s_[:, 0:n], g_[:, 0:n], AF.Sigmoid, scale=1.702)
                        P.act(u_[:, 0:n], bU[:, 0:n], AF.Identity, bias=bgu1[:, e, ffc:ffc + 1])
                        P.ts(u_[:, 0:n], u_[:, 0:n], 8.0, -6.0, ALU.min, ALU.max, eng='pool')
                        P.tt(t_[:, 0:n], g_[:, 0:n], s_[:, 0:n], ALU.mult, eng='pool')
                        P.tt(t_[:, 0:n], t_[:, 0:n], u_[:, 0:n], ALU.mult, eng='pool')
                        P.tt(actT[c][:, j, 0:n], t_[:, 0:n], bW[:, 0:n], ALU.mult)
                        u += 1
                for c in chunks:
                    t0, n, s = CH[c]
                    for dc in range(8):
                        bD = P.psum[4 + (u % 2)]
                        u += 1
                        for j in range(2):
                            P.mm(bD[:, 0:n], wdn[:, j, dc * 128:(dc + 1) * 128], actT[c][:, j, 0:n],
                                 start=(j == 0), stop=(j == 1))
                        P.stt(C.XT[c][:, dc, 0:n], bD[:, 0:n], C.modT[:, s, 40 + dc:41 + dc], C.XT[c][:, dc, 0:n],
                              ALU.mult, ALU.add)
        for c in chunks:
            P.dma('sp', C.xd(c), C.XT[c][:, :, :])
        P.barrier()
        P.release_dsems(C.XT + C.H2T + [C.WT, bdn, bgu] + actT + wgu + wd)


def phase_final(C):
    P = C.P
    with ExitStack() as es:
        xck = [P.sbuf(es, "fx%d" % i, [128, 8, 512], F32) for i in range(2)]
        sqt = [P.sbuf(es, "fsq%d" % i, [128, 512], F32) for i in range(2)]
        rs = [P.sbuf(es, "frs%d" % i, [128, 512], F32) for i in range(2)]
        for c in range(4):
            t0, n, s = CH[c]
            xc = xck[c % 2]
            P.dma('sp', xc[:, :, :], C.xd(c))
            bank = P.psum[c % 2]
            for k in range(8):
                sq = sqt[k % 2]
                P.tt(sq[:, :], xc[:, k, :], xc[:, k, :], ALU.mult, eng='pool')
                P.mm(bank[:, 0:n], C.ones_f[:, :], sq[:, :], start=(k == 0), stop=(k == 7))
            r = rs[c % 2]
            rstd_from_bank(P, r, bank, n, 1.0 / D)
            for k in range(8):
                P.stt(xc[:, k, :], xc[:, k, :], C.gfin_s[:, k:k + 1], r[:, :], ALU.mult, ALU.mult)
            P.dma('sp', C.outT.view(C.outT.h.ap()[:, t0:t0 + n].rearrange("(k p) t -> p k t", p=128)), xc[:, :, :])
        P.barrier()
        P.release_dsems(xck)


def _rope_tables():
    t = np.arange(NL)
    row = (t // 64).astype(np.float32)
    col = (t % 64).astype(np.float32)
    freqs = (np.float32(10000.0) ** (-np.arange(16, dtype=np.float32) / np.float32(16))).astype(np.float32)
    Ct = np.zeros((128, NL), np.float32)
    St = np.zeros((128, NL), np.float32)
    for p in range(128):
        d = p % 64
        axis = d // 32
        half = (d % 32) // 16
        i = d % 16
        ang = (row if axis == 0 else col) * freqs[i]
        Ct[p] = np.cos(ang)
        St[p] = (-1.0 if half == 0 else 1.0) * np.sin(ang)
    return Ct, St


def _rot_cols():
    idx = np.arange(512)
    d = idx % 64
    return (idx - d) + (d ^ 16)


def _dft(n, scale):
    k = np.arange(n, dtype=np.int64)
    ph = (np.outer(k, k) % n).astype(np.float64) * (2.0 * np.pi / n)
    return (np.cos(ph) * scale).astype(np.float32), (np.sin(ph) * scale).astype(np.float32)


def _na_tables(rpb):
    q = np.arange(64)
    k = np.arange(64)
    cs = np.clip(q - 8, 0, 48)
    mask = (k[None, :] >= cs[:, None]) & (k[None, :] < cs[:, None] + 16)
    dc = np.clip(k[None, :] - q[:, None], -15, 15) + 15
    Bt = rpb[:, :, :, dc]
    Bt = np.where(mask[None, None, None], Bt, np.float32(NEG)).astype(np.float32)
    Bt = Bt.transpose(0, 2, 4, 1, 3)
    Bt = np.ascontiguousarray(Bt[:, :, :, [0, 2, 4, 6, 1, 3, 5, 7], :]).reshape(4, 15, 64, 512)
    M = np.full((4, 64, 512), NEG, np.float32)
    T = np.empty((4, 16, 128, 512), np.float32)
    for i in range(14):
        T[:, i, 0:64] = Bt[:, i]
        T[:, i, 64:128] = Bt[:, i + 1]
    T[:, 14, 0:64] = M
    T[:, 14, 64:128] = Bt[:, 3]
    T[:, 15, 0:64] = Bt[:, 10]
    T[:, 15, 64:128] = M
    return T


def prep_shared(inp):
    f = lambda a: np.ascontiguousarray(np.asarray(a, dtype=np.float32))
    sh = {}
    sh["w_mod"] = f(inp["w_mod"])
    sh["b_modT"] = f(np.asarray(inp["b_mod"]).reshape(4, 48, 128).transpose(0, 2, 1))
    sh["gmix"] = f(np.asarray(inp["norm_mix_g"]).reshape(4, 8, 128).transpose(2, 0, 1))
    sh["gffn"] = f(np.asarray(inp["norm_ffn_g"]).reshape(4, 8, 128).transpose(2, 0, 1))
    sh["gfin"] = f(np.asarray(inp["final_g"]).reshape(8, 128).T)
    w_in = f(inp["w_in"])
    sh["w_in"] = w_in
    rc = _rot_cols()
    sh["w_rot"] = f(np.concatenate([w_in[:, :, 0:512][:, :, rc], w_in[:, :, 512:1024][:, :, rc]], axis=2))
    Ct, St = _rope_tables()
    sh["ropeC"] = Ct
    sh["ropeS"] = St
    sh["da_lam"] = f(np.asarray(inp["da_lambda"]).reshape(4, 256))
    sh["subln"] = f(np.asarray(inp["da_subln_g"]).T)
    sh["naT"] = _na_tables(np.asarray(inp["na_rpb"], dtype=np.float32))
    sh["w_branch"] = f(inp["w_branch"])
    sh["w_out"] = f(inp["w_out"])
    sh["w_router"] = f(inp["w_router"])
    sh["b_router"] = f(inp["b_router"])
    sh["w_gate_up"] = f(inp["w_gate_up"])
    sh["b_guT"] = f(np.asarray(inp["b_gate_up"]).reshape(4, 32, 16, 128).transpose(0, 3, 1, 2))
    sh["w_down"] = f(inp["w_down"])
    sh["b_down"] = f(inp["b_down"])
    c128, s128 = _dft(128, 1.0 / np.sqrt(128.0))
    sh["cs128"] = f(np.concatenate([c128, s128], axis=1))
    cn, sn = _dft(NL, 1.0 / np.sqrt(float(NL)))
    sh["cn"] = cn
    sh["sn"] = f(-sn)
    c256, s256 = _dft(256, 1.0 / 16.0)
    sh["c256"] = c256
    sh["s256"] = f(-s256)
    sh["ident"] = np.eye(128, dtype=np.float32)
    return sh


def prep_core(inp, b):
    x = np.asarray(inp["x"][b], dtype=np.float32)
    ctx = np.asarray(inp["ctx"][b], dtype=np.float32)
    xT = np.ascontiguousarray(np.concatenate([x, ctx], axis=0).T)
    c = np.asarray(inp["c"][b], dtype=np.float32).reshape(8, 128).T
    cctx = np.asarray(inp["c_ctx"], dtype=np.float32).reshape(8, 128).T
    cc = np.ascontiguousarray(np.stack([c, cctx], axis=-1))
    return {"xT": xT, "cc": cc}


_CACHE = {}


def kernel(**inputs):
    if "nc" not in _CACHE:
        _CACHE["nc"] = build(L=4)[0]
    nc = _CACHE["nc"]
    sh = prep_shared(inputs)
    in_maps = []
    for b in range(8):
        m = dict(sh)
        m.update(prep_core(inputs, b))
        in_maps.append(m)
    res = run_bass_kernel_spmd(nc, in_maps, core_ids=list(range(8)))
    out = np.stack([np.ascontiguousarray(res.results[b]["outT"].T) for b in range(8)], axis=0)
    return out.astype(np.float32)
```

```python
import math
import os
import numpy as np
BDBG = int(os.environ.get('BDBG', '0'))
import ml_dtypes
from contextlib import ExitStack
import concourse.bass as bass
import concourse.mybir as mybir
from concourse.bass_utils import run_bass_kernel_spmd

F32 = mybir.dt.float32
BF16 = mybir.dt.bfloat16
I32 = mybir.dt.int32
AF = mybir.ActivationFunctionType
ALU = mybir.AluOpType
AX = mybir.AxisListType

EPOCH = 12000
COMPUTE = ('pe', 'act', 'dve', 'pool')
SAME_ENGINE_SYNC = ('act', 'dve', 'pool')


class Buf:
    __slots__ = ('name', 'w', 'r', 'dsem')

    def __init__(self, name):
        self.name = name
        self.w = None
        self.r = {}
        self.dsem = None


class DSem:
    __slots__ = ('sem', 'cnt', 'idx')

    def __init__(self, sem, idx):
        self.sem = sem
        self.cnt = 0
        self.idx = idx


class V:
    __slots__ = ('ap', 'bufs', 'dram')

    def __init__(self, ap, bufs, dram=False):
        self.ap = ap
        self.bufs = bufs
        self.dram = dram


class TT:
    def __init__(self, P, handle, name, shape, slot_axis=None, is_dram=False, nslots=None):
        self.P = P
        self.h = handle
        self.name = name
        self.shape = list(shape)
        self.slot_axis = slot_axis
        self.is_dram = is_dram
        if nslots is not None:
            self.bufs = [Buf("%s.%d" % (name, i)) for i in range(nslots)]
        elif slot_axis is None:
            self.bufs = [Buf(name)]
        else:
            self.bufs = [Buf("%s.%d" % (name, i)) for i in range(shape[slot_axis])]

    def base(self):
        return self.h.ap() if self.is_dram else self.h

    def __getitem__(self, idx):
        if not isinstance(idx, tuple):
            idx = (idx,)
        ap = self.base()[idx]
        if self.slot_axis is None:
            return V(ap, self.bufs, self.is_dram)
        if self.slot_axis < len(idx):
            s = idx[self.slot_axis]
            if isinstance(s, int):
                return V(ap, [self.bufs[s]], self.is_dram)
            if isinstance(s, slice):
                return V(ap, self.bufs[s], self.is_dram)
        return V(ap, self.bufs, self.is_dram)

    def view(self, ap, slots=None):
        if slots is None:
            return V(ap, self.bufs, self.is_dram)
        return V(ap, [self.bufs[s] for s in slots], self.is_dram)


class Prog:
    def __init__(self, nc, es, n_dsem=56):
        self.nc = nc
        self.es = es
        self.eng = {'pe': nc.tensor, 'act': nc.scalar, 'dve': nc.vector, 'pool': nc.gpsimd, 'sp': nc.sync}
        self.cnt = {e: 0 for e in COMPUTE}
        self.esems = {e: [] for e in COMPUTE}
        self.seen_e = {e: {c: 0 for c in COMPUTE} for e in self.eng}
        self.seen_d = {e: {} for e in self.eng}
        self.dpool = []
        for i in range(n_dsem):
            self.dpool.append(DSem(es.enter_context(nc.semaphore("dsem%d" % i)), i))
        self.dfree = list(self.dpool)
        self.dused = []
        self.n_instr = 0
        self.n_wait = 0
        self.psum = []
        self.psum_i = 0

    def sbuf(self, es, name, shape, dtype, slot_axis=None):
        self.uid = getattr(self, 'uid', 0) + 1
        name = "s%d_%s" % (self.uid, name)
        h = es.enter_context(self.nc.sbuf_tensor(name, list(shape), dtype))
        return TT(self, h, name, shape, slot_axis)

    def dram(self, name, shape, dtype, kind="Internal", slot_axis=None, nslots=None):
        h = self.nc.dram_tensor(name, list(shape), dtype, kind=kind)
        return TT(self, h, name, shape, slot_axis, is_dram=True, nslots=nslots)

    def init_psum(self, es, n=8):
        for i in range(n):
            h = es.enter_context(self.nc.psum_tensor("psb%d" % i, [128, 512], F32))
            self.psum.append(TT(self, h, "psb%d" % i, [128, 512]))

    def bank(self):
        t = self.psum[self.psum_i % len(self.psum)]
        self.psum_i += 1
        return t

    def _esem(self, e, epoch):
        while len(self.esems[e]) <= epoch:
            self.esems[e].append(self.es.enter_context(self.nc.semaphore("es_%s_%d" % (e, len(self.esems[e])))))
        return self.esems[e][epoch]

    def _wait(self, e, t):
        if t is None:
            return
        if t[0] == 'e':
            _, c, n = t
            if c == e and e not in SAME_ENGINE_SYNC:
                return
            if self.seen_e[e][c] >= n:
                return
            self.seen_e[e][c] = n
            epoch = (n - 1) // EPOCH
            self.eng[e].wait_ge(self._esem(c, epoch), n - epoch * EPOCH)
            self.n_wait += 1
        else:
            _, ds, n = t
            if self.seen_d[e].get(ds.idx, 0) >= n:
                return
            self.seen_d[e][ds.idx] = n
            self.eng[e].wait_ge(ds.sem, n)
            self.n_wait += 1

    @staticmethod
    def _tkey(t):
        return (t[0], t[1] if t[0] == 'e' else t[1].idx)

    def _deps(self, e, rbufs, wbufs):
        for b in rbufs:
            self._wait(e, b.w)
        for b in wbufs:
            self._wait(e, b.w)
            for t in list(b.r.values()):
                self._wait(e, t)

    def _record(self, t, rbufs, wbufs):
        k = self._tkey(t)
        for b in rbufs:
            b.r[k] = t
        for b in wbufs:
            b.w = t
            b.r = {}

    def op(self, e, fn, reads=(), writes=()):
        rbufs = [b for v in reads for b in v.bufs]
        wbufs = [b for v in writes for b in v.bufs]
        self._deps(e, rbufs, wbufs)
        ins = fn(self.eng[e])
        self.cnt[e] += 1
        n = self.cnt[e]
        ep = (n - 1) // EPOCH
        ins.then_inc(self._esem(e, ep), 1)
        self.seen_e[e][e] = max(self.seen_e[e][e], 0)
        self._record(('e', e, n), rbufs, wbufs)
        self.n_instr += 1
        return ins

    def _dsem_for(self, buf):
        if buf.dsem is None:
            if not self.dfree:
                raise RuntimeError("out of DMA semaphores")
            buf.dsem = self.dfree.pop()
            self.dused.append(buf)
        return buf.dsem

    def release_dsems(self, tts):
        for tt in tts:
            for b in tt.bufs:
                if b.dsem is not None:
                    self.dfree.append(b.dsem)
                    b.dsem = None
                    if b in self.dused:
                        self.dused.remove(b)

    def dma(self, q, out, in_, semv=None, first=True, **kw):
        if semv is None:
            semv = in_ if out.dram else out
        sb = semv.bufs[0]
        ds = self._dsem_for(sb)
        rbufs = list(in_.bufs)
        wbufs = list(out.bufs)
        if first:
            self._deps(q, rbufs, wbufs)
            if ds.cnt:
                self._wait(q, ('d', ds, ds.cnt))
        ins = self.eng[q].dma_start(out=out.ap, in_=in_.ap, **kw)
        ds.cnt += 16
        ins.then_inc(ds.sem, 16)
        self._record(('d', ds, ds.cnt), rbufs, wbufs)
        self.n_instr += 1
        return ins

    def barrier(self, engines=None):
        engines = engines or list(self.eng.keys())
        for e in engines:
            for c in COMPUTE:
                if self.cnt[c]:
                    self._wait(e, ('e', c, self.cnt[c]))
            for ds in self.dpool:
                if ds.cnt:
                    self._wait(e, ('d', ds, ds.cnt))

    def mm(self, out, lhsT, rhs, start=True, stop=True, sgc=False):
        if sgc:
            return self.op('pe', lambda e: e.matmul(out.ap, lhsT.ap, rhs.ap, start=start, stop=stop, skip_group_check=True),
                           reads=[lhsT, rhs], writes=[out])
        return self.op('pe', lambda e: e.matmul(out.ap, lhsT.ap, rhs.ap, start=start, stop=stop),
                       reads=[lhsT, rhs], writes=[out])

    def act(self, out, in_, func, bias=None, scale=None, eng='act'):
        kw = {}
        reads = [in_]
        if bias is not None:
            if isinstance(bias, V):
                kw['bias'] = bias.ap
                reads.append(bias)
            else:
                kw['bias'] = bias
        if scale is not None:
            if isinstance(scale, V):
                kw['scale'] = scale.ap
                reads.append(scale)
            else:
                kw['scale'] = scale
        return self.op(eng, lambda e: e.activation(out=out.ap, in_=in_.ap, func=func, **kw),
                       reads=reads, writes=[out])

    def tt(self, out, in0, in1, op, eng='dve'):
        return self.op(eng, lambda e: e.tensor_tensor(out=out.ap, in0=in0.ap, in1=in1.ap, op=op),
                       reads=[in0, in1], writes=[out])

    def ts(self, out, in0, s1, s2, op0, op1=None, eng='dve'):
        reads = [in0]
        a1 = s1
        a2 = s2
        if isinstance(s1, V):
            a1 = s1.ap
            reads.append(s1)
        if isinstance(s2, V):
            a2 = s2.ap
            reads.append(s2)
        if op1 is None:
            return self.op(eng, lambda e: e.tensor_scalar(out=out.ap, in0=in0.ap, scalar1=a1, scalar2=None, op0=op0),
                           reads=reads, writes=[out])
        return self.op(eng, lambda e: e.tensor_scalar(out=out.ap, in0=in0.ap, scalar1=a1, scalar2=a2, op0=op0, op1=op1),
                       reads=reads, writes=[out])

    def stt(self, out, in0, s, in1, op0, op1, eng='dve'):
        reads = [in0, in1]
        a = s
        if isinstance(s, V):
            a = s.ap
            reads.append(s)
        return self.op(eng, lambda e: e.scalar_tensor_tensor(out=out.ap, in0=in0.ap, scalar=a, in1=in1.ap, op0=op0, op1=op1),
                       reads=reads, writes=[out])

    def copy(self, out, in_, eng='dve'):
        if eng == 'act':
            return self.act(out, in_, AF.Identity)
        return self.op(eng, lambda e: e.tensor_copy(out=out.ap, in_=in_.ap), reads=[in_], writes=[out])

    def memset(self, out, val, eng='dve'):
        return self.op(eng, lambda e: e.memset(out.ap, val), reads=[], writes=[out])


D = 1024
NT = 2304
NL = 2048
CH = [(0, 512, 0), (512, 512, 0), (1024, 512, 0), (1536, 512, 0), (2048, 256, 1)]
EPS = 1e-6
NEG = -30000.0


class Ctx:
    pass


def build(L=4, stop=None, dbg=False, NE=32):
    nc = bass.Bass("TRN2", target_bir_lowering=False)
    es = ExitStack()
    P = Prog(nc, es)
    P.init_psum(es)
    C = Ctx()
    C.P = P
    C.nc = nc
    C.L = L
    C.NE = NE

    def din(name, shape, dt=F32):
        return P.dram(name, shape, dt, kind="ExternalInput")

    C.xT = din("xT", [D, NT])
    C.cc = din("cc", [128, 8, 2])
    C.w_mod = din("w_mod", [4, D, 6144])
    C.b_modT = din("b_modT", [4, 128, 48])
    C.gmix = din("gmix", [128, 4, 8])
    C.gffn = din("gffn", [128, 4, 8])
    C.gfin = din("gfin", [128, 8])
    C.w_in = din("w_in", [4, D, 6656])
    C.w_rot = din("w_rot", [4, D, 1024])
    C.ropeC = din("ropeC", [128, NL])
    C.ropeS = din("ropeS", [128, NL])
    C.da_lam = din("da_lam", [4, 256])
    C.subln = din("subln", [128, 4])
    C.naT = din("naT", [4, 16, 128, 512])
    C.w_branch = din("w_branch", [4, 3, 512, D])
    C.w_out = din("w_out", [4, D, D])
    C.w_router = din("w_router", [4, D, 32])
    C.b_router = din("b_router", [4, 32])
    C.w_gate_up = din("w_gate_up", [4, NE, D, 2048])
    C.b_guT = din("b_guT", [4, 128, 32, 16])
    C.w_down = din("w_down", [4, NE, D, D])
    C.b_down = din("b_down", [4, 32, D])
    C.cs128 = din("cs128", [128, 256])
    C.cn = din("cn", [NL, NL])
    C.sn = din("sn", [NL, NL])
    C.c256 = din("c256", [256, 256])
    C.s256 = din("s256", [256, 256])
    C.identd = din("ident", [128, 128])
    C.outT = P.dram("outT", [D, NL], F32, kind="ExternalOutput")
    skind = "ExternalOutput" if dbg else "Internal"
    C.XD = P.dram("XD", [D, NT], F32, kind=skind, nslots=5)
    C.OD = [P.dram("OD%d" % i, [512, NT], BF16, kind=skind, nslots=5) for i in range(3)]
    if dbg:
        C.HD = P.dram("HD", [D, NT], BF16, kind="ExternalOutput", nslots=5)

    def xd(c):
        t0, n, s = CH[c]
        return C.XD.view(C.XD.h.ap()[:, t0:t0 + n].rearrange("(k p) t -> p k t", p=128), slots=[c])
    C.xd = xd

    def od(i, c):
        t0, n, s = CH[c]
        return C.OD[i].view(C.OD[i].h.ap()[:, t0:t0 + n].rearrange("(k p) t -> p k t", p=128), slots=[c])
    C.od = od

    g = es
    C.ones_f = P.sbuf(g, "ones_f", [128, 128], F32)
    C.ones_b = P.sbuf(g, "ones_b", [128, 128], BF16)
    C.zeros_b = P.sbuf(g, "zeros_b", [128, 128], BF16)
    C.ident = P.sbuf(g, "ident", [128, 128], F32)
    C.ident_b = P.sbuf(g, "ident_b", [128, 128], BF16)
    C.sc = P.sbuf(g, "silu_c", [128, 8, 2], F32)
    C.modT = P.sbuf(g, "modT", [128, 2, 48], F32)
    C.A1 = P.sbuf(g, "A1", [128, 2, 8], F32)
    C.A2 = P.sbuf(g, "A2", [128, 2, 8], F32)
    C.gmix_s = P.sbuf(g, "gmix_s", [128, 4, 8], F32)
    C.gffn_s = P.sbuf(g, "gffn_s", [128, 4, 8], F32)
    C.gfin_s = P.sbuf(g, "gfin_s", [128, 8], F32)
    C.subln_s = P.sbuf(g, "subln_s", [128, 4], F32)
    C.lamneg = P.sbuf(g, "lamneg", [128, 1], F32)
    C.sgc = P.sbuf(g, "sgc", [128, 1], F32)

    P.memset(C.ones_f[:, :], 1.0)
    P.memset(C.ones_b[:, :], 1.0)
    P.memset(C.zeros_b[:, :], 0.0)
    P.dma('sp', C.ident[:, :], C.identd[:, :])
    P.copy(C.ident_b[:, :], C.ident[:, :])
    P.dma('sp', C.sc[:, :, :], C.cc[:, :, :])
    P.act(C.sc[:, :, :], C.sc[:, :, :], AF.Silu)
    P.dma('sp', C.gmix_s[:, :, :], C.gmix[:, :, :])
    P.dma('sp', C.gffn_s[:, :, :], C.gffn[:, :, :])
    P.dma('sp', C.gfin_s[:, :], C.gfin[:, :])
    P.dma('sp', C.subln_s[:, :], C.subln[:, :])
    for c in range(5):
        t0, n, s = CH[c]
        P.dma('sp', C.XD.view(C.XD.h.ap()[:, t0:t0 + n], slots=[c]), C.xT.view(C.xT.h.ap()[:, t0:t0 + n]),
              semv=C.XD.view(C.XD.h.ap()[:, t0:t0 + n], slots=[c]))

    def done(tag):
        return stop is not None and stop == tag

    fin = False
    for l in range(L):
        last = (l == 3)
        chunks = [0, 1, 2, 3] if last else [0, 1, 2, 3, 4]
        phase_mod(C, l)
        P.barrier()
        if done("mod%d" % l):
            fin = True
            break
        with ExitStack() as hs:
            C.HT = [P.sbuf(hs, "HT%d" % c, [128, 8, CH[c][1]], BF16) for c in range(5)]
            phase_h(C, l, 1, [0, 1, 2, 3, 4])
            P.barrier()
            if dbg:
                for c in range(5):
                    t0, n, s = CH[c]
                    P.dma('sp', C.HD.view(C.HD.h.ap()[:, t0:t0 + n].rearrange("(k p) t -> p k t", p=128), slots=[c]),
                          C.HT[c][:, :, :])
            for tag, fn in (("a", phase_a), ("b", phase_b), ("c", phase_c), ("out", phase_out)):
                if fin:
                    break
                if done("h%d" % l):
                    fin = True
                    break
                fn(C, l, chunks)
                P.barrier()
                if done("%s%d" % (tag, l)):
                    fin = True
            P.barrier()
            P.release_dsems(C.HT)
        if fin:
            break
        phase_moe(C, l, chunks)
        P.barrier()
        if done("moe%d" % l):
            fin = True
            break
    if stop is None:
        phase_final(C)
    P.barrier()
    C.es = es
    return nc, C


def phase_mod(C, l):
    P = C.P
    lam_init = 0.8 - 0.6 * math.exp(-0.3 * l)
    with ExitStack() as es:
        wm = [P.sbuf(es, "wm%d" % i, [128, 8, 512], F32) for i in range(2)]
        bm = P.sbuf(es, "bm", [128, 48], F32)
        lamt = P.sbuf(es, "lamt", [128, 256], F32)
        pr = P.sbuf(es, "lampr", [128, 128], F32)
        s12 = P.sbuf(es, "lams12", [128, 2], F32)
        e12 = P.sbuf(es, "lame12", [128, 2], F32)
        P.dma('sp', bm[:, :], C.b_modT.view(C.b_modT.h.ap()[l]))
        bank = P.psum[0]
        for blk in range(12):
            w = wm[blk % 2]
            P.dma('sp', w[:, :, :], C.w_mod.view(
                C.w_mod.h.ap()[l][:, blk * 512:(blk + 1) * 512].rearrange("(k p) c -> p k c", p=128)))
            for jj in range(4):
                j = blk * 4 + jj
                for k in range(8):
                    P.mm(bank[:, 2 * j:2 * j + 2], w[:, k, jj * 128:(jj + 1) * 128], C.sc[:, k, :],
                         start=(k == 0), stop=(k == 7))
        for s in range(2):
            P.tt(C.modT[:, s, :], bank[:, s:96:2], bm[:, :], ALU.add)
            P.stt(C.A1[:, s, :], C.modT[:, s, 8:16], 1.0, C.gmix_s[:, l, :], ALU.add, ALU.mult)
            P.stt(C.A2[:, s, :], C.modT[:, s, 32:40], 1.0, C.gffn_s[:, l, :], ALU.add, ALU.mult)
        P.dma('sp', lamt[:, :], C.da_lam.view(C.da_lam.h.ap()[l].partition_broadcast(128)))
        P.tt(pr[:, 0:64], lamt[:, 0:64], lamt[:, 64:128], ALU.mult)
        P.tt(pr[:, 64:128], lamt[:, 128:192], lamt[:, 192:256], ALU.mult)
        P.op('dve', lambda e: e.reduce_sum(out=s12[:, 0:1].ap, in_=pr[:, 0:64].ap, axis=AX.X),
             reads=[pr[:, :]], writes=[s12[:, :]])
        P.op('dve', lambda e: e.reduce_sum(out=s12[:, 1:2].ap, in_=pr[:, 64:128].ap, axis=AX.X),
             reads=[pr[:, :]], writes=[s12[:, :]])
        P.act(e12[:, :], s12[:, :], AF.Exp)
        P.tt(C.lamneg[:, :], e12[:, 1:2], e12[:, 0:1], ALU.subtract)
        P.ts(C.lamneg[:, :], C.lamneg[:, :], -lam_init, None, ALU.add)
        P.ts(C.sgc[:, :], C.subln_s[:, l:l + 1], (1.0 - lam_init), None, ALU.mult)
        P.barrier()
        P.release_dsems(wm + [bm, lamt])


def rstd_from_bank(P, r, bank, n, inv_n):
    P.ts(r[:, 0:n], bank[:, 0:n], inv_n, EPS, ALU.mult, ALU.add)
    P.act(r[:, 0:n], r[:, 0:n], AF.Sqrt)
    P.op('dve', lambda e: e.reciprocal(out=r[:, 0:n].ap, in_=r[:, 0:n].ap), reads=[r[:, 0:n]], writes=[r[:, 0:n]])


def phase_h(C, l, which, chunks):
    P = C.P
    with ExitStack() as es:
        xck = [P.sbuf(es, "hx%d" % i, [128, 8, 512], F32) for i in range(2)] if which == 1 else []
        sqt = [P.sbuf(es, "hsq%d" % i, [128, 512], F32) for i in range(2)]
        rs = [P.sbuf(es, "hrs%d" % i, [128, 512], F32) for i in range(2)]
        tmp = [P.sbuf(es, "htm%d" % i, [128, 512], F32) for i in range(2)]
        allt = xck + sqt + rs + tmp
        if which == 2:
            h2f = P.sbuf(es, "h2f", [128, 8, 512], F32)
            wr = P.sbuf(es, "wr", [128, 8, 32], F32)
            brt = P.sbuf(es, "brt", [128, 32], F32)
            lg = P.sbuf(es, "lg", [128, 32], F32)
            m8 = P.sbuf(es, "m8", [128, 8], F32)
            mask = P.sbuf(es, "rmask", [128, 32], F32)
            negm = P.sbuf(es, "negm", [128, 1], F32)
            ex = P.sbuf(es, "rex", [128, 32], F32)
            den = P.sbuf(es, "rden", [128, 1], F32)
            wt = P.sbuf(es, "rwt", [128, 32], F32)
            allt += [h2f, wr, brt]
            P.dma('sp', wr[:, :, :], C.w_router.view(C.w_router.h.ap()[l].rearrange("(k p) e -> p k e", p=128)))
            P.dma('sp', brt[:, :], C.b_router.view(C.b_router.h.ap()[l].partition_broadcast(128)))
        for ci, c in enumerate(chunks):
            t0, n, s = CH[c]
            if which == 1:
                xc = xck[ci % 2]
                P.dma('sp', xc[:, :, 0:n], C.xd(c))
                xsrc = xc
            else:
                xsrc = C.XT[c]
            bank = P.psum[ci % 2]
            for k in range(8):
                sq = sqt[k % 2]
                P.tt(sq[:, 0:n], xsrc[:, k, 0:n], xsrc[:, k, 0:n], ALU.mult, eng='pool')
                P.mm(bank[:, 0:n], C.ones_f[:, :], sq[:, 0:n], start=(k == 0), stop=(k == 7))
            r = rs[ci % 2]
            rstd_from_bank(P, r, bank, n, 1.0 / D)
            A = C.A1 if which == 1 else C.A2
            boff = 0 if which == 1 else 24
            for k in range(8):
                tm = tmp[k % 2]
                P.stt(tm[:, 0:n], xsrc[:, k, 0:n], A[:, s, k:k + 1], r[:, 0:n], ALU.mult, ALU.mult)
                bv = C.modT[:, s, boff + k:boff + k + 1]
                if which == 1:
                    P.act(C.HT[c][:, k, 0:n], tm[:, 0:n], AF.Identity, bias=bv)
                else:
                    P.act(h2f[:, k, 0:n], tm[:, 0:n], AF.Identity, bias=bv)
                    P.copy(C.H2T[c][:, k, 0:n], h2f[:, k, 0:n], eng='pool')
            if which == 2:
                for tl in range(n // 128):
                    tile = t0 // 128 + tl
                    bankR = P.psum[2 + (tile % 2)]
                    for k in range(8):
                        P.mm(bankR[:, 0:32], h2f[:, k, tl * 128:(tl + 1) * 128], wr[:, k, :],
                             start=(k == 0), stop=(k == 7))
                    P.tt(lg[:, :], bankR[:, 0:32], brt[:, :], ALU.add)
                    P.op('dve', lambda e: e.max(out=m8[:, :].ap, in_=lg[:, :].ap), reads=[lg[:, :]], writes=[m8[:, :]])
                    P.ts(mask[:, :], lg[:, :], m8[:, 3:4], None, ALU.is_ge)
                    P.ts(negm[:, :], m8[:, 0:1], -1.0, None, ALU.mult)
                    P.act(ex[:, :], lg[:, :], AF.Exp, bias=negm[:, 0:1])
                    P.tt(ex[:, :], ex[:, :], mask[:, :], ALU.mult)
                    P.op('dve', lambda e: e.reduce_sum(out=den[:, :].ap, in_=ex[:, :].ap, axis=AX.X),
                         reads=[ex[:, :]], writes=[den[:, :]])
                    P.op('dve', lambda e: e.reciprocal(out=den[:, :].ap, in_=den[:, :].ap),
                         reads=[den[:, :]], writes=[den[:, :]])
                    P.ts(wt[:, :], ex[:, :], den[:, 0:1], None, ALU.mult)
                    bankT = P.psum[4 + (tile % 2)]
                    P.mm(bankT[0:32, 0:128], wt[:, 0:32], C.ident[:, :])
                    P.copy(C.WT[0:32, tile * 128:(tile + 1) * 128], bankT[0:32, 0:128])
        P.barrier()
        P.release_dsems(allt)


def load_w(P, dst, src_tt, ap, q='pool'):
    return P.dma(q, dst, src_tt.view(ap))


def proj_fm(P, bank, W, col0, HTc, n):
    for k in range(8):
        P.mm(bank[:, 0:n], W[:, k, col0:col0 + 128], HTc[:, k, 0:n], start=(k == 0), stop=(k == 7))


def rope_evac(C, out, bank, bankP, t0, n, t1, t2):
    P = C.P
    P.tt(t1[:, 0:n], bank[:, 0:n], C.ropeC_s[:, t0:t0 + n], ALU.mult)
    P.tt(t2[:, 0:n], bankP[:, 0:n], C.ropeS_s[:, t0:t0 + n], ALU.mult)
    P.tt(out, t1[:, 0:n], t2[:, 0:n], ALU.add, eng='pool')


def phase_a(C, l, chunks):
    P = C.P
    win = C.w_in.h.ap()[l]
    wrot = C.w_rot.h.ap()[l]
    kp = "(k p) c -> p k c"
    with ExitStack() as es:
        KT = P.sbuf(es, "KT", [128, 4, NT], BF16, slot_axis=1)
        Vt = P.sbuf(es, "Vt", [128, 18, 512], BF16, slot_axis=1)
        C.ropeC_s = P.sbuf(es, "ropeC_s", [128, NL], F32)
        C.ropeS_s = P.sbuf(es, "ropeS_s", [128, NL], F32)
        t1 = P.sbuf(es, "ropet1", [128, 512], F32)
        t2 = P.sbuf(es, "ropet2", [128, 512], F32)
        P.dma('sp', C.ropeC_s[:, :], C.ropeC[:, :])
        P.dma('sp', C.ropeS_s[:, :], C.ropeS[:, :])
        rel = [KT, Vt, C.ropeC_s, C.ropeS_s]
        with ExitStack() as e1:
            Wk = P.sbuf(e1, "Wk", [128, 8, 512], BF16)
            WkP = P.sbuf(e1, "WkP", [128, 8, 512], BF16)
            Wv = P.sbuf(e1, "Wv", [128, 8, 512], BF16)
            load_w(P, Wk[:, :, :], C.w_in, win[:, 512:1024].rearrange(kp, p=128))
            load_w(P, WkP[:, :, :], C.w_rot, wrot[:, 512:1024].rearrange(kp, p=128))
            load_w(P, Wv[:, :, :], C.w_in, win[:, 1024:1536].rearrange(kp, p=128))
            it = 0
            for c in range(5):
                t0, n, s = CH[c]
                for h in range(4):
                    bk = P.psum[(2 * it) % 8]
                    bp = P.psum[(2 * it + 1) % 8]
                    it += 1
                    proj_fm(P, bk, Wk, h * 128, C.HT[c], n)
                    if s == 0:
                        proj_fm(P, bp, WkP, h * 128, C.HT[c], n)
                        rope_evac(C, KT[:, h, t0:t0 + n], bk, bp, t0, n, t1, t2)
                    else:
                        P.copy(KT[:, h, t0:t0 + n], bk[:, 0:n], eng='act')
                for tl in range(n // 128):
                    tile = t0 // 128 + tl
                    bv = P.psum[(2 * it) % 8]
                    it += 1
                    for k in range(8):
                        P.mm(bv[:, 0:512], C.HT[c][:, k, tl * 128:(tl + 1) * 128], Wv[:, k, :],
                             start=(k == 0), stop=(k == 7))
                    P.copy(Vt[:, tile, :], bv[:, 0:512], eng='act')
            P.barrier()
            P.release_dsems([Wk, WkP, Wv])
        with ExitStack() as e2:
            Wq = P.sbuf(e2, "Wq", [128, 8, 512], BF16)
            WqP = P.sbuf(e2, "WqP", [128, 8, 512], BF16)
            load_w(P, Wq[:, :, :], C.w_in, win[:, 0:512].rearrange(kp, p=128))
            load_w(P, WqP[:, :, :], C.w_rot, wrot[:, 0:512].rearrange(kp, p=128))
            QT = [P.sbuf(e2, "QT%d" % i, [128, 4, 512], BF16) for i in range(2)]
            pT = [P.sbuf(e2, "pT%d" % i, [128, 512], BF16) for i in range(3)]
            rsum = P.sbuf(e2, "a_rsum", [128, 512], F32)
            om = [P.sbuf(e2, "a_om%d" % i, [128, 512], F32) for i in range(2)]
            odt = P.sbuf(e2, "a_od", [128, 512], F32)
            sq = P.sbuf(e2, "a_sq", [128, 512], F32)
            rst = P.sbuf(e2, "a_rst", [128, 512], F32)
            OA = [P.sbuf(e2, "OA%d" % i, [128, 4, 512], BF16) for i in range(2)]
            hm = 0
            for ci, c in enumerate(chunks):
                t0, n, s = CH[c]
                q = QT[ci % 2]
                oa = OA[ci % 2]
                for h in range(4):
                    bk = P.psum[7]
                    bp = P.psum[6]
                    proj_fm(P, bk, Wq, h * 128, C.HT[c], n)
                    if s == 0:
                        proj_fm(P, bp, WqP, h * 128, C.HT[c], n)
                        rope_evac(C, q[:, h, 0:n], bk, bp, t0, n, t1, t2)
                    else:
                        P.copy(q[:, h, 0:n], bk[:, 0:n], eng='act')
                tiles = list(range(18)) if s == 0 else [16, 17]
                for h in range(4):
                    for m in range(2):
                        bO = P.psum[3 + (hm % 2)]
                        bS = P.psum[5]
                        hm += 1
                        pb = slice(m * 64, (m + 1) * 64)

                        def st(i):
                            kt = tiles[i]
                            P.mm(P.psum[i % 3][:, 0:n], KT[pb, h, kt * 128:(kt + 1) * 128], q[pb, h, 0:n])
                        st(0)
                        for i, kt in enumerate(tiles):
                            if i + 1 < len(tiles):
                                st(i + 1)
                            p = pT[i % 3]
                            P.act(p[:, 0:n], P.psum[i % 3][:, 0:n], AF.Exp, scale=0.125)
                            P.mm(bO[:, 0:n], Vt[:, kt, h * 128:(h + 1) * 128], p[:, 0:n],
                                 start=(i == 0), stop=(i == len(tiles) - 1))
                            P.mm(bS[:, 0:n], C.ones_b[:, :], p[:, 0:n],
                                 start=(i == 0), stop=(i == len(tiles) - 1))
                        P.op('dve', lambda e: e.reciprocal(out=rsum[:, 0:n].ap, in_=bS[:, 0:n].ap),
                             reads=[bS[:, 0:n]], writes=[rsum[:, 0:n]])
                        P.tt(om[m][:, 0:n], bO[:, 0:n], rsum[:, 0:n], ALU.mult)
                    P.stt(odt[:, 0:n], om[1][:, 0:n], C.lamneg[:, 0:1], om[0][:, 0:n], ALU.mult, ALU.add)
                    P.tt(sq[:, 0:n], odt[:, 0:n], odt[:, 0:n], ALU.mult, eng='pool')
                    bN = P.psum[6]
                    P.mm(bN[:, 0:n], C.ones_f[:, :], sq[:, 0:n])
                    rstd_from_bank(P, rst, bN, n, 1.0 / 128)
                    P.stt(oa[:, h, 0:n], odt[:, 0:n], C.sgc[:, 0:1], rst[:, 0:n], ALU.mult, ALU.mult)
                P.dma('sp', C.od(0, c), oa[:, :, 0:n])
            P.barrier()
            P.release_dsems([Wq, WqP] + QT + OA)
        P.release_dsems(rel)


def na_tiles(r):
    rs_ = min(max(r - 4, 0), 24)
    out = []
    for a in range(rs_ // 2, (rs_ + 7) // 2 + 1):
        r0, r1 = 2 * a, 2 * a + 1
        v0 = rs_ <= r0 < rs_ + 8
        v1 = rs_ <= r1 < rs_ + 8
        if v0 and v1:
            idx = r0 - r + 7
            assert 0 <= idx <= 13
        elif v1:
            assert r1 - r + 7 == 3
            idx = 14
        else:
            assert v0 and r0 - r + 7 == 10
            idx = 15
        out.append((a, idx))
    return out


def phase_b(C, l, chunks):
    P = C.P
    win = C.w_in.h.ap()[l]
    kp = "(k p) c -> p k c"
    with ExitStack() as es:
        KT = P.sbuf(es, "KN", [128, 4, NT], BF16, slot_axis=1)
        Vt = P.sbuf(es, "VN", [128, 18, 512], BF16, slot_axis=1)
        NAT = P.sbuf(es, "NAT", [128, 16, 512], BF16)
        for i0 in range(0, 16, 4):
            load_w(P, NAT[:, i0:i0 + 4, :], C.naT, C.naT.h.ap()[l][i0:i0 + 4].rearrange("i p c -> p i c"))
        rel = [KT, Vt, NAT]
        with ExitStack() as e1:
            Wk = P.sbuf(e1, "Wkn", [128, 8, 512], BF16)
            Wv = P.sbuf(e1, "Wvn", [128, 8, 512], BF16)
            load_w(P, Wk[:, :, :], C.w_in, win[:, 2048:2560].rearrange(kp, p=128))
            load_w(P, Wv[:, :, :], C.w_in, win[:, 2560:3072].rearrange(kp, p=128))
            it = 0
            for c in range(5):
                t0, n, s = CH[c]
                for h in range(4):
                    bk = P.psum[it % 8]
                    it += 1
                    proj_fm(P, bk, Wk, h * 128, C.HT[c], n)
                    P.copy(KT[:, h, t0:t0 + n], bk[:, 0:n], eng=('act' if h % 2 else 'dve'))
                for tl in range(n // 128):
                    tile = t0 // 128 + tl
                    bv = P.psum[it % 8]
                    it += 1
                    for k in range(8):
                        P.mm(bv[:, 0:512], C.HT[c][:, k, tl * 128:(tl + 1) * 128], Wv[:, k, :],
                             start=(k == 0), stop=(k == 7))
                    P.copy(Vt[:, tile, :], bv[:, 0:512], eng=('act' if tl % 2 else 'dve'))
            P.barrier()
            P.release_dsems([Wk, Wv])
        with ExitStack() as e2:
            Wq = P.sbuf(e2, "Wqn", [128, 8, 512], BF16)
            load_w(P, Wq[:, :, :], C.w_in, win[:, 1536:2048].rearrange(kp, p=128))
            QT = [P.sbuf(e2, "QN%d" % i, [128, 4, 512], BF16) for i in range(2)]
            pT = [P.sbuf(e2, "pN%d" % i, [128, 512], BF16) for i in range(3)]
            rsum = P.sbuf(e2, "b_rsum", [128, 512], F32)
            OB = [P.sbuf(e2, "OB%d" % i, [128, 4, 512], BF16) for i in range(2)]
            rowi = 0
            for ci, c in enumerate(chunks):
                t0, n, s = CH[c]
                q = QT[ci % 2]
                ob = OB[ci % 2]
                for h in range(4):
                    bk = P.psum[7]
                    proj_fm(P, bk, Wq, h * 128, C.HT[c], n)
                    P.act(q[:, h, 0:n], bk[:, 0:n], AF.Identity, scale=0.125)
                for rr in range(n // 64):
                    rq = rr * 64
                    if s == 0:
                        r = t0 // 64 + rr
                        tiles = na_tiles(r) + [(16, None), (17, None)]
                    else:
                        tiles = [(16, None), (17, None)]
                    bO = P.psum[4 + (rowi % 2)]
                    bS = P.psum[6]
                    rowi += 1

                    def st(i):
                        a, idx = tiles[i]
                        for par in range(2):
                            bk_ = P.psum[(i % 2) * 2 + par]
                            pb = slice(par * 64, par * 64 + 64)
                            if idx is not None:
                                P.mm(bk_[:, 0:256], C.ident_b[:, :], NAT[:, idx, par * 256:(par + 1) * 256],
                                     start=True, stop=False, sgc=True)
                            for cc in range(4):
                                P.mm(bk_[:, cc * 64:(cc + 1) * 64], KT[pb, cc, a * 128:(a + 1) * 128],
                                     q[pb, cc, rq:rq + 64], start=(idx is None), stop=True, sgc=True)
                    st(0)
                    nt_ = len(tiles)
                    for i, (a, idx) in enumerate(tiles):
                        if i + 1 < nt_:
                            st(i + 1)
                        p = pT[i % 3]
                        for par in range(2):
                            P.act(p[:, par * 256:(par + 1) * 256], P.psum[(i % 2) * 2 + par][:, 0:256], AF.Exp)
                        if i == 0:
                            P.mm(bO[:, 0:512], C.zeros_b[:, :], p[:, :], start=True, stop=False, sgc=True)
                        for par in range(2):
                            for cc in range(4):
                                co = par * 256 + cc * 64
                                P.mm(bO[:, co:co + 64], Vt[:, a, cc * 128:(cc + 1) * 128],
                                     p[:, co:co + 64], start=False, stop=(i == nt_ - 1), sgc=True)
                        P.mm(bS[:, 0:512], C.ones_b[:, :], p[:, :], start=(i == 0), stop=(i == nt_ - 1))
                    P.op('dve', lambda e: e.reciprocal(out=rsum[:, :].ap, in_=bS[:, 0:512].ap),
                         reads=[bS[:, 0:512]], writes=[rsum[:, :]])
                    for par in range(2):
                        pb = slice(par * 64, par * 64 + 64)
                        o_v = ob.view(ob.h[pb, :, rq:rq + 64])
                        b_v = bO.view(bO.h[pb, par * 256:(par + 1) * 256].rearrange("p (c q) -> p c q", c=4))
                        r_v = rsum.view(rsum.h[pb, par * 256:(par + 1) * 256].rearrange("p (c q) -> p c q", c=4))
                        P.tt(o_v, b_v, r_v, ALU.mult)
                P.dma('sp', C.od(1, c), ob[:, :, 0:n])
            P.barrier()
            P.release_dsems([Wq] + QT + OB)
        P.release_dsems(rel)


def phase_c(C, l, chunks):
    P = C.P
    win = C.w_in.h.ap()[l]
    kp = "(k p) c -> p k c"
    with ExitStack() as es:
        AB = P.sbuf(es, "AB", [128, 18, 4, 256], BF16, slot_axis=1)
        CS = P.sbuf(es, "CS128", [128, 256], BF16)
        Wf = P.sbuf(es, "Wf", [128, 8, 512], BF16)
        fT = [P.sbuf(es, "fT%d" % i, [128, 4, 512], BF16) for i in range(2)]
        CNk = P.sbuf(es, "CNk", [128, 16, 512], BF16)
        SNk = P.sbuf(es, "SNk", [128, 16, 512], BF16)
        OC = [P.sbuf(es, "OC%d" % i, [128, 4, 512], BF16) for i in range(2)]
        load_w(P, CS[:, :], C.cs128, C.cs128.h.ap())
        load_w(P, Wf[:, :, :], C.w_in, win[:, 3072:3584].rearrange(kp, p=128))
        it = 0
        for c in range(5):
            t0, n, s = CH[c]
            if s == 1 and 4 not in chunks:
                continue
            f = fT[c % 2]
            for g_ in range(4):
                bk = P.psum[it % 4]
                it += 1
                proj_fm(P, bk, Wf, g_ * 128, C.HT[c], n)
                P.copy(f[:, g_, 0:n], bk[:, 0:n], eng=('act' if g_ % 2 else 'dve'))
            for tl in range(n // 128):
                tile = t0 // 128 + tl
                for half in range(2):
                    bk = P.psum[4 + (it % 4)]
                    it += 1
                    for gg in range(2):
                        g_ = half * 2 + gg
                        P.mm(bk[:, gg * 256:(gg + 1) * 256], f[:, g_, tl * 128:(tl + 1) * 128], CS[:, :])
                    P.copy(AB.view(AB.h[:, tile, half * 2:half * 2 + 2, :], slots=[tile]),
                           bk.view(bk.h[:, 0:512].rearrange("p (g m) -> p g m", g=2)),
                           eng=('act' if half else 'dve'))
        for kc in range(4):
            for t4 in range(0, 16, 4):
                load_w(P, CNk[:, t4:t4 + 4, :], C.cn,
                       C.cn.h.ap()[t4 * 128:(t4 + 4) * 128, kc * 512:(kc + 1) * 512].rearrange("(t p) c -> p t c", p=128))
                load_w(P, SNk[:, t4:t4 + 4, :], C.sn,
                       C.sn.h.ap()[t4 * 128:(t4 + 4) * 128, kc * 512:(kc + 1) * 512].rearrange("(t p) c -> p t c", p=128))
            oc = OC[kc % 2]
            for g_ in range(4):
                bk = P.psum[g_ % 4]
                for nt_ in range(16):
                    P.mm(bk[:, 0:512], AB[:, nt_, g_, 0:128], CNk[:, nt_, :], start=(nt_ == 0), stop=False)
                    P.mm(bk[:, 0:512], AB[:, nt_, g_, 128:256], SNk[:, nt_, :], start=False, stop=(nt_ == 15))
                P.copy(oc[:, g_, :], bk[:, 0:512], eng=('act' if g_ % 2 else 'dve'))
            P.dma('sp', C.od(2, kc), oc[:, :, :])
        if 4 in chunks:
            P.barrier()
            load_w(P, CNk[:, 0:2, 0:256], C.c256, C.c256.h.ap().rearrange("(t p) c -> p t c", p=128))
            load_w(P, SNk[:, 0:2, 0:256], C.s256, C.s256.h.ap().rearrange("(t p) c -> p t c", p=128))
            oc = OC[0]
            for g_ in range(4):
                bk = P.psum[g_ % 4]
                for nt_ in range(2):
                    P.mm(bk[:, 0:256], AB[:, 16 + nt_, g_, 0:128], CNk[:, nt_, 0:256], start=(nt_ == 0), stop=False)
                    P.mm(bk[:, 0:256], AB[:, 16 + nt_, g_, 128:256], SNk[:, nt_, 0:256], start=False, stop=(nt_ == 1))
                P.copy(oc[:, g_, 0:256], bk[:, 0:256], eng=('act' if g_ % 2 else 'dve'))
            P.dma('sp', C.od(2, 4), oc[:, :, 0:256])
        P.barrier()
        P.release_dsems([AB, CS, Wf, CNk, SNk] + fT + OC)


def phase_out(C, l, chunks):
    P = C.P
    win = C.w_in.h.ap()[l]
    with ExitStack() as es:
        MG = [P.sbuf(es, "MG%d" % c, [128, 8, CH[c][1]], BF16) for c in range(5)]
        Oi = [P.sbuf(es, "Oi%d" % c, [128, 4, CH[c][1]], BF16) for c in range(5)]
        Wb = [P.sbuf(es, "Wb%d" % i, [128, 4, 128], BF16) for i in range(2)]
        Wg = [P.sbuf(es, "Wg%d" % i, [128, 8, 128], BF16) for i in range(2)]
        sg = [P.sbuf(es, "sg%d" % i, [128, 512], F32) for i in range(2)]
        tm = [P.sbuf(es, "otm%d" % i, [128, 512], F32) for i in range(2)]
        it = 0
        for i in range(3):
            for c in chunks:
                t0, n, s = CH[c]
                P.dma('sp', Oi[c][:, :, :], C.od(i, c))
            for dc in range(8):
                wb = Wb[it % 2]
                wg = Wg[it % 2]
                load_w(P, wb[:, :, :], C.w_branch,
                       C.w_branch.h.ap()[l, i][:, dc * 128:(dc + 1) * 128].rearrange("(k p) c -> p k c", p=128))
                gc0 = 3584 + i * 1024 + dc * 128
                load_w(P, wg[:, :, :], C.w_in, win[:, gc0:gc0 + 128].rearrange("(k p) c -> p k c", p=128))
                for c in chunks:
                    t0, n, s = CH[c]
                    bP = P.psum[(2 * it) % 8]
                    bG = P.psum[(2 * it + 1) % 8]
                    it += 1
                    for k in range(4):
                        P.mm(bP[:, 0:n], wb[:, k, :], Oi[c][:, k, 0:n], start=(k == 0), stop=(k == 3))
                    for k in range(8):
                        P.mm(bG[:, 0:n], wg[:, k, :], C.HT[c][:, k, 0:n], start=(k == 0), stop=(k == 7))
                    s_ = sg[it % 2]
                    P.act(s_[:, 0:n], bG[:, 0:n], AF.Sigmoid)
                    if i == 0:
                        P.tt(MG[c][:, dc, 0:n], bP[:, 0:n], s_[:, 0:n], ALU.mult)
                    else:
                        t_ = tm[it % 2]
                        P.tt(t_[:, 0:n], bP[:, 0:n], s_[:, 0:n], ALU.mult)
                        P.tt(MG[c][:, dc, 0:n], MG[c][:, dc, 0:n], t_[:, 0:n], ALU.add, eng='pool')
        P.barrier()
        P.release_dsems(Oi + Wb + Wg)
        with ExitStack() as e2:
            Wo = P.sbuf(e2, "Wo", [128, 8, D], BF16)
            xck = [P.sbuf(e2, "ox%d" % i, [128, 8, 512], F32) for i in range(2)]
            load_w(P, Wo[:, :, :], C.w_out, C.w_out.h.ap()[l].rearrange("(k p) c -> p k c", p=128))
            it = 0
            for ci, c in enumerate(chunks):
                t0, n, s = CH[c]
                xc = xck[ci % 2]
                P.dma('sp', xc[:, :, 0:n], C.xd(c))
                for dc in range(8):
                    bk = P.psum[it % 8]
                    it += 1
                    for k in range(8):
                        P.mm(bk[:, 0:n], Wo[:, k, dc * 128:(dc + 1) * 128], MG[c][:, k, 0:n],
                             start=(k == 0), stop=(k == 7))
                    P.stt(xc[:, dc, 0:n], bk[:, 0:n], C.modT[:, s, 16 + dc:17 + dc], xc[:, dc, 0:n], ALU.mult, ALU.add)
                P.dma('sp', C.xd(c), xc[:, :, 0:n])
            P.barrier()
            P.release_dsems([Wo] + xck)


def phase_moe(C, l, chunks):
    P = C.P
    with ExitStack() as es:
        C.XT = [P.sbuf(es, "XT%d" % c, [128, 8, CH[c][1]], F32, slot_axis=1) for c in range(5)]
        C.H2T = [P.sbuf(es, "H2T%d" % c, [128, 8, CH[c][1]], BF16) for c in range(5)]
        C.WT = P.sbuf(es, "WT", [32, NT], F32)
        for c in chunks:
            P.dma('sp', C.XT[c][:, :, :], C.xd(c))
        phase_h(C, l, 2, chunks)
        wmk = [P.sbuf(es, "wmk%d" % i, [32, 512], F32) for i in range(2)]
        bdn = P.sbuf(es, "bdn", [32, D], F32)
        bgu = P.sbuf(es, "bgu", [128, 32, 16], F32)
        P.dma('sp', bdn[:, :], C.b_down.view(C.b_down.h.ap()[l]))
        P.dma('sp', bgu[:, :, :], C.b_guT.view(C.b_guT.h.ap()[l]))
        bgu1 = P.sbuf(es, "bgu1", [128, 32, 8], F32)
        P.ts(bgu1[:, :, :], bgu[:, :, 8:16], 1.0, None, ALU.add)
        actT = [[P.sbuf(es, "actT%d_%d" % (i, c), [128, 2, CH[c][1]], BF16, slot_axis=1) for c in range(5)]
                for i in range(2)]
        wgu = [P.sbuf(es, "wgu%d" % i, [128, 8, 2, 256], BF16) for i in range(2)]
        wd = [P.sbuf(es, "wd%d" % i, [128, 2, D], BF16) for i in range(3)]
        g1 = [P.sbuf(es, "g1_%d" % i, [128, 512], BF16) for i in range(2)]
        sgm = [P.sbuf(es, "sgm%d" % i, [128, 512], BF16) for i in range(2)]
        u0 = [P.sbuf(es, "u0_%d" % i, [128, 512], BF16) for i in range(2)]
        tq = [P.sbuf(es, "tq%d" % i, [128, 512], BF16) for i in range(4)]
        it = 0
        for c in chunks:
            t0, n, s = CH[c]
            for dc in range(8):
                bk = P.psum[it % 2]
                it += 1
                P.mm(bk[:, 0:n], bdn[0:32, dc * 128:(dc + 1) * 128], C.WT[0:32, t0:t0 + n])
                P.stt(C.XT[c][:, dc, 0:n], bk[:, 0:n], C.modT[:, s, 40 + dc:41 + dc], C.XT[c][:, dc, 0:n],
                      ALU.mult, ALU.add)
        wgu_ap = C.w_gate_up.h.ap()
        wd_ap = C.w_down.h.ap()
        quarters = [(e, qq) for e in range(C.NE) for qq in range(4)]

        def issue_weights(Qi):
            e, qq = quarters[Qi]
            wg = wgu[Qi % 2]
            wdn = wd[Qi % 3]
            c0 = qq * 256
            P.dma('pool', wg[:, :, 0, :], C.w_gate_up.view(
                wgu_ap[l, e][:, c0:c0 + 256].rearrange("(k p) c -> p k c", p=128)))
            P.dma('pool', wg[:, :, 1, :], C.w_gate_up.view(
                wgu_ap[l, e][:, 1024 + c0:1024 + c0 + 256].rearrange("(k p) c -> p k c", p=128)), first=False)
            P.dma('pool', wdn[:, :, :], C.w_down.view(
                wd_ap[l, e][c0:c0 + 256, :].rearrange("(j p) c -> p j c", p=128)))

        st_ = {"u": 0, "d": 0}
        pend = []
        e2done = {}

        def emit_e2():
            Qi, c, j, t_, bW = pend.pop(0)
            t0, n, s = CH[c]
            P.tt(actT[Qi % 2][c][:, j, 0:n], t_[:, 0:n], bW[:, 0:n], ALU.mult)
            e2done[(Qi, c)] = e2done.get((Qi, c), 0) + 1

        def emit_d(Qi, c, dc):
            t0, n, s = CH[c]
            wdn = wd[Qi % 3]
            bD = P.psum[4 + (st_["d"] % 2)]
            st_["d"] += 1
            for j in range(2):
                P.mm(bD[:, 0:n], wdn[:, j, dc * 128:(dc + 1) * 128], actT[Qi % 2][c][:, j, 0:n],
                     start=(j == 0), stop=(j == 1))
            P.stt(C.XT[c][:, dc, 0:n], bD[:, 0:n], C.modT[:, s, 40 + dc:41 + dc], C.XT[c][:, dc, 0:n],
                  ALU.mult, ALU.add)

        issue_weights(0)
        dq = []
        nun = len(chunks) * 2
        per_unit = (len(chunks) * 8 + nun - 1) // nun
        cidx = 0
        for Qi, (e, qq) in enumerate(quarters):
            if Qi + 1 < len(quarters):
                issue_weights(Qi + 1)
            wg = wgu[Qi % 2]
            for c in chunks:
                t0, n, s = CH[c]
                bW = P.psum[6 + (cidx % 2)]
                wm_ = wmk[cidx % 2]
                cidx += 1
                P.ts(wm_[0:32, 0:n], C.WT[0:32, t0:t0 + n], C.ident[0:32, e:e + 1], None, ALU.mult)
                P.mm(bW[:, 0:n], C.ones_f[0:32, :], wm_[0:32, 0:n])
                for j in range(2):
                    u = st_["u"]
                    st_["u"] += 1
                    ffc = qq * 2 + j
                    bG = P.psum[(2 * u) % 4]
                    bU = P.psum[(2 * u + 1) % 4]
                    for k in range(8):
                        P.mm(bG[:, 0:n], wg[:, k, 0, j * 128:(j + 1) * 128], C.H2T[c][:, k, 0:n],
                             start=(k == 0), stop=(k == 7))
                    for k in range(8):
                        P.mm(bU[:, 0:n], wg[:, k, 1, j * 128:(j + 1) * 128], C.H2T[c][:, k, 0:n],
                             start=(k == 0), stop=(k == 7))
                    g_ = g1[u % 2]
                    s_ = sgm[u % 2]
                    u_ = u0[u % 2]
                    t_ = tq[u % 4]
                    P.ts(g_[:, 0:n], bG[:, 0:n], bgu[:, e, ffc:ffc + 1], 7.0, ALU.add, ALU.min)
                    P.act(s_[:, 0:n], g_[:, 0:n], AF.Sigmoid, scale=1.702)
                    P.act(u_[:, 0:n], bU[:, 0:n], AF.Identity, bias=bgu1[:, e, ffc:ffc + 1])
                    P.ts(u_[:, 0:n], u_[:, 0:n], 8.0, -6.0, ALU.min, ALU.max, eng='pool')
                    P.tt(t_[:, 0:n], g_[:, 0:n], s_[:, 0:n], ALU.mult, eng='pool')
                    P.tt(t_[:, 0:n], t_[:, 0:n], u_[:, 0:n], ALU.mult, eng='pool')
                    pend.append((Qi, c, j, t_, bW))
                    if len(pend) > 2:
                        emit_e2()
                    k_ = 0
                    while dq and k_ < per_unit and e2done.get((dq[0][0], dq[0][1]), 0) == 2:
                        emit_d(*dq.pop(0))
                        k_ += 1
            while dq and dq[0][0] < Qi:
                while e2done.get((dq[0][0], dq[0][1]), 0) < 2:
                    emit_e2()
                emit_d(*dq.pop(0))
            dq.extend((Qi, c, dc) for c in chunks for dc in range(8))
        while pend:
            emit_e2()
        while dq:
            emit_d(*dq.pop(0))
        for c in chunks:
            P.dma('sp', C.xd(c), C.XT[c][:, :, :])
        P.barrier()
        P.release_dsems(C.XT + C.H2T + [C.WT, bdn, bgu] + actT[0] + actT[1] + wgu + wd)


def phase_final(C):
    P = C.P
    with ExitStack() as es:
        xck = [P.sbuf(es, "fx%d" % i, [128, 8, 512], F32) for i in range(2)]
        sqt = [P.sbuf(es, "fsq%d" % i, [128, 512], F32) for i in range(2)]
        rs = [P.sbuf(es, "frs%d" % i, [128, 512], F32) for i in range(2)]
        for c in range(4):
            t0, n, s = CH[c]
            xc = xck[c % 2]
            P.dma('sp', xc[:, :, :], C.xd(c))
            bank = P.psum[c % 2]
            for k in range(8):
                sq = sqt[k % 2]
                P.tt(sq[:, :], xc[:, k, :], xc[:, k, :], ALU.mult, eng='pool')
                P.mm(bank[:, 0:n], C.ones_f[:, :], sq[:, :], start=(k == 0), stop=(k == 7))
            r = rs[c % 2]
            rstd_from_bank(P, r, bank, n, 1.0 / D)
            for k in range(8):
                P.stt(xc[:, k, :], xc[:, k, :], C.gfin_s[:, k:k + 1], r[:, :], ALU.mult, ALU.mult)
            P.dma('sp', C.outT.view(C.outT.h.ap()[:, t0:t0 + n].rearrange("(k p) t -> p k t", p=128)), xc[:, :, :])
        P.barrier()
        P.release_dsems(xck)


def _rope_tables():
    t = np.arange(NL)
    row = (t // 64).astype(np.float32)
    col = (t % 64).astype(np.float32)
    freqs = (np.float32(10000.0) ** (-np.arange(16, dtype=np.float32) / np.float32(16))).astype(np.float32)
    Ct = np.zeros((128, NL), np.float32)
    St = np.zeros((128, NL), np.float32)
    for p in range(128):
        d = p % 64
        axis = d // 32
        half = (d % 32) // 16
        i = d % 16
        ang = (row if axis == 0 else col) * freqs[i]
        Ct[p] = np.cos(ang)
        St[p] = (-1.0 if half == 0 else 1.0) * np.sin(ang)
    return Ct, St


def _rot_cols():
    idx = np.arange(512)
    d = idx % 64
    return (idx - d) + (d ^ 16)


def _dft(n, scale):
    k = np.arange(n, dtype=np.int64)
    ph = (np.outer(k, k) % n).astype(np.float64) * (2.0 * np.pi / n)
    return (np.cos(ph) * scale).astype(np.float32), (np.sin(ph) * scale).astype(np.float32)


def _na_tables(rpb):
    q = np.arange(64)
    k = np.arange(64)
    cs = np.clip(q - 8, 0, 48)
    mask = (k[None, :] >= cs[:, None]) & (k[None, :] < cs[:, None] + 16)
    dc = np.clip(k[None, :] - q[:, None], -15, 15) + 15
    Bt = rpb[:, :, :, dc]
    Bt = np.where(mask[None, None, None], Bt, np.float32(NEG)).astype(np.float32)
    Bt = Bt.transpose(0, 2, 4, 1, 3)
    Bt = np.ascontiguousarray(Bt[:, :, :, [0, 2, 4, 6, 1, 3, 5, 7], :]).reshape(4, 15, 64, 512)
    M = np.full((4, 64, 512), NEG, np.float32)
    T = np.empty((4, 16, 128, 512), np.float32)
    for i in range(14):
        T[:, i, 0:64] = Bt[:, i]
        T[:, i, 64:128] = Bt[:, i + 1]
    T[:, 14, 0:64] = M
    T[:, 14, 64:128] = Bt[:, 3]
    T[:, 15, 0:64] = Bt[:, 10]
    T[:, 15, 64:128] = M
    return T


def prep_shared(inp):
    f = lambda a: np.ascontiguousarray(np.asarray(a, dtype=np.float32))
    sh = {}
    sh["w_mod"] = f(inp["w_mod"])
    sh["b_modT"] = f(np.asarray(inp["b_mod"]).reshape(4, 48, 128).transpose(0, 2, 1))
    sh["gmix"] = f(np.asarray(inp["norm_mix_g"]).reshape(4, 8, 128).transpose(2, 0, 1))
    sh["gffn"] = f(np.asarray(inp["norm_ffn_g"]).reshape(4, 8, 128).transpose(2, 0, 1))
    sh["gfin"] = f(np.asarray(inp["final_g"]).reshape(8, 128).T)
    w_in = f(inp["w_in"])
    sh["w_in"] = w_in
    rc = _rot_cols()
    sh["w_rot"] = f(np.concatenate([w_in[:, :, 0:512][:, :, rc], w_in[:, :, 512:1024][:, :, rc]], axis=2))
    Ct, St = _rope_tables()
    sh["ropeC"] = Ct
    sh["ropeS"] = St
    sh["da_lam"] = f(np.asarray(inp["da_lambda"]).reshape(4, 256))
    sh["subln"] = f(np.asarray(inp["da_subln_g"]).T)
    sh["naT"] = _na_tables(np.asarray(inp["na_rpb"], dtype=np.float32))
    sh["w_branch"] = f(inp["w_branch"])
    sh["w_out"] = f(inp["w_out"])
    sh["w_router"] = f(inp["w_router"])
    sh["b_router"] = f(inp["b_router"])
    sh["w_gate_up"] = f(inp["w_gate_up"])
    sh["b_guT"] = f(np.asarray(inp["b_gate_up"]).reshape(4, 32, 16, 128).transpose(0, 3, 1, 2))
    sh["w_down"] = f(inp["w_down"])
    sh["b_down"] = f(inp["b_down"])
    c128, s128 = _dft(128, 1.0 / np.sqrt(128.0))
    sh["cs128"] = f(np.concatenate([c128, s128], axis=1))
    cn, sn = _dft(NL, 1.0 / np.sqrt(float(NL)))
    sh["cn"] = cn
    sh["sn"] = f(-sn)
    c256, s256 = _dft(256, 1.0 / 16.0)
    sh["c256"] = c256
    sh["s256"] = f(-s256)
    sh["ident"] = np.eye(128, dtype=np.float32)
    return sh


def prep_core(inp, b):
    x = np.asarray(inp["x"][b], dtype=np.float32)
    ctx = np.asarray(inp["ctx"][b], dtype=np.float32)
    xT = np.ascontiguousarray(np.concatenate([x, ctx], axis=0).T)
    c = np.asarray(inp["c"][b], dtype=np.float32).reshape(8, 128).T
    cctx = np.asarray(inp["c_ctx"], dtype=np.float32).reshape(8, 128).T
    cc = np.ascontiguousarray(np.stack([c, cctx], axis=-1))
    return {"xT": xT, "cc": cc}


_CACHE = {}


def kernel(**inputs):
    if "nc" not in _CACHE:
        _CACHE["nc"] = build(L=4)[0]
    nc = _CACHE["nc"]
    sh = prep_shared(inputs)
    in_maps = []
    for b in range(8):
        m = dict(sh)
        m.update(prep_core(inputs, b))
        in_maps.append(m)
    res = run_bass_kernel_spmd(nc, in_maps, core_ids=list(range(8)))
    out = np.stack([np.ascontiguousarray(res.results[b]["outT"].T) for b in range(8)], axis=0)
    return out.astype(np.float32)
```

```python
import math
import os
import numpy as np
BDBG = int(os.environ.get('BDBG', '0'))
import ml_dtypes
from contextlib import ExitStack
import concourse.bass as bass
import concourse.mybir as mybir
from concourse.bass_utils import run_bass_kernel_spmd

F32 = mybir.dt.float32
BF16 = mybir.dt.bfloat16
I32 = mybir.dt.int32
AF = mybir.ActivationFunctionType
ALU = mybir.AluOpType
AX = mybir.AxisListType

EPOCH = 12000
COMPUTE = ('pe', 'act', 'dve', 'pool')
SAME_ENGINE_SYNC = ('act', 'dve', 'pool')


class Buf:
    __slots__ = ('name', 'w', 'r', 'dsem')

    def __init__(self, name):
        self.name = name
        self.w = None
        self.r = {}
        self.dsem = None


class DSem:
    __slots__ = ('sem', 'cnt', 'idx')

    def __init__(self, sem, idx):
        self.sem = sem
        self.cnt = 0
        self.idx = idx


class V:
    __slots__ = ('ap', 'bufs', 'dram')

    def __init__(self, ap, bufs, dram=False):
        self.ap = ap
        self.bufs = bufs
        self.dram = dram


class TT:
    def __init__(self, P, handle, name, shape, slot_axis=None, is_dram=False, nslots=None):
        self.P = P
        self.h = handle
        self.name = name
        self.shape = list(shape)
        self.slot_axis = slot_axis
        self.is_dram = is_dram
        if nslots is not None:
            self.bufs = [Buf("%s.%d" % (name, i)) for i in range(nslots)]
        elif slot_axis is None:
            self.bufs = [Buf(name)]
        else:
            self.bufs = [Buf("%s.%d" % (name, i)) for i in range(shape[slot_axis])]

    def base(self):
        return self.h.ap() if self.is_dram else self.h

    def __getitem__(self, idx):
        if not isinstance(idx, tuple):
            idx = (idx,)
        ap = self.base()[idx]
        if self.slot_axis is None:
            return V(ap, self.bufs, self.is_dram)
        if self.slot_axis < len(idx):
            s = idx[self.slot_axis]
            if isinstance(s, int):
                return V(ap, [self.bufs[s]], self.is_dram)
            if isinstance(s, slice):
                return V(ap, self.bufs[s], self.is_dram)
        return V(ap, self.bufs, self.is_dram)

    def view(self, ap, slots=None):
        if slots is None:
            return V(ap, self.bufs, self.is_dram)
        return V(ap, [self.bufs[s] for s in slots], self.is_dram)


class Prog:
    def __init__(self, nc, es, n_dsem=56):
        self.nc = nc
        self.es = es
        self.eng = {'pe': nc.tensor, 'act': nc.scalar, 'dve': nc.vector, 'pool': nc.gpsimd, 'sp': nc.sync}
        self.cnt = {e: 0 for e in COMPUTE}
        self.esems = {e: [] for e in COMPUTE}
        self.seen_e = {e: {c: 0 for c in COMPUTE} for e in self.eng}
        self.seen_d = {e: {} for e in self.eng}
        self.dpool = []
        for i in range(n_dsem):
            self.dpool.append(DSem(es.enter_context(nc.semaphore("dsem%d" % i)), i))
        self.dfree = list(self.dpool)
        self.dused = []
        self.n_instr = 0
        self.n_wait = 0
        self.psum = []
        self.psum_i = 0

    def sbuf(self, es, name, shape, dtype, slot_axis=None):
        self.uid = getattr(self, 'uid', 0) + 1
        name = "s%d_%s" % (self.uid, name)
        h = es.enter_context(self.nc.sbuf_tensor(name, list(shape), dtype))
        return TT(self, h, name, shape, slot_axis)

    def dram(self, name, shape, dtype, kind="Internal", slot_axis=None, nslots=None):
        h = self.nc.dram_tensor(name, list(shape), dtype, kind=kind)
        return TT(self, h, name, shape, slot_axis, is_dram=True, nslots=nslots)

    def init_psum(self, es, n=8):
        for i in range(n):
            h = es.enter_context(self.nc.psum_tensor("psb%d" % i, [128, 512], F32))
            self.psum.append(TT(self, h, "psb%d" % i, [128, 512]))

    def bank(self):
        t = self.psum[self.psum_i % len(self.psum)]
        self.psum_i += 1
        return t

    def _esem(self, e, epoch):
        while len(self.esems[e]) <= epoch:
            self.esems[e].append(self.es.enter_context(self.nc.semaphore("es_%s_%d" % (e, len(self.esems[e])))))
        return self.esems[e][epoch]

    def _wait(self, e, t):
        if t is None:
            return
        if t[0] == 'e':
            _, c, n = t
            if c == e and e not in SAME_ENGINE_SYNC:
                return
            if self.seen_e[e][c] >= n:
                return
            self.seen_e[e][c] = n
            epoch = (n - 1) // EPOCH
            self.eng[e].wait_ge(self._esem(c, epoch), n - epoch * EPOCH)
            self.n_wait += 1
        else:
            _, ds, n = t
            if self.seen_d[e].get(ds.idx, 0) >= n:
                return
            self.seen_d[e][ds.idx] = n
            self.eng[e].wait_ge(ds.sem, n)
            self.n_wait += 1

    @staticmethod
    def _tkey(t):
        return (t[0], t[1] if t[0] == 'e' else t[1].idx)

    def _deps(self, e, rbufs, wbufs):
        for b in rbufs:
            self._wait(e, b.w)
        for b in wbufs:
            self._wait(e, b.w)
            for t in list(b.r.values()):
                self._wait(e, t)

    def _record(self, t, rbufs, wbufs):
        k = self._tkey(t)
        for b in rbufs:
            b.r[k] = t
        for b in wbufs:
            b.w = t
            b.r = {}

    def op(self, e, fn, reads=(), writes=()):
        rbufs = [b for v in reads for b in v.bufs]
        wbufs = [b for v in writes for b in v.bufs]
        self._deps(e, rbufs, wbufs)
        ins = fn(self.eng[e])
        self.cnt[e] += 1
        n = self.cnt[e]
        ep = (n - 1) // EPOCH
        ins.then_inc(self._esem(e, ep), 1)
        self.seen_e[e][e] = max(self.seen_e[e][e], 0)
        self._record(('e', e, n), rbufs, wbufs)
        self.n_instr += 1
        return ins

    def _dsem_for(self, buf):
        if buf.dsem is None:
            if not self.dfree:
                raise RuntimeError("out of DMA semaphores")
            buf.dsem = self.dfree.pop()
            self.dused.append(buf)
        return buf.dsem

    def release_dsems(self, tts):
        for tt in tts:
            for b in tt.bufs:
                if b.dsem is not None:
                    self.dfree.append(b.dsem)
                    b.dsem = None
                    if b in self.dused:
                        self.dused.remove(b)

    def dma(self, q, out, in_, semv=None, first=True, **kw):
        if semv is None:
            semv = in_ if out.dram else out
        sb = semv.bufs[0]
        ds = self._dsem_for(sb)
        rbufs = list(in_.bufs)
        wbufs = list(out.bufs)
        if first:
            self._deps(q, rbufs, wbufs)
            if ds.cnt:
                self._wait(q, ('d', ds, ds.cnt))
        ins = self.eng[q].dma_start(out=out.ap, in_=in_.ap, **kw)
        ds.cnt += 16
        ins.then_inc(ds.sem, 16)
        self._record(('d', ds, ds.cnt), rbufs, wbufs)
        self.n_instr += 1
        return ins

    def barrier(self, engines=None):
        engines = engines or list(self.eng.keys())
        for e in engines:
            for c in COMPUTE:
                if self.cnt[c]:
                    self._wait(e, ('e', c, self.cnt[c]))
            for ds in self.dpool:
                if ds.cnt:
                    self._wait(e, ('d', ds, ds.cnt))

    def mm(self, out, lhsT, rhs, start=True, stop=True, sgc=False):
        if sgc:
            return self.op('pe', lambda e: e.matmul(out.ap, lhsT.ap, rhs.ap, start=start, stop=stop, skip_group_check=True),
                           reads=[lhsT, rhs], writes=[out])
        return self.op('pe', lambda e: e.matmul(out.ap, lhsT.ap, rhs.ap, start=start, stop=stop),
                       reads=[lhsT, rhs], writes=[out])

    def act(self, out, in_, func, bias=None, scale=None, eng='act'):
        kw = {}
        reads = [in_]
        if bias is not None:
            if isinstance(bias, V):
                kw['bias'] = bias.ap
                reads.append(bias)
            else:
                kw['bias'] = bias
        if scale is not None:
            if isinstance(scale, V):
                kw['scale'] = scale.ap
                reads.append(scale)
            else:
                kw['scale'] = scale
        return self.op(eng, lambda e: e.activation(out=out.ap, in_=in_.ap, func=func, **kw),
                       reads=reads, writes=[out])

    def tt(self, out, in0, in1, op, eng='dve'):
        return self.op(eng, lambda e: e.tensor_tensor(out=out.ap, in0=in0.ap, in1=in1.ap, op=op),
                       reads=[in0, in1], writes=[out])

    def ts(self, out, in0, s1, s2, op0, op1=None, eng='dve'):
        reads = [in0]
        a1 = s1
        a2 = s2
        if isinstance(s1, V):
            a1 = s1.ap
            reads.append(s1)
        if isinstance(s2, V):
            a2 = s2.ap
            reads.append(s2)
        if op1 is None:
            return self.op(eng, lambda e: e.tensor_scalar(out=out.ap, in0=in0.ap, scalar1=a1, scalar2=None, op0=op0),
                           reads=reads, writes=[out])
        return self.op(eng, lambda e: e.tensor_scalar(out=out.ap, in0=in0.ap, scalar1=a1, scalar2=a2, op0=op0, op1=op1),
                       reads=reads, writes=[out])

    def stt(self, out, in0, s, in1, op0, op1, eng='dve'):
        reads = [in0, in1]
        a = s
        if isinstance(s, V):
            a = s.ap
            reads.append(s)
        return self.op(eng, lambda e: e.scalar_tensor_tensor(out=out.ap, in0=in0.ap, scalar=a, in1=in1.ap, op0=op0, op1=op1),
                       reads=reads, writes=[out])

    def copy(self, out, in_, eng='dve'):
        if eng == 'act':
            return self.act(out, in_, AF.Identity)
        return self.op(eng, lambda e: e.tensor_copy(out=out.ap, in_=in_.ap), reads=[in_], writes=[out])

    def memset(self, out, val, eng='dve'):
        return self.op(eng, lambda e: e.memset(out.ap, val), reads=[], writes=[out])


D = 1024
NT = 2304
NL = 2048
CH = [(0, 512, 0), (512, 512, 0), (1024, 512, 0), (1536, 512, 0), (2048, 256, 1)]
EPS = 1e-6
NEG = -30000.0


class Ctx:
    pass


def build(L=4, stop=None, dbg=False, NE=32):
    nc = bass.Bass("TRN2", target_bir_lowering=False)
    es = ExitStack()
    P = Prog(nc, es)
    P.init_psum(es)
    C = Ctx()
    C.P = P
    C.nc = nc
    C.L = L
    C.NE = NE

    def din(name, shape, dt=F32):
        return P.dram(name, shape, dt, kind="ExternalInput")

    C.xT = din("xT", [D, NT])
    C.cc = din("cc", [128, 8, 2])
    C.w_mod = din("w_mod", [4, D, 6144])
    C.b_modT = din("b_modT", [4, 128, 48])
    C.gmix = din("gmix", [128, 4, 8])
    C.gffn = din("gffn", [128, 4, 8])
    C.gfin = din("gfin", [128, 8])
    C.w_in = din("w_in", [4, D, 6656])
    C.w_rot = din("w_rot", [4, D, 1024])
    C.ropeC = din("ropeC", [128, NL])
    C.ropeS = din("ropeS", [128, NL])
    C.da_lam = din("da_lam", [4, 256])
    C.subln = din("subln", [128, 4])
    C.naT = din("naT", [4, 16, 128, 512])
    C.w_branch = din("w_branch", [4, 3, 512, D])
    C.w_out = din("w_out", [4, D, D])
    C.w_router = din("w_router", [4, D, 32])
    C.b_router = din("b_router", [4, 32])
    C.w_gate_up = din("w_gate_up", [4, NE, D, 2048])
    C.b_guT = din("b_guT", [4, 128, 32, 16])
    C.w_down = din("w_down", [4, NE, D, D])
    C.b_down = din("b_down", [4, 32, D])
    C.cs128 = din("cs128", [128, 256])
    C.cn = din("cn", [NL, NL])
    C.sn = din("sn", [NL, NL])
    C.c256 = din("c256", [256, 256])
    C.s256 = din("s256", [256, 256])
    C.identd = din("ident", [128, 128])
    C.outT = P.dram("outT", [D, NL], F32, kind="ExternalOutput")
    skind = "ExternalOutput" if dbg else "Internal"
    C.XD = P.dram("XD", [D, NT], F32, kind=skind, nslots=5)
    C.OD = [P.dram("OD%d" % i, [512, NT], BF16, kind=skind, nslots=5) for i in range(3)]
    if dbg:
        C.HD = P.dram("HD", [D, NT], BF16, kind="ExternalOutput", nslots=5)

    def xd(c):
        t0, n, s = CH[c]
        return C.XD.view(C.XD.h.ap()[:, t0:t0 + n].rearrange("(k p) t -> p k t", p=128), slots=[c])
    C.xd = xd

    def od(i, c):
        t0, n, s = CH[c]
        return C.OD[i].view(C.OD[i].h.ap()[:, t0:t0 + n].rearrange("(k p) t -> p k t", p=128), slots=[c])
    C.od = od

    g = es
    C.ones_f = P.sbuf(g, "ones_f", [128, 128], F32)
    C.ones_b = P.sbuf(g, "ones_b", [128, 128], BF16)
    C.zeros_b = P.sbuf(g, "zeros_b", [128, 128], BF16)
    C.ident = P.sbuf(g, "ident", [128, 128], F32)
    C.ident_b = P.sbuf(g, "ident_b", [128, 128], BF16)
    C.sc = P.sbuf(g, "silu_c", [128, 8, 2], F32)
    C.modT = P.sbuf(g, "modT", [128, 2, 48], F32)
    C.A1 = P.sbuf(g, "A1", [128, 2, 8], F32)
    C.A2 = P.sbuf(g, "A2", [128, 2, 8], F32)
    C.gmix_s = P.sbuf(g, "gmix_s", [128, 4, 8], F32)
    C.gffn_s = P.sbuf(g, "gffn_s", [128, 4, 8], F32)
    C.gfin_s = P.sbuf(g, "gfin_s", [128, 8], F32)
    C.subln_s = P.sbuf(g, "subln_s", [128, 4], F32)
    C.lamneg = P.sbuf(g, "lamneg", [128, 1], F32)
    C.sgc = P.sbuf(g, "sgc", [128, 1], F32)

    P.memset(C.ones_f[:, :], 1.0)
    P.memset(C.ones_b[:, :], 1.0)
    P.memset(C.zeros_b[:, :], 0.0)
    P.dma('sp', C.ident[:, :], C.identd[:, :])
    P.copy(C.ident_b[:, :], C.ident[:, :])
    P.dma('sp', C.sc[:, :, :], C.cc[:, :, :])
    P.act(C.sc[:, :, :], C.sc[:, :, :], AF.Silu)
    P.dma('sp', C.gmix_s[:, :, :], C.gmix[:, :, :])
    P.dma('sp', C.gffn_s[:, :, :], C.gffn[:, :, :])
    P.dma('sp', C.gfin_s[:, :], C.gfin[:, :])
    P.dma('sp', C.subln_s[:, :], C.subln[:, :])
    for c in range(5):
        t0, n, s = CH[c]
        P.dma('sp', C.XD.view(C.XD.h.ap()[:, t0:t0 + n], slots=[c]), C.xT.view(C.xT.h.ap()[:, t0:t0 + n]),
              semv=C.XD.view(C.XD.h.ap()[:, t0:t0 + n], slots=[c]))

    def done(tag):
        return stop is not None and stop == tag

    fin = False
    for l in range(L):
        last = (l == 3)
        chunks = [0, 1, 2, 3] if last else [0, 1, 2, 3, 4]
        phase_mod(C, l)
        P.barrier()
        if done("mod%d" % l):
            fin = True
            break
        with ExitStack() as hs:
            C.HT = [P.sbuf(hs, "HT%d" % c, [128, 8, CH[c][1]], BF16) for c in range(5)]
            phase_h(C, l, 1, [0, 1, 2, 3, 4])
            P.barrier()
            if dbg:
                for c in range(5):
                    t0, n, s = CH[c]
                    P.dma('sp', C.HD.view(C.HD.h.ap()[:, t0:t0 + n].rearrange("(k p) t -> p k t", p=128), slots=[c]),
                          C.HT[c][:, :, :])
            for tag, fn in (("a", phase_a), ("b", phase_b), ("c", phase_c), ("out", phase_out)):
                if fin:
                    break
                if done("h%d" % l):
                    fin = True
                    break
                fn(C, l, chunks)
                P.barrier()
                if done("%s%d" % (tag, l)):
                    fin = True
            P.barrier()
            P.release_dsems(C.HT)
        if fin:
            break
        phase_moe(C, l, chunks)
        P.barrier()
        if done("moe%d" % l):
            fin = True
            break
    if stop is None:
        phase_final(C)
    P.barrier()
    C.es = es
    return nc, C


def phase_mod(C, l):
    P = C.P
    lam_init = 0.8 - 0.6 * math.exp(-0.3 * l)
    with ExitStack() as es:
        wm = [P.sbuf(es, "wm%d" % i, [128, 8, 512], F32) for i in range(2)]
        bm = P.sbuf(es, "bm", [128, 48], F32)
        lamt = P.sbuf(es, "lamt", [128, 256], F32)
        pr = P.sbuf(es, "lampr", [128, 128], F32)
        s12 = P.sbuf(es, "lams12", [128, 2], F32)
        e12 = P.sbuf(es, "lame12", [128, 2], F32)
        P.dma('sp', bm[:, :], C.b_modT.view(C.b_modT.h.ap()[l]))
        bank = P.psum[0]
        for blk in range(12):
            w = wm[blk % 2]
            P.dma('sp', w[:, :, :], C.w_mod.view(
                C.w_mod.h.ap()[l][:, blk * 512:(blk + 1) * 512].rearrange("(k p) c -> p k c", p=128)))
            for jj in range(4):
                j = blk * 4 + jj
                for k in range(8):
                    P.mm(bank[:, 2 * j:2 * j + 2], w[:, k, jj * 128:(jj + 1) * 128], C.sc[:, k, :],
                         start=(k == 0), stop=(k == 7))
        for s in range(2):
            P.tt(C.modT[:, s, :], bank[:, s:96:2], bm[:, :], ALU.add)
            P.stt(C.A1[:, s, :], C.modT[:, s, 8:16], 1.0, C.gmix_s[:, l, :], ALU.add, ALU.mult)
            P.stt(C.A2[:, s, :], C.modT[:, s, 32:40], 1.0, C.gffn_s[:, l, :], ALU.add, ALU.mult)
        P.dma('sp', lamt[:, :], C.da_lam.view(C.da_lam.h.ap()[l].partition_broadcast(128)))
        P.tt(pr[:, 0:64], lamt[:, 0:64], lamt[:, 64:128], ALU.mult)
        P.tt(pr[:, 64:128], lamt[:, 128:192], lamt[:, 192:256], ALU.mult)
        P.op('dve', lambda e: e.reduce_sum(out=s12[:, 0:1].ap, in_=pr[:, 0:64].ap, axis=AX.X),
             reads=[pr[:, :]], writes=[s12[:, :]])
        P.op('dve', lambda e: e.reduce_sum(out=s12[:, 1:2].ap, in_=pr[:, 64:128].ap, axis=AX.X),
             reads=[pr[:, :]], writes=[s12[:, :]])
        P.act(e12[:, :], s12[:, :], AF.Exp)
        P.tt(C.lamneg[:, :], e12[:, 1:2], e12[:, 0:1], ALU.subtract)
        P.ts(C.lamneg[:, :], C.lamneg[:, :], -lam_init, None, ALU.add)
        P.ts(C.sgc[:, :], C.subln_s[:, l:l + 1], (1.0 - lam_init), None, ALU.mult)
        P.barrier()
        P.release_dsems(wm + [bm, lamt])


def rstd_from_bank(P, r, bank, n, inv_n):
    P.ts(r[:, 0:n], bank[:, 0:n], inv_n, EPS, ALU.mult, ALU.add)
    P.act(r[:, 0:n], r[:, 0:n], AF.Sqrt)
    P.op('dve', lambda e: e.reciprocal(out=r[:, 0:n].ap, in_=r[:, 0:n].ap), reads=[r[:, 0:n]], writes=[r[:, 0:n]])


def phase_h(C, l, which, chunks):
    P = C.P
    with ExitStack() as es:
        xck = [P.sbuf(es, "hx%d" % i, [128, 8, 512], F32) for i in range(2)] if which == 1 else []
        sqt = [P.sbuf(es, "hsq%d" % i, [128, 512], F32) for i in range(2)]
        rs = [P.sbuf(es, "hrs%d" % i, [128, 512], F32) for i in range(2)]
        tmp = [P.sbuf(es, "htm%d" % i, [128, 512], F32) for i in range(2)]
        allt = xck + sqt + rs + tmp
        if which == 2:
            h2f = P.sbuf(es, "h2f", [128, 8, 512], F32)
            wr = P.sbuf(es, "wr", [128, 8, 32], F32)
            brt = P.sbuf(es, "brt", [128, 32], F32)
            lg = P.sbuf(es, "lg", [128, 32], F32)
            m8 = P.sbuf(es, "m8", [128, 8], F32)
            mask = P.sbuf(es, "rmask", [128, 32], F32)
            negm = P.sbuf(es, "negm", [128, 1], F32)
            ex = P.sbuf(es, "rex", [128, 32], F32)
            den = P.sbuf(es, "rden", [128, 1], F32)
            wt = P.sbuf(es, "rwt", [128, 32], F32)
            allt += [h2f, wr, brt]
            P.dma('sp', wr[:, :, :], C.w_router.view(C.w_router.h.ap()[l].rearrange("(k p) e -> p k e", p=128)))
            P.dma('sp', brt[:, :], C.b_router.view(C.b_router.h.ap()[l].partition_broadcast(128)))
        for ci, c in enumerate(chunks):
            t0, n, s = CH[c]
            if which == 1:
                xc = xck[ci % 2]
                P.dma('sp', xc[:, :, 0:n], C.xd(c))
                xsrc = xc
            else:
                xsrc = C.XT[c]
            bank = P.psum[ci % 2]
            for k in range(8):
                sq = sqt[k % 2]
                P.tt(sq[:, 0:n], xsrc[:, k, 0:n], xsrc[:, k, 0:n], ALU.mult, eng='pool')
                P.mm(bank[:, 0:n], C.ones_f[:, :], sq[:, 0:n], start=(k == 0), stop=(k == 7))
            r = rs[ci % 2]
            rstd_from_bank(P, r, bank, n, 1.0 / D)
            A = C.A1 if which == 1 else C.A2
            boff = 0 if which == 1 else 24
            for k in range(8):
                tm = tmp[k % 2]
                P.stt(tm[:, 0:n], xsrc[:, k, 0:n], A[:, s, k:k + 1], r[:, 0:n], ALU.mult, ALU.mult)
                bv = C.modT[:, s, boff + k:boff + k + 1]
                if which == 1:
                    P.act(C.HT[c][:, k, 0:n], tm[:, 0:n], AF.Identity, bias=bv)
                else:
                    P.act(h2f[:, k, 0:n], tm[:, 0:n], AF.Identity, bias=bv)
                    P.copy(C.H2T[c][:, k, 0:n], h2f[:, k, 0:n], eng='pool')
            if which == 2:
                for tl in range(n // 128):
                    tile = t0 // 128 + tl
                    bankR = P.psum[2 + (tile % 2)]
                    for k in range(8):
                        P.mm(bankR[:, 0:32], h2f[:, k, tl * 128:(tl + 1) * 128], wr[:, k, :],
                             start=(k == 0), stop=(k == 7))
                    P.tt(lg[:, :], bankR[:, 0:32], brt[:, :], ALU.add)
                    P.op('dve', lambda e: e.max(out=m8[:, :].ap, in_=lg[:, :].ap), reads=[lg[:, :]], writes=[m8[:, :]])
                    P.ts(mask[:, :], lg[:, :], m8[:, 3:4], None, ALU.is_ge)
                    P.ts(negm[:, :], m8[:, 0:1], -1.0, None, ALU.mult)
                    P.act(ex[:, :], lg[:, :], AF.Exp, bias=negm[:, 0:1])
                    P.tt(ex[:, :], ex[:, :], mask[:, :], ALU.mult)
                    P.op('dve', lambda e: e.reduce_sum(out=den[:, :].ap, in_=ex[:, :].ap, axis=AX.X),
                         reads=[ex[:, :]], writes=[den[:, :]])
                    P.op('dve', lambda e: e.reciprocal(out=den[:, :].ap, in_=den[:, :].ap),
                         reads=[den[:, :]], writes=[den[:, :]])
                    P.ts(wt[:, :], ex[:, :], den[:, 0:1], None, ALU.mult)
                    bankT = P.psum[4 + (tile % 2)]
                    P.mm(bankT[0:32, 0:128], wt[:, 0:32], C.ident[:, :])
                    P.copy(C.WT[0:32, tile * 128:(tile + 1) * 128], bankT[0:32, 0:128])
        P.barrier()
        P.release_dsems(allt)


def load_w(P, dst, src_tt, ap, q='pool'):
    return P.dma(q, dst, src_tt.view(ap))


def proj_fm(P, bank, W, col0, HTc, n):
    for k in range(8):
        P.mm(bank[:, 0:n], W[:, k, col0:col0 + 128], HTc[:, k, 0:n], start=(k == 0), stop=(k == 7))


def rope_evac(C, out, bank, bankP, t0, n, t1, t2):
    P = C.P
    P.tt(t1[:, 0:n], bank[:, 0:n], C.ropeC_s[:, t0:t0 + n], ALU.mult)
    P.tt(t2[:, 0:n], bankP[:, 0:n], C.ropeS_s[:, t0:t0 + n], ALU.mult)
    P.tt(out, t1[:, 0:n], t2[:, 0:n], ALU.add, eng='pool')


def phase_a(C, l, chunks):
    P = C.P
    win = C.w_in.h.ap()[l]
    wrot = C.w_rot.h.ap()[l]
    kp = "(k p) c -> p k c"
    with ExitStack() as es:
        KT = P.sbuf(es, "KT", [128, 4, NT], BF16, slot_axis=1)
        Vt = P.sbuf(es, "Vt", [128, 18, 512], BF16, slot_axis=1)
        C.ropeC_s = P.sbuf(es, "ropeC_s", [128, NL], F32)
        C.ropeS_s = P.sbuf(es, "ropeS_s", [128, NL], F32)
        t1 = P.sbuf(es, "ropet1", [128, 512], F32)
        t2 = P.sbuf(es, "ropet2", [128, 512], F32)
        P.dma('sp', C.ropeC_s[:, :], C.ropeC[:, :])
        P.dma('sp', C.ropeS_s[:, :], C.ropeS[:, :])
        rel = [KT, Vt, C.ropeC_s, C.ropeS_s]
        with ExitStack() as e1:
            Wk = P.sbuf(e1, "Wk", [128, 8, 512], BF16)
            WkP = P.sbuf(e1, "WkP", [128, 8, 512], BF16)
            Wv = P.sbuf(e1, "Wv", [128, 8, 512], BF16)
            load_w(P, Wk[:, :, :], C.w_in, win[:, 512:1024].rearrange(kp, p=128))
            load_w(P, WkP[:, :, :], C.w_rot, wrot[:, 512:1024].rearrange(kp, p=128))
            load_w(P, Wv[:, :, :], C.w_in, win[:, 1024:1536].rearrange(kp, p=128))
            it = 0
            for c in range(5):
                t0, n, s = CH[c]
                for h in range(4):
                    bk = P.psum[(2 * it) % 8]
                    bp = P.psum[(2 * it + 1) % 8]
                    it += 1
                    proj_fm(P, bk, Wk, h * 128, C.HT[c], n)
                    if s == 0:
                        proj_fm(P, bp, WkP, h * 128, C.HT[c], n)
                        rope_evac(C, KT[:, h, t0:t0 + n], bk, bp, t0, n, t1, t2)
                    else:
                        P.copy(KT[:, h, t0:t0 + n], bk[:, 0:n], eng='act')
                for tl in range(n // 128):
                    tile = t0 // 128 + tl
                    bv = P.psum[(2 * it) % 8]
                    it += 1
                    for k in range(8):
                        P.mm(bv[:, 0:512], C.HT[c][:, k, tl * 128:(tl + 1) * 128], Wv[:, k, :],
                             start=(k == 0), stop=(k == 7))
                    P.copy(Vt[:, tile, :], bv[:, 0:512], eng='act')
            P.barrier()
            P.release_dsems([Wk, WkP, Wv])
        with ExitStack() as e2:
            Wq = P.sbuf(e2, "Wq", [128, 8, 512], BF16)
            WqP = P.sbuf(e2, "WqP", [128, 8, 512], BF16)
            load_w(P, Wq[:, :, :], C.w_in, win[:, 0:512].rearrange(kp, p=128))
            load_w(P, WqP[:, :, :], C.w_rot, wrot[:, 0:512].rearrange(kp, p=128))
            QT = [P.sbuf(e2, "QT%d" % i, [128, 4, 512], BF16) for i in range(2)]
            pT = [P.sbuf(e2, "pT%d" % i, [128, 512], BF16) for i in range(3)]
            rsum = P.sbuf(e2, "a_rsum", [128, 512], F32)
            om = [P.sbuf(e2, "a_om%d" % i, [128, 512], F32) for i in range(2)]
            odt = P.sbuf(e2, "a_od", [128, 512], F32)
            sq = P.sbuf(e2, "a_sq", [128, 512], F32)
            rst = P.sbuf(e2, "a_rst", [128, 512], F32)
            OA = [P.sbuf(e2, "OA%d" % i, [128, 4, 512], BF16) for i in range(2)]
            hm = 0
            for ci, c in enumerate(chunks):
                t0, n, s = CH[c]
                q = QT[ci % 2]
                oa = OA[ci % 2]
                for h in range(4):
                    bk = P.psum[7]
                    bp = P.psum[6]
                    proj_fm(P, bk, Wq, h * 128, C.HT[c], n)
                    if s == 0:
                        proj_fm(P, bp, WqP, h * 128, C.HT[c], n)
                        rope_evac(C, q[:, h, 0:n], bk, bp, t0, n, t1, t2)
                    else:
                        P.copy(q[:, h, 0:n], bk[:, 0:n], eng='act')
                tiles = list(range(18)) if s == 0 else [16, 17]
                for h in range(4):
                    for m in range(2):
                        bO = P.psum[3 + (hm % 2)]
                        bS = P.psum[5]
                        hm += 1
                        pb = slice(m * 64, (m + 1) * 64)

                        def st(i):
                            kt = tiles[i]
                            P.mm(P.psum[i % 3][:, 0:n], KT[pb, h, kt * 128:(kt + 1) * 128], q[pb, h, 0:n])
                        st(0)
                        for i, kt in enumerate(tiles):
                            if i + 1 < len(tiles):
                                st(i + 1)
                            p = pT[i % 3]
                            P.act(p[:, 0:n], P.psum[i % 3][:, 0:n], AF.Exp, scale=0.125)
                            P.mm(bO[:, 0:n], Vt[:, kt, h * 128:(h + 1) * 128], p[:, 0:n],
                                 start=(i == 0), stop=(i == len(tiles) - 1))
                            P.mm(bS[:, 0:n], C.ones_b[:, :], p[:, 0:n],
                                 start=(i == 0), stop=(i == len(tiles) - 1))
                        P.op('dve', lambda e: e.reciprocal(out=rsum[:, 0:n].ap, in_=bS[:, 0:n].ap),
                             reads=[bS[:, 0:n]], writes=[rsum[:, 0:n]])
                        P.tt(om[m][:, 0:n], bO[:, 0:n], rsum[:, 0:n], ALU.mult)
                    P.stt(odt[:, 0:n], om[1][:, 0:n], C.lamneg[:, 0:1], om[0][:, 0:n], ALU.mult, ALU.add)
                    P.tt(sq[:, 0:n], odt[:, 0:n], odt[:, 0:n], ALU.mult, eng='pool')
                    bN = P.psum[6]
                    P.mm(bN[:, 0:n], C.ones_f[:, :], sq[:, 0:n])
                    rstd_from_bank(P, rst, bN, n, 1.0 / 128)
                    P.stt(oa[:, h, 0:n], odt[:, 0:n], C.sgc[:, 0:1], rst[:, 0:n], ALU.mult, ALU.mult)
                P.dma('sp', C.od(0, c), oa[:, :, 0:n])
            P.barrier()
            P.release_dsems([Wq, WqP] + QT + OA)
        P.release_dsems(rel)


def na_tiles(r):
    rs_ = min(max(r - 4, 0), 24)
    out = []
    for a in range(rs_ // 2, (rs_ + 7) // 2 + 1):
        r0, r1 = 2 * a, 2 * a + 1
        v0 = rs_ <= r0 < rs_ + 8
        v1 = rs_ <= r1 < rs_ + 8
        if v0 and v1:
            idx = r0 - r + 7
            assert 0 <= idx <= 13
        elif v1:
            assert r1 - r + 7 == 3
            idx = 14
        else:
            assert v0 and r0 - r + 7 == 10
            idx = 15
        out.append((a, idx))
    return out


def phase_b(C, l, chunks):
    P = C.P
    win = C.w_in.h.ap()[l]
    kp = "(k p) c -> p k c"
    with ExitStack() as es:
        KT = P.sbuf(es, "KN", [128, 4, NT], BF16, slot_axis=1)
        Vt = P.sbuf(es, "VN", [128, 18, 512], BF16, slot_axis=1)
        NAT = P.sbuf(es, "NAT", [128, 16, 512], BF16)
        for i0 in range(0, 16, 4):
            load_w(P, NAT[:, i0:i0 + 4, :], C.naT, C.naT.h.ap()[l][i0:i0 + 4].rearrange("i p c -> p i c"))
        rel = [KT, Vt, NAT]
        with ExitStack() as e1:
            Wk = P.sbuf(e1, "Wkn", [128, 8, 512], BF16)
            Wv = P.sbuf(e1, "Wvn", [128, 8, 512], BF16)
            load_w(P, Wk[:, :, :], C.w_in, win[:, 2048:2560].rearrange(kp, p=128))
            load_w(P, Wv[:, :, :], C.w_in, win[:, 2560:3072].rearrange(kp, p=128))
            it = 0
            for c in range(5):
                t0, n, s = CH[c]
                for h in range(4):
                    bk = P.psum[it % 8]
                    it += 1
                    proj_fm(P, bk, Wk, h * 128, C.HT[c], n)
                    P.copy(KT[:, h, t0:t0 + n], bk[:, 0:n], eng=('act' if h % 2 else 'dve'))
                for tl in range(n // 128):
                    tile = t0 // 128 + tl
                    bv = P.psum[it % 8]
                    it += 1
                    for k in range(8):
                        P.mm(bv[:, 0:512], C.HT[c][:, k, tl * 128:(tl + 1) * 128], Wv[:, k, :],
                             start=(k == 0), stop=(k == 7))
                    P.copy(Vt[:, tile, :], bv[:, 0:512], eng=('act' if tl % 2 else 'dve'))
            P.barrier()
            P.release_dsems([Wk, Wv])
        with ExitStack() as e2:
            Wq = P.sbuf(e2, "Wqn", [128, 8, 512], BF16)
            load_w(P, Wq[:, :, :], C.w_in, win[:, 1536:2048].rearrange(kp, p=128))
            QT = [P.sbuf(e2, "QN%d" % i, [128, 4, 512], BF16) for i in range(2)]
            pT = [P.sbuf(e2, "pN%d" % i, [128, 512], BF16) for i in range(3)]
            rsum = P.sbuf(e2, "b_rsum", [128, 512], F32)
            OB = [P.sbuf(e2, "OB%d" % i, [128, 4, 512], BF16) for i in range(2)]
            rowi = 0
            for ci, c in enumerate(chunks):
                t0, n, s = CH[c]
                q = QT[ci % 2]
                ob = OB[ci % 2]
                for h in range(4):
                    bk = P.psum[7]
                    proj_fm(P, bk, Wq, h * 128, C.HT[c], n)
                    P.act(q[:, h, 0:n], bk[:, 0:n], AF.Identity, scale=0.125)
                for rr in range(n // 64):
                    rq = rr * 64
                    if s == 0:
                        r = t0 // 64 + rr
                        tiles = na_tiles(r) + [(16, None), (17, None)]
                    else:
                        tiles = [(16, None), (17, None)]
                    bO = P.psum[4 + (rowi % 2)]
                    bS = P.psum[6]
                    rowi += 1

                    def st(i):
                        a, idx = tiles[i]
                        for par in range(2):
                            bk_ = P.psum[(i % 2) * 2 + par]
                            pb = slice(par * 64, par * 64 + 64)
                            if idx is not None:
                                P.mm(bk_[:, 0:256], C.ident_b[:, :], NAT[:, idx, par * 256:(par + 1) * 256],
                                     start=True, stop=False, sgc=True)
                            for cc in range(4):
                                P.mm(bk_[:, cc * 64:(cc + 1) * 64], KT[pb, cc, a * 128:(a + 1) * 128],
                                     q[pb, cc, rq:rq + 64], start=(idx is None), stop=True, sgc=True)
                    st(0)
                    nt_ = len(tiles)
                    for i, (a, idx) in enumerate(tiles):
                        if i + 1 < nt_:
                            st(i + 1)
                        p = pT[i % 3]
                        for par in range(2):
                            P.act(p[:, par * 256:(par + 1) * 256], P.psum[(i % 2) * 2 + par][:, 0:256], AF.Exp)
                        if i == 0:
                            P.mm(bO[:, 0:512], C.zeros_b[:, :], p[:, :], start=True, stop=False, sgc=True)
                        for par in range(2):
                            for cc in range(4):
                                co = par * 256 + cc * 64
                                P.mm(bO[:, co:co + 64], Vt[:, a, cc * 128:(cc + 1) * 128],
                                     p[:, co:co + 64], start=False, stop=(i == nt_ - 1), sgc=True)
                        P.mm(bS[:, 0:512], C.ones_b[:, :], p[:, :], start=(i == 0), stop=(i == nt_ - 1))
                    P.op('dve', lambda e: e.reciprocal(out=rsum[:, :].ap, in_=bS[:, 0:512].ap),
                         reads=[bS[:, 0:512]], writes=[rsum[:, :]])
                    for par in range(2):
                        pb = slice(par * 64, par * 64 + 64)
                        o_v = ob.view(ob.h[pb, :, rq:rq + 64])
                        b_v = bO.view(bO.h[pb, par * 256:(par + 1) * 256].rearrange("p (c q) -> p c q", c=4))
                        r_v = rsum.view(rsum.h[pb, par * 256:(par + 1) * 256].rearrange("p (c q) -> p c q", c=4))
                        P.tt(o_v, b_v, r_v, ALU.mult)
                P.dma('sp', C.od(1, c), ob[:, :, 0:n])
            P.barrier()
            P.release_dsems([Wq] + QT + OB)
        P.release_dsems(rel)


def phase_c(C, l, chunks):
    P = C.P
    win = C.w_in.h.ap()[l]
    kp = "(k p) c -> p k c"
    with ExitStack() as es:
        AB = P.sbuf(es, "AB", [128, 18, 4, 256], BF16, slot_axis=1)
        CS = P.sbuf(es, "CS128", [128, 256], BF16)
        Wf = P.sbuf(es, "Wf", [128, 8, 512], BF16)
        fT = [P.sbuf(es, "fT%d" % i, [128, 4, 512], BF16) for i in range(2)]
        CNks = [P.sbuf(es, "CNk%d" % i, [128, 16, 512], BF16) for i in range(2)]
        SNks = [P.sbuf(es, "SNk%d" % i, [128, 16, 512], BF16) for i in range(2)]
        CNk, SNk = CNks[0], SNks[0]
        OC = [P.sbuf(es, "OC%d" % i, [128, 4, 512], BF16) for i in range(2)]
        load_w(P, CS[:, :], C.cs128, C.cs128.h.ap())
        load_w(P, Wf[:, :, :], C.w_in, win[:, 3072:3584].rearrange(kp, p=128))
        it = 0
        for c in range(5):
            t0, n, s = CH[c]
            if s == 1 and 4 not in chunks:
                continue
            f = fT[c % 2]
            for g_ in range(4):
                bk = P.psum[it % 4]
                it += 1
                proj_fm(P, bk, Wf, g_ * 128, C.HT[c], n)
                P.copy(f[:, g_, 0:n], bk[:, 0:n], eng=('act' if g_ % 2 else 'dve'))
            for tl in range(n // 128):
                tile = t0 // 128 + tl
                for half in range(2):
                    bk = P.psum[4 + (it % 4)]
                    it += 1
                    for gg in range(2):
                        g_ = half * 2 + gg
                        P.mm(bk[:, gg * 256:(gg + 1) * 256], f[:, g_, tl * 128:(tl + 1) * 128], CS[:, :])
                    P.copy(AB.view(AB.h[:, tile, half * 2:half * 2 + 2, :], slots=[tile]),
                           bk.view(bk.h[:, 0:512].rearrange("p (g m) -> p g m", g=2)),
                           eng=('act' if half else 'dve'))
        for kc in range(4):
            CNk, SNk = CNks[kc % 2], SNks[kc % 2]
            for t4 in range(0, 16, 4):
                load_w(P, CNk[:, t4:t4 + 4, :], C.cn,
                       C.cn.h.ap()[t4 * 128:(t4 + 4) * 128, kc * 512:(kc + 1) * 512].rearrange("(t p) c -> p t c", p=128))
                load_w(P, SNk[:, t4:t4 + 4, :], C.sn,
                       C.sn.h.ap()[t4 * 128:(t4 + 4) * 128, kc * 512:(kc + 1) * 512].rearrange("(t p) c -> p t c", p=128))
            oc = OC[kc % 2]
            for g_ in range(4):
                bk = P.psum[g_ % 4]
                for nt_ in range(16):
                    P.mm(bk[:, 0:512], AB[:, nt_, g_, 0:128], CNk[:, nt_, :], start=(nt_ == 0), stop=False)
                    P.mm(bk[:, 0:512], AB[:, nt_, g_, 128:256], SNk[:, nt_, :], start=False, stop=(nt_ == 15))
                P.copy(oc[:, g_, :], bk[:, 0:512], eng=('act' if g_ % 2 else 'dve'))
            P.dma('sp', C.od(2, kc), oc[:, :, :])
        if 4 in chunks:
            P.barrier()
            load_w(P, CNk[:, 0:2, 0:256], C.c256, C.c256.h.ap().rearrange("(t p) c -> p t c", p=128))
            load_w(P, SNk[:, 0:2, 0:256], C.s256, C.s256.h.ap().rearrange("(t p) c -> p t c", p=128))
            oc = OC[0]
            for g_ in range(4):
                bk = P.psum[g_ % 4]
                for nt_ in range(2):
                    P.mm(bk[:, 0:256], AB[:, 16 + nt_, g_, 0:128], CNk[:, nt_, 0:256], start=(nt_ == 0), stop=False)
                    P.mm(bk[:, 0:256], AB[:, 16 + nt_, g_, 128:256], SNk[:, nt_, 0:256], start=False, stop=(nt_ == 1))
                P.copy(oc[:, g_, 0:256], bk[:, 0:256], eng=('act' if g_ % 2 else 'dve'))
            P.dma('sp', C.od(2, 4), oc[:, :, 0:256])
        P.barrier()
        P.release_dsems([AB, CS, Wf] + CNks + SNks + fT + OC)


def phase_out(C, l, chunks):
    P = C.P
    win = C.w_in.h.ap()[l]
    with ExitStack() as es:
        MG = [P.sbuf(es, "MG%d" % c, [128, 8, CH[c][1]], BF16) for c in range(5)]
        Oi = [P.sbuf(es, "Oi%d" % c, [128, 4, CH[c][1]], BF16) for c in range(5)]
        Wb = [P.sbuf(es, "Wb%d" % i, [128, 4, 128], BF16) for i in range(2)]
        Wg = [P.sbuf(es, "Wg%d" % i, [128, 8, 128], BF16) for i in range(2)]
        sg = [P.sbuf(es, "sg%d" % i, [128, 512], F32) for i in range(2)]
        tm = [P.sbuf(es, "otm%d" % i, [128, 512], F32) for i in range(2)]
        it = 0
        for i in range(3):
            for c in chunks:
                t0, n, s = CH[c]
                P.dma('sp', Oi[c][:, :, :], C.od(i, c))
            for dc in range(8):
                wb = Wb[it % 2]
                wg = Wg[it % 2]
                load_w(P, wb[:, :, :], C.w_branch,
                       C.w_branch.h.ap()[l, i][:, dc * 128:(dc + 1) * 128].rearrange("(k p) c -> p k c", p=128))
                gc0 = 3584 + i * 1024 + dc * 128
                load_w(P, wg[:, :, :], C.w_in, win[:, gc0:gc0 + 128].rearrange("(k p) c -> p k c", p=128))
                for c in chunks:
                    t0, n, s = CH[c]
                    bP = P.psum[(2 * it) % 8]
                    bG = P.psum[(2 * it + 1) % 8]
                    it += 1
                    for k in range(4):
                        P.mm(bP[:, 0:n], wb[:, k, :], Oi[c][:, k, 0:n], start=(k == 0), stop=(k == 3))
                    for k in range(8):
                        P.mm(bG[:, 0:n], wg[:, k, :], C.HT[c][:, k, 0:n], start=(k == 0), stop=(k == 7))
                    s_ = sg[it % 2]
                    P.act(s_[:, 0:n], bG[:, 0:n], AF.Sigmoid)
                    if i == 0:
                        P.tt(MG[c][:, dc, 0:n], bP[:, 0:n], s_[:, 0:n], ALU.mult)
                    else:
                        t_ = tm[it % 2]
                        P.tt(t_[:, 0:n], bP[:, 0:n], s_[:, 0:n], ALU.mult)
                        P.tt(MG[c][:, dc, 0:n], MG[c][:, dc, 0:n], t_[:, 0:n], ALU.add, eng='pool')
        P.barrier()
        P.release_dsems(Oi + Wb + Wg)
        with ExitStack() as e2:
            Wo = P.sbuf(e2, "Wo", [128, 8, D], BF16)
            xck = [P.sbuf(e2, "ox%d" % i, [128, 8, 512], F32) for i in range(2)]
            load_w(P, Wo[:, :, :], C.w_out, C.w_out.h.ap()[l].rearrange("(k p) c -> p k c", p=128))
            it = 0
            for ci, c in enumerate(chunks):
                t0, n, s = CH[c]
                xc = xck[ci % 2]
                P.dma('sp', xc[:, :, 0:n], C.xd(c))
                for dc in range(8):
                    bk = P.psum[it % 8]
                    it += 1
                    for k in range(8):
                        P.mm(bk[:, 0:n], Wo[:, k, dc * 128:(dc + 1) * 128], MG[c][:, k, 0:n],
                             start=(k == 0), stop=(k == 7))
                    P.stt(xc[:, dc, 0:n], bk[:, 0:n], C.modT[:, s, 16 + dc:17 + dc], xc[:, dc, 0:n], ALU.mult, ALU.add)
                P.dma('sp', C.xd(c), xc[:, :, 0:n])
            P.barrier()
            P.release_dsems([Wo] + xck)


def phase_moe(C, l, chunks):
    P = C.P
    with ExitStack() as es:
        C.XT = [P.sbuf(es, "XT%d" % c, [128, 8, CH[c][1]], F32, slot_axis=1) for c in range(5)]
        C.H2T = [P.sbuf(es, "H2T%d" % c, [128, 8, CH[c][1]], BF16) for c in range(5)]
        C.WT = P.sbuf(es, "WT", [32, NT], F32)
        for c in chunks:
            P.dma('sp', C.XT[c][:, :, :], C.xd(c))
        phase_h(C, l, 2, chunks)
        wmk = [P.sbuf(es, "wmk%d" % i, [32, 512], BF16) for i in range(2)]
        bdn = P.sbuf(es, "bdn", [32, D], F32)
        bgu = P.sbuf(es, "bgu", [128, 32, 16], F32)
        P.dma('sp', bdn[:, :], C.b_down.view(C.b_down.h.ap()[l]))
        P.dma('sp', bgu[:, :, :], C.b_guT.view(C.b_guT.h.ap()[l]))
        bgu1 = P.sbuf(es, "bgu1", [128, 32, 8], F32)
        P.ts(bgu1[:, :, :], bgu[:, :, 8:16], 1.0, None, ALU.add)
        actT = [[P.sbuf(es, "actT%d_%d" % (i, c), [128, 2, CH[c][1]], BF16, slot_axis=1) for c in range(5)]
                for i in range(2)]
        wgu = [P.sbuf(es, "wgu%d" % i, [128, 8, 2, 256], BF16) for i in range(2)]
        wd = [P.sbuf(es, "wd%d" % i, [128, 2, D], BF16) for i in range(3)]
        g1 = [P.sbuf(es, "g1_%d" % i, [128, 512], BF16) for i in range(2)]
        sgm = [P.sbuf(es, "sgm%d" % i, [128, 512], BF16) for i in range(2)]
        u0 = [P.sbuf(es, "u0_%d" % i, [128, 512], BF16) for i in range(2)]
        tq = [P.sbuf(es, "tq%d" % i, [128, 512], BF16) for i in range(4)]
        it = 0
        for c in chunks:
            t0, n, s = CH[c]
            for dc in range(8):
                bk = P.psum[it % 2]
                it += 1
                P.mm(bk[:, 0:n], bdn[0:32, dc * 128:(dc + 1) * 128], C.WT[0:32, t0:t0 + n])
                P.stt(C.XT[c][:, dc, 0:n], bk[:, 0:n], C.modT[:, s, 40 + dc:41 + dc], C.XT[c][:, dc, 0:n],
                      ALU.mult, ALU.add)
        wgu_ap = C.w_gate_up.h.ap()
        wd_ap = C.w_down.h.ap()
        quarters = [(e, qq) for e in range(C.NE) for qq in range(4)]

        def issue_weights(Qi):
            e, qq = quarters[Qi]
            wg = wgu[Qi % 2]
            wdn = wd[Qi % 3]
            c0 = qq * 256
            P.dma('pool', wg[:, :, 0, :], C.w_gate_up.view(
                wgu_ap[l, e][:, c0:c0 + 256].rearrange("(k p) c -> p k c", p=128)))
            P.dma('pool', wg[:, :, 1, :], C.w_gate_up.view(
                wgu_ap[l, e][:, 1024 + c0:1024 + c0 + 256].rearrange("(k p) c -> p k c", p=128)), first=False)
            P.dma('pool', wdn[:, :, :], C.w_down.view(
                wd_ap[l, e][c0:c0 + 256, :].rearrange("(j p) c -> p j c", p=128)))

        st_ = {"u": 0, "d": 0}
        pend = []
        e2done = {}

        def emit_e2():
            Qi, c, j, t_, bW = pend.pop(0)
            t0, n, s = CH[c]
            P.tt(actT[Qi % 2][c][:, j, 0:n], t_[:, 0:n], bW[:, 0:n], ALU.mult)
            e2done[(Qi, c)] = e2done.get((Qi, c), 0) + 1

        def emit_d(Qi, c, dc):
            t0, n, s = CH[c]
            wdn = wd[Qi % 3]
            bD = P.psum[4 + (st_["d"] % 2)]
            st_["d"] += 1
            for j in range(2):
                P.mm(bD[:, 0:n], wdn[:, j, dc * 128:(dc + 1) * 128], actT[Qi % 2][c][:, j, 0:n],
                     start=(j == 0), stop=(j == 1))
            P.stt(C.XT[c][:, dc, 0:n], bD[:, 0:n], C.modT[:, s, 40 + dc:41 + dc], C.XT[c][:, dc, 0:n],
                  ALU.mult, ALU.add)

        issue_weights(0)
        dq = []
        nun = len(chunks) * 2
        per_unit = (len(chunks) * 8 + nun - 1) // nun
        cidx = 0
        for Qi, (e, qq) in enumerate(quarters):
            if Qi + 1 < len(quarters):
                issue_weights(Qi + 1)
            wg = wgu[Qi % 2]
            for c in chunks:
                t0, n, s = CH[c]
                bW = P.psum[6 + (cidx % 2)]
                wm_ = wmk[cidx % 2]
                cidx += 1
                P.ts(wm_[0:32, 0:n], C.WT[0:32, t0:t0 + n], C.ident[0:32, e:e + 1], None, ALU.mult)
                P.mm(bW[:, 0:n], C.ones_b[0:32, :], wm_[0:32, 0:n])
                for j in range(2):
                    u = st_["u"]
                    st_["u"] += 1
                    ffc = qq * 2 + j
                    bG = P.psum[(2 * u) % 4]
                    bU = P.psum[(2 * u + 1) % 4]
                    def try_d():
                        if dq and e2done.get((dq[0][0], dq[0][1]), 0) == 2:
                            emit_d(*dq.pop(0))
                    for k in range(8):
                        P.mm(bG[:, 0:n], wg[:, k, 0, j * 128:(j + 1) * 128], C.H2T[c][:, k, 0:n],
                             start=(k == 0), stop=(k == 7))
                        if k == 3 or k == 7:
                            try_d()
                    for k in range(8):
                        P.mm(bU[:, 0:n], wg[:, k, 1, j * 128:(j + 1) * 128], C.H2T[c][:, k, 0:n],
                             start=(k == 0), stop=(k == 7))
                        if k == 3 or k == 7:
                            try_d()
                    g_ = g1[u % 2]
                    s_ = sgm[u % 2]
                    u_ = u0[u % 2]
                    t_ = tq[u % 4]
                    P.ts(g_[:, 0:n], bG[:, 0:n], bgu[:, e, ffc:ffc + 1], 7.0, ALU.add, ALU.min)
                    P.act(s_[:, 0:n], g_[:, 0:n], AF.Sigmoid, scale=1.702)
                    P.act(u_[:, 0:n], bU[:, 0:n], AF.Identity, bias=bgu1[:, e, ffc:ffc + 1])
                    P.ts(u_[:, 0:n], u_[:, 0:n], 8.0, -6.0, ALU.min, ALU.max, eng='pool')
                    P.tt(t_[:, 0:n], g_[:, 0:n], s_[:, 0:n], ALU.mult, eng='pool')
                    P.tt(t_[:, 0:n], t_[:, 0:n], u_[:, 0:n], ALU.mult, eng='pool')
                    pend.append((Qi, c, j, t_, bW))
                    if len(pend) > 2:
                        emit_e2()
            while dq and dq[0][0] < Qi:
                while e2done.get((dq[0][0], dq[0][1]), 0) < 2:
                    emit_e2()
                emit_d(*dq.pop(0))
            dq.extend((Qi, c, dc) for c in chunks for dc in range(8))
        while pend:
            emit_e2()
        while dq:
            emit_d(*dq.pop(0))
        for c in chunks:
            P.dma('sp', C.xd(c), C.XT[c][:, :, :])
        P.barrier()
        P.release_dsems(C.XT + C.H2T + [C.WT, bdn, bgu] + actT[0] + actT[1] + wgu + wd)


def phase_final(C):
    P = C.P
    with ExitStack() as es:
        xck = [P.sbuf(es, "fx%d" % i, [128, 8, 512], F32) for i in range(2)]
        sqt = [P.sbuf(es, "fsq%d" % i, [128, 512], F32) for i in range(2)]
        rs = [P.sbuf(es, "frs%d" % i, [128, 512], F32) for i in range(2)]
        for c in range(4):
            t0, n, s = CH[c]
            xc = xck[c % 2]
            P.dma('sp', xc[:, :, :], C.xd(c))
            bank = P.psum[c % 2]
            for k in range(8):
                sq = sqt[k % 2]
                P.tt(sq[:, :], xc[:, k, :], xc[:, k, :], ALU.mult, eng='pool')
                P.mm(bank[:, 0:n], C.ones_f[:, :], sq[:, :], start=(k == 0), stop=(k == 7))
            r = rs[c % 2]
            rstd_from_bank(P, r, bank, n, 1.0 / D)
            for k in range(8):
                P.stt(xc[:, k, :], xc[:, k, :], C.gfin_s[:, k:k + 1], r[:, :], ALU.mult, ALU.mult)
            P.dma('sp', C.outT.view(C.outT.h.ap()[:, t0:t0 + n].rearrange("(k p) t -> p k t", p=128)), xc[:, :, :])
        P.barrier()
        P.release_dsems(xck)


def _rope_tables():
    t = np.arange(NL)
    row = (t // 64).astype(np.float32)
    col = (t % 64).astype(np.float32)
    freqs = (np.float32(10000.0) ** (-np.arange(16, dtype=np.float32) / np.float32(16))).astype(np.float32)
    Ct = np.zeros((128, NL), np.float32)
    St = np.zeros((128, NL), np.float32)
    for p in range(128):
        d = p % 64
        axis = d // 32
        half = (d % 32) // 16
        i = d % 16
        ang = (row if axis == 0 else col) * freqs[i]
        Ct[p] = np.cos(ang)
        St[p] = (-1.0 if half == 0 else 1.0) * np.sin(ang)
    return Ct, St


def _rot_cols():
    idx = np.arange(512)
    d = idx % 64
    return (idx - d) + (d ^ 16)


def _dft(n, scale):
    k = np.arange(n, dtype=np.int64)
    ph = (np.outer(k, k) % n).astype(np.float64) * (2.0 * np.pi / n)
    return (np.cos(ph) * scale).astype(np.float32), (np.sin(ph) * scale).astype(np.float32)


def _na_tables(rpb):
    q = np.arange(64)
    k = np.arange(64)
    cs = np.clip(q - 8, 0, 48)
    mask = (k[None, :] >= cs[:, None]) & (k[None, :] < cs[:, None] + 16)
    dc = np.clip(k[None, :] - q[:, None], -15, 15) + 15
    Bt = rpb[:, :, :, dc]
    Bt = np.where(mask[None, None, None], Bt, np.float32(NEG)).astype(np.float32)
    Bt = Bt.transpose(0, 2, 4, 1, 3)
    Bt = np.ascontiguousarray(Bt[:, :, :, [0, 2, 4, 6, 1, 3, 5, 7], :]).reshape(4, 15, 64, 512)
    M = np.full((4, 64, 512), NEG, np.float32)
    T = np.empty((4, 16, 128, 512), np.float32)
    for i in range(14):
        T[:, i, 0:64] = Bt[:, i]
        T[:, i, 64:128] = Bt[:, i + 1]
    T[:, 14, 0:64] = M
    T[:, 14, 64:128] = Bt[:, 3]
    T[:, 15, 0:64] = Bt[:, 10]
    T[:, 15, 64:128] = M
    return T


def prep_shared(inp):
    f = lambda a: np.ascontiguousarray(np.asarray(a, dtype=np.float32))
    sh = {}
    sh["w_mod"] = f(inp["w_mod"])
    sh["b_modT"] = f(np.asarray(inp["b_mod"]).reshape(4, 48, 128).transpose(0, 2, 1))
    sh["gmix"] = f(np.asarray(inp["norm_mix_g"]).reshape(4, 8, 128).transpose(2, 0, 1))
    sh["gffn"] = f(np.asarray(inp["norm_ffn_g"]).reshape(4, 8, 128).transpose(2, 0, 1))
    sh["gfin"] = f(np.asarray(inp["final_g"]).reshape(8, 128).T)
    w_in = f(inp["w_in"])
    sh["w_in"] = w_in
    rc = _rot_cols()
    sh["w_rot"] = f(np.concatenate([w_in[:, :, 0:512][:, :, rc], w_in[:, :, 512:1024][:, :, rc]], axis=2))
    Ct, St = _rope_tables()
    sh["ropeC"] = Ct
    sh["ropeS"] = St
    sh["da_lam"] = f(np.asarray(inp["da_lambda"]).reshape(4, 256))
    sh["subln"] = f(np.asarray(inp["da_subln_g"]).T)
    sh["naT"] = _na_tables(np.asarray(inp["na_rpb"], dtype=np.float32))
    sh["w_branch"] = f(inp["w_branch"])
    sh["w_out"] = f(inp["w_out"])
    sh["w_router"] = f(inp["w_router"])
    sh["b_router"] = f(inp["b_router"])
    sh["w_gate_up"] = f(inp["w_gate_up"])
    sh["b_guT"] = f(np.asarray(inp["b_gate_up"]).reshape(4, 32, 16, 128).transpose(0, 3, 1, 2))
    sh["w_down"] = f(inp["w_down"])
    sh["b_down"] = f(inp["b_down"])
    c128, s128 = _dft(128, 1.0 / np.sqrt(128.0))
    sh["cs128"] = f(np.concatenate([c128, s128], axis=1))
    cn, sn = _dft(NL, 1.0 / np.sqrt(float(NL)))
    sh["cn"] = cn
    sh["sn"] = f(-sn)
    c256, s256 = _dft(256, 1.0 / 16.0)
    sh["c256"] = c256
    sh["s256"] = f(-s256)
    sh["ident"] = np.eye(128, dtype=np.float32)
    return sh


def prep_core(inp, b):
    x = np.asarray(inp["x"][b], dtype=np.float32)
    ctx = np.asarray(inp["ctx"][b], dtype=np.float32)
    xT = np.ascontiguousarray(np.concatenate([x, ctx], axis=0).T)
    c = np.asarray(inp["c"][b], dtype=np.float32).reshape(8, 128).T
    cctx = np.asarray(inp["c_ctx"], dtype=np.float32).reshape(8, 128).T
    cc = np.ascontiguousarray(np.stack([c, cctx], axis=-1))
    return {"xT": xT, "cc": cc}


_CACHE = {}


def kernel(**inputs):
    if "nc" not in _CACHE:
        _CACHE["nc"] = build(L=4)[0]
    nc = _CACHE["nc"]
    sh = prep_shared(inputs)
    in_maps = []
    for b in range(8):
        m = dict(sh)
        m.update(prep_core(inputs, b))
        in_maps.append(m)
    res = run_bass_kernel_spmd(nc, in_maps, core_ids=list(range(8)))
    out = np.stack([np.ascontiguousarray(res.results[b]["outT"].T) for b in range(8)], axis=0)
    return out.astype(np.float32)
```

```python
import math
import os
import numpy as np
BDBG = int(os.environ.get('BDBG', '0'))
import ml_dtypes
from contextlib import ExitStack
import concourse.bass as bass
import concourse.mybir as mybir
from concourse.bass_utils import run_bass_kernel_spmd

F32 = mybir.dt.float32
BF16 = mybir.dt.bfloat16
I32 = mybir.dt.int32
AF = mybir.ActivationFunctionType
ALU = mybir.AluOpType
AX = mybir.AxisListType

EPOCH = 12000
COMPUTE = ('pe', 'act', 'dve', 'pool')
SAME_ENGINE_SYNC = ('act', 'dve', 'pool')


class Buf:
    __slots__ = ('name', 'w', 'r', 'dsem')

    def __init__(self, name):
        self.name = name
        self.w = None
        self.r = {}
        self.dsem = None


class DSem:
    __slots__ = ('sem', 'cnt', 'idx')

    def __init__(self, sem, idx):
        self.sem = sem
        self.cnt = 0
        self.idx = idx


class V:
    __slots__ = ('ap', 'bufs', 'dram')

    def __init__(self, ap, bufs, dram=False):
        self.ap = ap
        self.bufs = bufs
        self.dram = dram


class TT:
    def __init__(self, P, handle, name, shape, slot_axis=None, is_dram=False, nslots=None):
        self.P = P
        self.h = handle
        self.name = name
        self.shape = list(shape)
        self.slot_axis = slot_axis
        self.is_dram = is_dram
        if nslots is not None:
            self.bufs = [Buf("%s.%d" % (name, i)) for i in range(nslots)]
        elif slot_axis is None:
            self.bufs = [Buf(name)]
        else:
            self.bufs = [Buf("%s.%d" % (name, i)) for i in range(shape[slot_axis])]

    def base(self):
        return self.h.ap() if self.is_dram else self.h

    def __getitem__(self, idx):
        if not isinstance(idx, tuple):
            idx = (idx,)
        ap = self.base()[idx]
        if self.slot_axis is None:
            return V(ap, self.bufs, self.is_dram)
        if self.slot_axis < len(idx):
            s = idx[self.slot_axis]
            if isinstance(s, int):
                return V(ap, [self.bufs[s]], self.is_dram)
            if isinstance(s, slice):
                return V(ap, self.bufs[s], self.is_dram)
        return V(ap, self.bufs, self.is_dram)

    def view(self, ap, slots=None):
        if slots is None:
            return V(ap, self.bufs, self.is_dram)
        return V(ap, [self.bufs[s] for s in slots], self.is_dram)


class Prog:
    def __init__(self, nc, es, n_dsem=56):
        self.nc = nc
        self.es = es
        self.eng = {'pe': nc.tensor, 'act': nc.scalar, 'dve': nc.vector, 'pool': nc.gpsimd, 'sp': nc.sync}
        self.cnt = {e: 0 for e in COMPUTE}
        self.esems = {e: [] for e in COMPUTE}
        self.seen_e = {e: {c: 0 for c in COMPUTE} for e in self.eng}
        self.seen_d = {e: {} for e in self.eng}
        self.dpool = []
        for i in range(n_dsem):
            self.dpool.append(DSem(es.enter_context(nc.semaphore("dsem%d" % i)), i))
        self.dfree = list(self.dpool)
        self.dused = []
        self.n_instr = 0
        self.n_wait = 0
        self.psum = []
        self.psum_i = 0

    def sbuf(self, es, name, shape, dtype, slot_axis=None):
        self.uid = getattr(self, 'uid', 0) + 1
        name = "s%d_%s" % (self.uid, name)
        h = es.enter_context(self.nc.sbuf_tensor(name, list(shape), dtype))
        return TT(self, h, name, shape, slot_axis)

    def dram(self, name, shape, dtype, kind="Internal", slot_axis=None, nslots=None):
        h = self.nc.dram_tensor(name, list(shape), dtype, kind=kind)
        return TT(self, h, name, shape, slot_axis, is_dram=True, nslots=nslots)

    def init_psum(self, es, n=8):
        for i in range(n):
            h = es.enter_context(self.nc.psum_tensor("psb%d" % i, [128, 512], F32))
            self.psum.append(TT(self, h, "psb%d" % i, [128, 512]))

    def bank(self):
        t = self.psum[self.psum_i % len(self.psum)]
        self.psum_i += 1
        return t

    def _esem(self, e, epoch):
        while len(self.esems[e]) <= epoch:
            self.esems[e].append(self.es.enter_context(self.nc.semaphore("es_%s_%d" % (e, len(self.esems[e])))))
        return self.esems[e][epoch]

    def _wait(self, e, t):
        if t is None:
            return
        if t[0] == 'e':
            _, c, n = t
            if c == e and e not in SAME_ENGINE_SYNC:
                return
            if self.seen_e[e][c] >= n:
                return
            self.seen_e[e][c] = n
            epoch = (n - 1) // EPOCH
            self.eng[e].wait_ge(self._esem(c, epoch), n - epoch * EPOCH)
            self.n_wait += 1
        else:
            _, ds, n = t
            if self.seen_d[e].get(ds.idx, 0) >= n:
                return
            self.seen_d[e][ds.idx] = n
            self.eng[e].wait_ge(ds.sem, n)
            self.n_wait += 1

    @staticmethod
    def _tkey(t):
        return (t[0], t[1] if t[0] == 'e' else t[1].idx)

    def _deps(self, e, rbufs, wbufs):
        for b in rbufs:
            self._wait(e, b.w)
        for b in wbufs:
            self._wait(e, b.w)
            for t in list(b.r.values()):
                self._wait(e, t)

    def _record(self, t, rbufs, wbufs):
        k = self._tkey(t)
        for b in rbufs:
            b.r[k] = t
        for b in wbufs:
            b.w = t
            b.r = {}

    def op(self, e, fn, reads=(), writes=()):
        rbufs = [b for v in reads for b in v.bufs]
        wbufs = [b for v in writes for b in v.bufs]
        self._deps(e, rbufs, wbufs)
        ins = fn(self.eng[e])
        self.cnt[e] += 1
        n = self.cnt[e]
        ep = (n - 1) // EPOCH
        ins.then_inc(self._esem(e, ep), 1)
        self.seen_e[e][e] = max(self.seen_e[e][e], 0)
        self._record(('e', e, n), rbufs, wbufs)
        self.n_instr += 1
        return ins

    def _dsem_for(self, buf):
        if buf.dsem is None:
            if not self.dfree:
                raise RuntimeError("out of DMA semaphores")
            buf.dsem = self.dfree.pop()
            self.dused.append(buf)
        return buf.dsem

    def release_dsems(self, tts):
        for tt in tts:
            for b in tt.bufs:
                if b.dsem is not None:
                    self.dfree.append(b.dsem)
                    b.dsem = None
                    if b in self.dused:
                        self.dused.remove(b)

    def dma(self, q, out, in_, semv=None, first=True, **kw):
        if semv is None:
            semv = in_ if out.dram else out
        sb = semv.bufs[0]
        ds = self._dsem_for(sb)
        rbufs = list(in_.bufs)
        wbufs = list(out.bufs)
        if first:
            self._deps(q, rbufs, wbufs)
            if ds.cnt:
                self._wait(q, ('d', ds, ds.cnt))
        ins = self.eng[q].dma_start(out=out.ap, in_=in_.ap, **kw)
        ds.cnt += 16
        ins.then_inc(ds.sem, 16)
        self._record(('d', ds, ds.cnt), rbufs, wbufs)
        self.n_instr += 1
        return ins

    def barrier(self, engines=None):
        engines = engines or list(self.eng.keys())
        for e in engines:
            for c in COMPUTE:
                if self.cnt[c]:
                    self._wait(e, ('e', c, self.cnt[c]))
            for ds in self.dpool:
                if ds.cnt:
                    self._wait(e, ('d', ds, ds.cnt))

    def mm(self, out, lhsT, rhs, start=True, stop=True, sgc=False):
        if sgc:
            return self.op('pe', lambda e: e.matmul(out.ap, lhsT.ap, rhs.ap, start=start, stop=stop, skip_group_check=True),
                           reads=[lhsT, rhs], writes=[out])
        return self.op('pe', lambda e: e.matmul(out.ap, lhsT.ap, rhs.ap, start=start, stop=stop),
                       reads=[lhsT, rhs], writes=[out])

    def act(self, out, in_, func, bias=None, scale=None, eng='act'):
        kw = {}
        reads = [in_]
        if bias is not None:
            if isinstance(bias, V):
                kw['bias'] = bias.ap
                reads.append(bias)
            else:
                kw['bias'] = bias
        if scale is not None:
            if isinstance(scale, V):
                kw['scale'] = scale.ap
                reads.append(scale)
            else:
                kw['scale'] = scale
        return self.op(eng, lambda e: e.activation(out=out.ap, in_=in_.ap, func=func, **kw),
                       reads=reads, writes=[out])

    def tt(self, out, in0, in1, op, eng='dve'):
        return self.op(eng, lambda e: e.tensor_tensor(out=out.ap, in0=in0.ap, in1=in1.ap, op=op),
                       reads=[in0, in1], writes=[out])

    def ts(self, out, in0, s1, s2, op0, op1=None, eng='dve'):
        reads = [in0]
        a1 = s1
        a2 = s2
        if isinstance(s1, V):
            a1 = s1.ap
            reads.append(s1)
        if isinstance(s2, V):
            a2 = s2.ap
            reads.append(s2)
        if op1 is None:
            return self.op(eng, lambda e: e.tensor_scalar(out=out.ap, in0=in0.ap, scalar1=a1, scalar2=None, op0=op0),
                           reads=reads, writes=[out])
        return self.op(eng, lambda e: e.tensor_scalar(out=out.ap, in0=in0.ap, scalar1=a1, scalar2=a2, op0=op0, op1=op1),
                       reads=reads, writes=[out])

    def stt(self, out, in0, s, in1, op0, op1, eng='dve'):
        reads = [in0, in1]
        a = s
        if isinstance(s, V):
            a = s.ap
            reads.append(s)
        return self.op(eng, lambda e: e.scalar_tensor_tensor(out=out.ap, in0=in0.ap, scalar=a, in1=in1.ap, op0=op0, op1=op1),
                       reads=reads, writes=[out])

    def copy(self, out, in_, eng='dve'):
        if eng == 'act':
            return self.act(out, in_, AF.Identity)
        return self.op(eng, lambda e: e.tensor_copy(out=out.ap, in_=in_.ap), reads=[in_], writes=[out])

    def memset(self, out, val, eng='dve'):
        return self.op(eng, lambda e: e.memset(out.ap, val), reads=[], writes=[out])


D = 1024
NT = 2304
NL = 2048
CH = [(0, 512, 0), (512, 512, 0), (1024, 512, 0), (1536, 512, 0), (2048, 256, 1)]
EPS = 1e-6
NEG = -30000.0


class Ctx:
    pass


def build(L=4, stop=None, dbg=False, NE=32):
    nc = bass.Bass("TRN2", target_bir_lowering=False)
    es = ExitStack()
    P = Prog(nc, es)
    P.init_psum(es)
    C = Ctx()
    C.P = P
    C.nc = nc
    C.L = L
    C.NE = NE

    def din(name, shape, dt=F32):
        return P.dram(name, shape, dt, kind="ExternalInput")

    C.xT = din("xT", [D, NT])
    C.cc = din("cc", [128, 8, 2])
    C.w_mod = din("w_mod", [4, D, 6144])
    C.b_modT = din("b_modT", [4, 128, 48])
    C.gmix = din("gmix", [128, 4, 8])
    C.gffn = din("gffn", [128, 4, 8])
    C.gfin = din("gfin", [128, 8])
    C.w_in = din("w_in", [4, D, 6656])
    C.w_rot = din("w_rot", [4, D, 1024])
    C.ropeC = din("ropeC", [128, NL])
    C.ropeS = din("ropeS", [128, NL])
    C.da_lam = din("da_lam", [4, 256])
    C.subln = din("subln", [128, 4])
    C.naT = din("naT", [4, 16, 128, 512])
    C.w_branch = din("w_branch", [4, 3, 512, D])
    C.w_out = din("w_out", [4, D, D])
    C.w_router = din("w_router", [4, D, 32])
    C.b_router = din("b_router", [4, 32])
    C.w_gate_up = din("w_gate_up", [4, NE, D, 2048])
    C.b_guT = din("b_guT", [4, 128, 32, 16])
    C.w_down = din("w_down", [4, NE, D, D])
    C.b_down = din("b_down", [4, 32, D])
    C.cs128 = din("cs128", [128, 256])
    C.cn = din("cn", [NL, NL])
    C.sn = din("sn", [NL, NL])
    C.c256 = din("c256", [256, 256])
    C.s256 = din("s256", [256, 256])
    C.identd = din("ident", [128, 128])
    C.outT = P.dram("outT", [D, NL], F32, kind="ExternalOutput")
    skind = "ExternalOutput" if dbg else "Internal"
    C.XD = P.dram("XD", [D, NT], F32, kind=skind, nslots=5)
    C.OD = [P.dram("OD%d" % i, [512, NT], BF16, kind=skind, nslots=5) for i in range(3)]
    if dbg:
        C.HD = P.dram("HD", [D, NT], BF16, kind="ExternalOutput", nslots=5)

    def xd(c):
        t0, n, s = CH[c]
        return C.XD.view(C.XD.h.ap()[:, t0:t0 + n].rearrange("(k p) t -> p k t", p=128), slots=[c])
    C.xd = xd

    def od(i, c):
        t0, n, s = CH[c]
        return C.OD[i].view(C.OD[i].h.ap()[:, t0:t0 + n].rearrange("(k p) t -> p k t", p=128), slots=[c])
    C.od = od

    g = es
    C.ones_f = P.sbuf(g, "ones_f", [128, 128], F32)
    C.ones_b = P.sbuf(g, "ones_b", [128, 128], BF16)
    C.zeros_b = P.sbuf(g, "zeros_b", [128, 128], BF16)
    C.ident = P.sbuf(g, "ident", [128, 128], F32)
    C.ident_b = P.sbuf(g, "ident_b", [128, 128], BF16)
    C.sc = P.sbuf(g, "silu_c", [128, 8, 2], F32)
    C.modT = P.sbuf(g, "modT", [128, 2, 48], F32)
    C.A1 = P.sbuf(g, "A1", [128, 2, 8], F32)
    C.A2 = P.sbuf(g, "A2", [128, 2, 8], F32)
    C.gmix_s = P.sbuf(g, "gmix_s", [128, 4, 8], F32)
    C.gffn_s = P.sbuf(g, "gffn_s", [128, 4, 8], F32)
    C.gfin_s = P.sbuf(g, "gfin_s", [128, 8], F32)
    C.subln_s = P.sbuf(g, "subln_s", [128, 4], F32)
    C.lamneg = P.sbuf(g, "lamneg", [128, 1], F32)
    C.sgc = P.sbuf(g, "sgc", [128, 1], F32)

    P.memset(C.ones_f[:, :], 1.0)
    P.memset(C.ones_b[:, :], 1.0)
    P.memset(C.zeros_b[:, :], 0.0)
    P.dma('sp', C.ident[:, :], C.identd[:, :])
    P.copy(C.ident_b[:, :], C.ident[:, :])
    P.dma('sp', C.sc[:, :, :], C.cc[:, :, :])
    P.act(C.sc[:, :, :], C.sc[:, :, :], AF.Silu)
    P.dma('sp', C.gmix_s[:, :, :], C.gmix[:, :, :])
    P.dma('sp', C.gffn_s[:, :, :], C.gffn[:, :, :])
    P.dma('sp', C.gfin_s[:, :], C.gfin[:, :])
    P.dma('sp', C.subln_s[:, :], C.subln[:, :])
    for c in range(5):
        t0, n, s = CH[c]
        P.dma('sp', C.XD.view(C.XD.h.ap()[:, t0:t0 + n], slots=[c]), C.xT.view(C.xT.h.ap()[:, t0:t0 + n]),
              semv=C.XD.view(C.XD.h.ap()[:, t0:t0 + n], slots=[c]))

    def done(tag):
        return stop is not None and stop == tag

    fin = False
    for l in range(L):
        last = (l == 3)
        chunks = [0, 1, 2, 3] if last else [0, 1, 2, 3, 4]
        phase_mod(C, l)
        P.barrier()
        if done("mod%d" % l):
            fin = True
            break
        with ExitStack() as hs:
            C.HT = [P.sbuf(hs, "HT%d" % c, [128, 8, CH[c][1]], BF16) for c in range(5)]
            phase_h(C, l, 1, [0, 1, 2, 3, 4])
            P.barrier()
            if dbg:
                for c in range(5):
                    t0, n, s = CH[c]
                    P.dma('sp', C.HD.view(C.HD.h.ap()[:, t0:t0 + n].rearrange("(k p) t -> p k t", p=128), slots=[c]),
                          C.HT[c][:, :, :])
            for tag, fn in (("a", phase_a), ("b", phase_b), ("c", phase_c), ("out", phase_out)):
                if fin:
                    break
                if done("h%d" % l):
                    fin = True
                    break
                fn(C, l, chunks)
                P.barrier()
                if done("%s%d" % (tag, l)):
                    fin = True
            P.barrier()
            P.release_dsems(C.HT)
        if fin:
            break
        phase_moe(C, l, chunks)
        P.barrier()
        if done("moe%d" % l):
            fin = True
            break
    if stop is None:
        phase_final(C)
    P.barrier()
    C.es = es
    return nc, C


def phase_mod(C, l):
    P = C.P
    lam_init = 0.8 - 0.6 * math.exp(-0.3 * l)
    with ExitStack() as es:
        wm = [P.sbuf(es, "wm%d" % i, [128, 8, 512], F32) for i in range(2)]
        bm = P.sbuf(es, "bm", [128, 48], F32)
        lamt = P.sbuf(es, "lamt", [128, 256], F32)
        pr = P.sbuf(es, "lampr", [128, 128], F32)
        s12 = P.sbuf(es, "lams12", [128, 2], F32)
        e12 = P.sbuf(es, "lame12", [128, 2], F32)
        P.dma('sp', bm[:, :], C.b_modT.view(C.b_modT.h.ap()[l]))
        bank = P.psum[0]
        for blk in range(12):
            w = wm[blk % 2]
            P.dma('sp', w[:, :, :], C.w_mod.view(
                C.w_mod.h.ap()[l][:, blk * 512:(blk + 1) * 512].rearrange("(k p) c -> p k c", p=128)))
            for jj in range(4):
                j = blk * 4 + jj
                for k in range(8):
                    P.mm(bank[:, 2 * j:2 * j + 2], w[:, k, jj * 128:(jj + 1) * 128], C.sc[:, k, :],
                         start=(k == 0), stop=(k == 7))
        for s in range(2):
            P.tt(C.modT[:, s, :], bank[:, s:96:2], bm[:, :], ALU.add)
            P.stt(C.A1[:, s, :], C.modT[:, s, 8:16], 1.0, C.gmix_s[:, l, :], ALU.add, ALU.mult)
            P.stt(C.A2[:, s, :], C.modT[:, s, 32:40], 1.0, C.gffn_s[:, l, :], ALU.add, ALU.mult)
        P.dma('sp', lamt[:, :], C.da_lam.view(C.da_lam.h.ap()[l].partition_broadcast(128)))
        P.tt(pr[:, 0:64], lamt[:, 0:64], lamt[:, 64:128], ALU.mult)
        P.tt(pr[:, 64:128], lamt[:, 128:192], lamt[:, 192:256], ALU.mult)
        P.op('dve', lambda e: e.reduce_sum(out=s12[:, 0:1].ap, in_=pr[:, 0:64].ap, axis=AX.X),
             reads=[pr[:, :]], writes=[s12[:, :]])
        P.op('dve', lambda e: e.reduce_sum(out=s12[:, 1:2].ap, in_=pr[:, 64:128].ap, axis=AX.X),
             reads=[pr[:, :]], writes=[s12[:, :]])
        P.act(e12[:, :], s12[:, :], AF.Exp)
        P.tt(C.lamneg[:, :], e12[:, 1:2], e12[:, 0:1], ALU.subtract)
        P.ts(C.lamneg[:, :], C.lamneg[:, :], -lam_init, None, ALU.add)
        P.ts(C.sgc[:, :], C.subln_s[:, l:l + 1], (1.0 - lam_init), None, ALU.mult)
        P.barrier()
        P.release_dsems(wm + [bm, lamt])


def rstd_from_bank(P, r, bank, n, inv_n):
    P.ts(r[:, 0:n], bank[:, 0:n], inv_n, EPS, ALU.mult, ALU.add)
    P.act(r[:, 0:n], r[:, 0:n], AF.Sqrt)
    P.op('dve', lambda e: e.reciprocal(out=r[:, 0:n].ap, in_=r[:, 0:n].ap), reads=[r[:, 0:n]], writes=[r[:, 0:n]])


def phase_h(C, l, which, chunks):
    P = C.P
    with ExitStack() as es:
        xck = [P.sbuf(es, "hx%d" % i, [128, 8, 512], F32) for i in range(2)] if which == 1 else []
        sqt = [P.sbuf(es, "hsq%d" % i, [128, 512], F32) for i in range(2)]
        rs = [P.sbuf(es, "hrs%d" % i, [128, 512], F32) for i in range(2)]
        tmp = [P.sbuf(es, "htm%d" % i, [128, 512], F32) for i in range(2)]
        allt = xck + sqt + rs + tmp
        if which == 2:
            h2f = P.sbuf(es, "h2f", [128, 8, 512], F32)
            wr = P.sbuf(es, "wr", [128, 8, 32], F32)
            brt = P.sbuf(es, "brt", [128, 32], F32)
            lg = P.sbuf(es, "lg", [128, 32], F32)
            m8 = P.sbuf(es, "m8", [128, 8], F32)
            mask = P.sbuf(es, "rmask", [128, 32], F32)
            negm = P.sbuf(es, "negm", [128, 1], F32)
            ex = P.sbuf(es, "rex", [128, 32], F32)
            den = P.sbuf(es, "rden", [128, 1], F32)
            wt = P.sbuf(es, "rwt", [128, 32], F32)
            allt += [h2f, wr, brt]
            P.dma('sp', wr[:, :, :], C.w_router.view(C.w_router.h.ap()[l].rearrange("(k p) e -> p k e", p=128)))
            P.dma('sp', brt[:, :], C.b_router.view(C.b_router.h.ap()[l].partition_broadcast(128)))
        for ci, c in enumerate(chunks):
            t0, n, s = CH[c]
            if which == 1:
                xc = xck[ci % 2]
                P.dma('sp', xc[:, :, 0:n], C.xd(c))
                xsrc = xc
            else:
                xsrc = C.XT[c]
            bank = P.psum[ci % 2]
            for k in range(8):
                sq = sqt[k % 2]
                P.tt(sq[:, 0:n], xsrc[:, k, 0:n], xsrc[:, k, 0:n], ALU.mult, eng='pool')
                P.mm(bank[:, 0:n], C.ones_f[:, :], sq[:, 0:n], start=(k == 0), stop=(k == 7))
            r = rs[ci % 2]
            rstd_from_bank(P, r, bank, n, 1.0 / D)
            A = C.A1 if which == 1 else C.A2
            boff = 0 if which == 1 else 24
            for k in range(8):
                tm = tmp[k % 2]
                P.stt(tm[:, 0:n], xsrc[:, k, 0:n], A[:, s, k:k + 1], r[:, 0:n], ALU.mult, ALU.mult)
                bv = C.modT[:, s, boff + k:boff + k + 1]
                if which == 1:
                    P.act(C.HT[c][:, k, 0:n], tm[:, 0:n], AF.Identity, bias=bv)
                else:
                    P.act(h2f[:, k, 0:n], tm[:, 0:n], AF.Identity, bias=bv)
                    P.copy(C.H2T[c][:, k, 0:n], h2f[:, k, 0:n], eng='pool')
            if which == 2:
                for tl in range(n // 128):
                    tile = t0 // 128 + tl
                    bankR = P.psum[2 + (tile % 2)]
                    for k in range(8):
                        P.mm(bankR[:, 0:32], h2f[:, k, tl * 128:(tl + 1) * 128], wr[:, k, :],
                             start=(k == 0), stop=(k == 7))
                    P.tt(lg[:, :], bankR[:, 0:32], brt[:, :], ALU.add)
                    P.op('dve', lambda e: e.max(out=m8[:, :].ap, in_=lg[:, :].ap), reads=[lg[:, :]], writes=[m8[:, :]])
                    P.ts(mask[:, :], lg[:, :], m8[:, 3:4], None, ALU.is_ge)
                    P.ts(negm[:, :], m8[:, 0:1], -1.0, None, ALU.mult)
                    P.act(ex[:, :], lg[:, :], AF.Exp, bias=negm[:, 0:1])
                    P.tt(ex[:, :], ex[:, :], mask[:, :], ALU.mult)
                    P.op('dve', lambda e: e.reduce_sum(out=den[:, :].ap, in_=ex[:, :].ap, axis=AX.X),
                         reads=[ex[:, :]], writes=[den[:, :]])
                    P.op('dve', lambda e: e.reciprocal(out=den[:, :].ap, in_=den[:, :].ap),
                         reads=[den[:, :]], writes=[den[:, :]])
                    P.ts(wt[:, :], ex[:, :], den[:, 0:1], None, ALU.mult)
                    bankT = P.psum[4 + (tile % 2)]
                    P.mm(bankT[0:32, 0:128], wt[:, 0:32], C.ident[:, :])
                    P.copy(C.WT[0:32, tile * 128:(tile + 1) * 128], bankT[0:32, 0:128])
        P.barrier()
        P.release_dsems(allt)


def load_w(P, dst, src_tt, ap, q='pool'):
    return P.dma(q, dst, src_tt.view(ap))


def proj_fm(P, bank, W, col0, HTc, n):
    for k in range(8):
        P.mm(bank[:, 0:n], W[:, k, col0:col0 + 128], HTc[:, k, 0:n], start=(k == 0), stop=(k == 7))


def rope_evac(C, out, bank, bankP, t0, n, t1, t2):
    P = C.P
    P.tt(t1[:, 0:n], bank[:, 0:n], C.ropeC_s[:, t0:t0 + n], ALU.mult)
    P.tt(t2[:, 0:n], bankP[:, 0:n], C.ropeS_s[:, t0:t0 + n], ALU.mult)
    P.tt(out, t1[:, 0:n], t2[:, 0:n], ALU.add, eng='pool')


def phase_a(C, l, chunks):
    P = C.P
    win = C.w_in.h.ap()[l]
    wrot = C.w_rot.h.ap()[l]
    kp = "(k p) c -> p k c"
    with ExitStack() as es:
        KT = P.sbuf(es, "KT", [128, 4, NT], BF16, slot_axis=1)
        Vt = P.sbuf(es, "Vt", [128, 18, 512], BF16, slot_axis=1)
        C.ropeC_s = P.sbuf(es, "ropeC_s", [128, NL], F32)
        C.ropeS_s = P.sbuf(es, "ropeS_s", [128, NL], F32)
        t1 = P.sbuf(es, "ropet1", [128, 512], F32)
        t2 = P.sbuf(es, "ropet2", [128, 512], F32)
        P.dma('sp', C.ropeC_s[:, :], C.ropeC[:, :])
        P.dma('sp', C.ropeS_s[:, :], C.ropeS[:, :])
        rel = [KT, Vt, C.ropeC_s, C.ropeS_s]
        with ExitStack() as e1:
            Wk = P.sbuf(e1, "Wk", [128, 8, 512], BF16)
            WkP = P.sbuf(e1, "WkP", [128, 8, 512], BF16)
            Wv = P.sbuf(e1, "Wv", [128, 8, 512], BF16)
            load_w(P, Wk[:, :, :], C.w_in, win[:, 512:1024].rearrange(kp, p=128))
            load_w(P, WkP[:, :, :], C.w_rot, wrot[:, 512:1024].rearrange(kp, p=128))
            load_w(P, Wv[:, :, :], C.w_in, win[:, 1024:1536].rearrange(kp, p=128))
            it = 0
            for c in range(5):
                t0, n, s = CH[c]
                for h in range(4):
                    bk = P.psum[(2 * it) % 8]
                    bp = P.psum[(2 * it + 1) % 8]
                    it += 1
                    proj_fm(P, bk, Wk, h * 128, C.HT[c], n)
                    if s == 0:
                        proj_fm(P, bp, WkP, h * 128, C.HT[c], n)
                        rope_evac(C, KT[:, h, t0:t0 + n], bk, bp, t0, n, t1, t2)
                    else:
                        P.copy(KT[:, h, t0:t0 + n], bk[:, 0:n], eng='act')
                for tl in range(n // 128):
                    tile = t0 // 128 + tl
                    bv = P.psum[(2 * it) % 8]
                    it += 1
                    for k in range(8):
                        P.mm(bv[:, 0:512], C.HT[c][:, k, tl * 128:(tl + 1) * 128], Wv[:, k, :],
                             start=(k == 0), stop=(k == 7))
                    P.copy(Vt[:, tile, :], bv[:, 0:512], eng='act')
            P.barrier()
            P.release_dsems([Wk, WkP, Wv])
        with ExitStack() as e2:
            Wq = P.sbuf(e2, "Wq", [128, 8, 512], BF16)
            WqP = P.sbuf(e2, "WqP", [128, 8, 512], BF16)
            load_w(P, Wq[:, :, :], C.w_in, win[:, 0:512].rearrange(kp, p=128))
            load_w(P, WqP[:, :, :], C.w_rot, wrot[:, 0:512].rearrange(kp, p=128))
            QT = [P.sbuf(e2, "QT%d" % i, [128, 4, 512], BF16) for i in range(2)]
            pT = [P.sbuf(e2, "pT%d" % i, [128, 512], BF16) for i in range(3)]
            rsum = P.sbuf(e2, "a_rsum", [128, 512], F32)
            om = [[P.sbuf(e2, "a_om%d_%d" % (hp, i), [128, 512], F32) for i in range(2)] for hp in range(2)]
            odt = P.sbuf(e2, "a_od", [128, 512], F32)
            sq = P.sbuf(e2, "a_sq", [128, 512], F32)
            rst = P.sbuf(e2, "a_rst", [128, 512], F32)
            OA = [P.sbuf(e2, "OA%d" % i, [128, 4, 512], BF16) for i in range(2)]
            hm = 0
            for ci, c in enumerate(chunks):
                t0, n, s = CH[c]
                q = QT[ci % 2]
                oa = OA[ci % 2]
                for h in range(4):
                    bk = P.psum[7]
                    bp = P.psum[6]
                    proj_fm(P, bk, Wq, h * 128, C.HT[c], n)
                    if s == 0:
                        proj_fm(P, bp, WqP, h * 128, C.HT[c], n)
                        rope_evac(C, q[:, h, 0:n], bk, bp, t0, n, t1, t2)
                    else:
                        P.copy(q[:, h, 0:n], bk[:, 0:n], eng='act')
                tiles = list(range(18)) if s == 0 else [16, 17]

                def epilogue(h):
                    omh = om[h % 2]
                    P.stt(odt[:, 0:n], omh[1][:, 0:n], C.lamneg[:, 0:1], omh[0][:, 0:n], ALU.mult, ALU.add)
                    P.tt(sq[:, 0:n], odt[:, 0:n], odt[:, 0:n], ALU.mult, eng='pool')
                    bN = P.psum[7]
                    P.mm(bN[:, 0:n], C.ones_f[:, :], sq[:, 0:n])
                    rstd_from_bank(P, rst, bN, n, 1.0 / 128)
                    P.stt(oa[:, h, 0:n], odt[:, 0:n], C.sgc[:, 0:1], rst[:, 0:n], ALU.mult, ALU.mult)

                for h in range(4):
                    for m in range(2):
                        bO = P.psum[3 + (hm % 2)]
                        bS = P.psum[5 + (hm % 2)]
                        hm += 1
                        pb = slice(m * 64, (m + 1) * 64)

                        def st(i):
                            kt = tiles[i]
                            P.mm(P.psum[i % 3][:, 0:n], KT[pb, h, kt * 128:(kt + 1) * 128], q[pb, h, 0:n])
                        st(0)
                        if len(tiles) > 1:
                            st(1)
                        for i, kt in enumerate(tiles):
                            if i + 2 < len(tiles):
                                st(i + 2)
                            p = pT[i % 3]
                            P.act(p[:, 0:n], P.psum[i % 3][:, 0:n], AF.Exp, scale=0.125)
                            P.mm(bO[:, 0:n], Vt[:, kt, h * 128:(h + 1) * 128], p[:, 0:n],
                                 start=(i == 0), stop=(i == len(tiles) - 1))
                            P.mm(bS[:, 0:n], C.ones_b[:, :], p[:, 0:n],
                                 start=(i == 0), stop=(i == len(tiles) - 1))
                        P.op('dve', lambda e: e.reciprocal(out=rsum[:, 0:n].ap, in_=bS[:, 0:n].ap),
                             reads=[bS[:, 0:n]], writes=[rsum[:, 0:n]])
                        P.tt(om[h % 2][m][:, 0:n], bO[:, 0:n], rsum[:, 0:n], ALU.mult)
                    if h >= 1:
                        epilogue(h - 1)
                epilogue(3)
                P.dma('sp', C.od(0, c), oa[:, :, 0:n])
            P.barrier()
            P.release_dsems([Wq, WqP] + QT + OA)
        P.release_dsems(rel)


def na_tiles(r):
    rs_ = min(max(r - 4, 0), 24)
    out = []
    for a in range(rs_ // 2, (rs_ + 7) // 2 + 1):
        r0, r1 = 2 * a, 2 * a + 1
        v0 = rs_ <= r0 < rs_ + 8
        v1 = rs_ <= r1 < rs_ + 8
        if v0 and v1:
            idx = r0 - r + 7
            assert 0 <= idx <= 13
        elif v1:
            assert r1 - r + 7 == 3
            idx = 14
        else:
            assert v0 and r0 - r + 7 == 10
            idx = 15
        out.append((a, idx))
    return out


def phase_b(C, l, chunks):
    P = C.P
    win = C.w_in.h.ap()[l]
    kp = "(k p) c -> p k c"
    with ExitStack() as es:
        KT = P.sbuf(es, "KN", [128, 4, NT], BF16, slot_axis=1)
        Vt = P.sbuf(es, "VN", [128, 18, 512], BF16, slot_axis=1)
        NAT = P.sbuf(es, "NAT", [128, 16, 512], BF16)
        for i0 in range(0, 16, 4):
            load_w(P, NAT[:, i0:i0 + 4, :], C.naT, C.naT.h.ap()[l][i0:i0 + 4].rearrange("i p c -> p i c"))
        rel = [KT, Vt, NAT]
        with ExitStack() as e1:
            Wk = P.sbuf(e1, "Wkn", [128, 8, 512], BF16)
            Wv = P.sbuf(e1, "Wvn", [128, 8, 512], BF16)
            load_w(P, Wk[:, :, :], C.w_in, win[:, 2048:2560].rearrange(kp, p=128))
            load_w(P, Wv[:, :, :], C.w_in, win[:, 2560:3072].rearrange(kp, p=128))
            it = 0
            for c in range(5):
                t0, n, s = CH[c]
                for h in range(4):
                    bk = P.psum[it % 8]
                    it += 1
                    proj_fm(P, bk, Wk, h * 128, C.HT[c], n)
                    P.copy(KT[:, h, t0:t0 + n], bk[:, 0:n], eng=('act' if h % 2 else 'dve'))
                for tl in range(n // 128):
                    tile = t0 // 128 + tl
                    bv = P.psum[it % 8]
                    it += 1
                    for k in range(8):
                        P.mm(bv[:, 0:512], C.HT[c][:, k, tl * 128:(tl + 1) * 128], Wv[:, k, :],
                             start=(k == 0), stop=(k == 7))
                    P.copy(Vt[:, tile, :], bv[:, 0:512], eng=('act' if tl % 2 else 'dve'))
            P.barrier()
            P.release_dsems([Wk, Wv])
        with ExitStack() as e2:
            Wq = P.sbuf(e2, "Wqn", [128, 8, 512], BF16)
            load_w(P, Wq[:, :, :], C.w_in, win[:, 1536:2048].rearrange(kp, p=128))
            QT = [P.sbuf(e2, "QN%d" % i, [128, 4, 512], BF16) for i in range(2)]
            pT = [P.sbuf(e2, "pN%d" % i, [128, 512], BF16) for i in range(3)]
            rsum = P.sbuf(e2, "b_rsum", [128, 512], F32)
            OB = [P.sbuf(e2, "OB%d" % i, [128, 4, 512], BF16) for i in range(2)]
            rowi = 0
            for ci, c in enumerate(chunks):
                t0, n, s = CH[c]
                q = QT[ci % 2]
                ob = OB[ci % 2]
                for h in range(4):
                    bk = P.psum[7]
                    proj_fm(P, bk, Wq, h * 128, C.HT[c], n)
                    P.act(q[:, h, 0:n], bk[:, 0:n], AF.Identity, scale=0.125)
                for rr in range(n // 64):
                    rq = rr * 64
                    if s == 0:
                        r = t0 // 64 + rr
                        tiles = na_tiles(r) + [(16, None), (17, None)]
                    else:
                        tiles = [(16, None), (17, None)]
                    bO = P.psum[4 + (rowi % 2)]
                    bS = P.psum[6]
                    rowi += 1

                    def st(i):
                        a, idx = tiles[i]
                        for par in range(2):
                            bk_ = P.psum[(i % 2) * 2 + par]
                            pb = slice(par * 64, par * 64 + 64)
                            if idx is not None:
                                P.mm(bk_[:, 0:256], C.ident_b[:, :], NAT[:, idx, par * 256:(par + 1) * 256],
                                     start=True, stop=False, sgc=True)
                            for cc in range(4):
                                P.mm(bk_[:, cc * 64:(cc + 1) * 64], KT[pb, cc, a * 128:(a + 1) * 128],
                                     q[pb, cc, rq:rq + 64], start=(idx is None), stop=True, sgc=True)
                    st(0)
                    nt_ = len(tiles)
                    for i, (a, idx) in enumerate(tiles):
                        if i + 1 < nt_:
                            st(i + 1)
                        p = pT[i % 3]
                        for par in range(2):
                            P.act(p[:, par * 256:(par + 1) * 256], P.psum[(i % 2) * 2 + par][:, 0:256], AF.Exp)
                        if i == 0:
                            P.mm(bO[:, 0:512], C.zeros_b[:, :], p[:, :], start=True, stop=False, sgc=True)
                        for par in range(2):
                            for cc in range(4):
                                co = par * 256 + cc * 64
                                P.mm(bO[:, co:co + 64], Vt[:, a, cc * 128:(cc + 1) * 128],
                                     p[:, co:co + 64], start=False, stop=(i == nt_ - 1), sgc=True)
                        P.mm(bS[:, 0:512], C.ones_b[:, :], p[:, :], start=(i == 0), stop=(i == nt_ - 1))
                    P.op('dve', lambda e: e.reciprocal(out=rsum[:, :].ap, in_=bS[:, 0:512].ap),
                         reads=[bS[:, 0:512]], writes=[rsum[:, :]])
                    for par in range(2):
                        pb = slice(par * 64, par * 64 + 64)
                        o_v = ob.view(ob.h[pb, :, rq:rq + 64])
                        b_v = bO.view(bO.h[pb, par * 256:(par + 1) * 256].rearrange("p (c q) -> p c q", c=4))
                        r_v = rsum.view(rsum.h[pb, par * 256:(par + 1) * 256].rearrange("p (c q) -> p c q", c=4))
                        P.tt(o_v, b_v, r_v, ALU.mult)
                P.dma('sp', C.od(1, c), ob[:, :, 0:n])
            P.barrier()
            P.release_dsems([Wq] + QT + OB)
        P.release_dsems(rel)


def phase_c(C, l, chunks):
    P = C.P
    win = C.w_in.h.ap()[l]
    kp = "(k p) c -> p k c"
    with ExitStack() as es:
        AB = P.sbuf(es, "AB", [128, 18, 4, 256], BF16, slot_axis=1)
        CS = P.sbuf(es, "CS128", [128, 256], BF16)
        Wf = P.sbuf(es, "Wf", [128, 8, 512], BF16)
        fT = [P.sbuf(es, "fT%d" % i, [128, 4, 512], BF16) for i in range(2)]
        CNks = [P.sbuf(es, "CNk%d" % i, [128, 16, 512], BF16) for i in range(2)]
        SNks = [P.sbuf(es, "SNk%d" % i, [128, 16, 512], BF16) for i in range(2)]
        CNk, SNk = CNks[0], SNks[0]
        OC = [P.sbuf(es, "OC%d" % i, [128, 4, 512], BF16) for i in range(2)]
        load_w(P, CS[:, :], C.cs128, C.cs128.h.ap())
        load_w(P, Wf[:, :, :], C.w_in, win[:, 3072:3584].rearrange(kp, p=128))
        it = 0
        for c in range(5):
            t0, n, s = CH[c]
            if s == 1 and 4 not in chunks:
                continue
            f = fT[c % 2]
            for g_ in range(4):
                bk = P.psum[it % 4]
                it += 1
                proj_fm(P, bk, Wf, g_ * 128, C.HT[c], n)
                P.copy(f[:, g_, 0:n], bk[:, 0:n], eng=('act' if g_ % 2 else 'dve'))
            for tl in range(n // 128):
                tile = t0 // 128 + tl
                for half in range(2):
                    bk = P.psum[4 + (it % 4)]
                    it += 1
                    for gg in range(2):
                        g_ = half * 2 + gg
                        P.mm(bk[:, gg * 256:(gg + 1) * 256], f[:, g_, tl * 128:(tl + 1) * 128], CS[:, :])
                    P.copy(AB.view(AB.h[:, tile, half * 2:half * 2 + 2, :], slots=[tile]),
                           bk.view(bk.h[:, 0:512].rearrange("p (g m) -> p g m", g=2)),
                           eng=('act' if half else 'dve'))
        for kc in range(4):
            CNk, SNk = CNks[kc % 2], SNks[kc % 2]
            for t4 in range(0, 16, 4):
                load_w(P, CNk[:, t4:t4 + 4, :], C.cn,
                       C.cn.h.ap()[t4 * 128:(t4 + 4) * 128, kc * 512:(kc + 1) * 512].rearrange("(t p) c -> p t c", p=128))
                load_w(P, SNk[:, t4:t4 + 4, :], C.sn,
                       C.sn.h.ap()[t4 * 128:(t4 + 4) * 128, kc * 512:(kc + 1) * 512].rearrange("(t p) c -> p t c", p=128))
            oc = OC[kc % 2]
            for g_ in range(4):
                bk = P.psum[g_ % 4]
                for nt_ in range(16):
                    P.mm(bk[:, 0:512], AB[:, nt_, g_, 0:128], CNk[:, nt_, :], start=(nt_ == 0), stop=False)
                    P.mm(bk[:, 0:512], AB[:, nt_, g_, 128:256], SNk[:, nt_, :], start=False, stop=(nt_ == 15))
                P.copy(oc[:, g_, :], bk[:, 0:512], eng=('act' if g_ % 2 else 'dve'))
            P.dma('sp', C.od(2, kc), oc[:, :, :])
        if 4 in chunks:
            P.barrier()
            load_w(P, CNk[:, 0:2, 0:256], C.c256, C.c256.h.ap().rearrange("(t p) c -> p t c", p=128))
            load_w(P, SNk[:, 0:2, 0:256], C.s256, C.s256.h.ap().rearrange("(t p) c -> p t c", p=128))
            oc = OC[0]
            for g_ in range(4):
                bk = P.psum[g_ % 4]
                for nt_ in range(2):
                    P.mm(bk[:, 0:256], AB[:, 16 + nt_, g_, 0:128], CNk[:, nt_, 0:256], start=(nt_ == 0), stop=False)
                    P.mm(bk[:, 0:256], AB[:, 16 + nt_, g_, 128:256], SNk[:, nt_, 0:256], start=False, stop=(nt_ == 1))
                P.copy(oc[:, g_, 0:256], bk[:, 0:256], eng=('act' if g_ % 2 else 'dve'))
            P.dma('sp', C.od(2, 4), oc[:, :, 0:256])
        P.barrier()
        P.release_dsems([AB, CS, Wf] + CNks + SNks + fT + OC)


def phase_out(C, l, chunks):
    P = C.P
    win = C.w_in.h.ap()[l]
    with ExitStack() as es:
        MG = [P.sbuf(es, "MG%d" % c, [128, 8, CH[c][1]], BF16) for c in range(5)]
        Oi = [P.sbuf(es, "Oi%d" % c, [128, 4, CH[c][1]], BF16) for c in range(5)]
        Wb = [P.sbuf(es, "Wb%d" % i, [128, 4, 128], BF16) for i in range(2)]
        Wg = [P.sbuf(es, "Wg%d" % i, [128, 8, 128], BF16) for i in range(2)]
        sg = [P.sbuf(es, "sg%d" % i, [128, 512], F32) for i in range(2)]
        tm = [P.sbuf(es, "otm%d" % i, [128, 512], F32) for i in range(2)]
        it = 0
        for i in range(3):
            for c in chunks:
                t0, n, s = CH[c]
                P.dma('sp', Oi[c][:, :, :], C.od(i, c))
            for dc in range(8):
                wb = Wb[it % 2]
                wg = Wg[it % 2]
                load_w(P, wb[:, :, :], C.w_branch,
                       C.w_branch.h.ap()[l, i][:, dc * 128:(dc + 1) * 128].rearrange("(k p) c -> p k c", p=128))
                gc0 = 3584 + i * 1024 + dc * 128
                load_w(P, wg[:, :, :], C.w_in, win[:, gc0:gc0 + 128].rearrange("(k p) c -> p k c", p=128))
                for c in chunks:
                    t0, n, s = CH[c]
                    bP = P.psum[(2 * it) % 8]
                    bG = P.psum[(2 * it + 1) % 8]
                    it += 1
                    for k in range(4):
                        P.mm(bP[:, 0:n], wb[:, k, :], Oi[c][:, k, 0:n], start=(k == 0), stop=(k == 3))
                    for k in range(8):
                        P.mm(bG[:, 0:n], wg[:, k, :], C.HT[c][:, k, 0:n], start=(k == 0), stop=(k == 7))
                    s_ = sg[it % 2]
                    P.act(s_[:, 0:n], bG[:, 0:n], AF.Sigmoid)
                    if i == 0:
                        P.tt(MG[c][:, dc, 0:n], bP[:, 0:n], s_[:, 0:n], ALU.mult)
                    else:
                        t_ = tm[it % 2]
                        P.tt(t_[:, 0:n], bP[:, 0:n], s_[:, 0:n], ALU.mult)
                        P.tt(MG[c][:, dc, 0:n], MG[c][:, dc, 0:n], t_[:, 0:n], ALU.add, eng='pool')
        P.barrier()
        P.release_dsems(Oi + Wb + Wg)
        with ExitStack() as e2:
            Wo = P.sbuf(e2, "Wo", [128, 8, D], BF16)
            xck = [P.sbuf(e2, "ox%d" % i, [128, 8, 512], F32) for i in range(2)]
            load_w(P, Wo[:, :, :], C.w_out, C.w_out.h.ap()[l].rearrange("(k p) c -> p k c", p=128))
            it = 0
            for ci, c in enumerate(chunks):
                t0, n, s = CH[c]
                xc = xck[ci % 2]
                P.dma('sp', xc[:, :, 0:n], C.xd(c))
                for dc in range(8):
                    bk = P.psum[it % 8]
                    it += 1
                    for k in range(8):
                        P.mm(bk[:, 0:n], Wo[:, k, dc * 128:(dc + 1) * 128], MG[c][:, k, 0:n],
                             start=(k == 0), stop=(k == 7))
                    P.stt(xc[:, dc, 0:n], bk[:, 0:n], C.modT[:, s, 16 + dc:17 + dc], xc[:, dc, 0:n], ALU.mult, ALU.add)
                P.dma('sp', C.xd(c), xc[:, :, 0:n])
            P.barrier()
            P.release_dsems([Wo] + xck)


def phase_moe(C, l, chunks):
    P = C.P
    with ExitStack() as es:
        C.XT = [P.sbuf(es, "XT%d" % c, [128, 8, CH[c][1]], F32, slot_axis=1) for c in range(5)]
        C.H2T = [P.sbuf(es, "H2T%d" % c, [128, 8, CH[c][1]], BF16) for c in range(5)]
        C.WT = P.sbuf(es, "WT", [32, NT], F32)
        for c in chunks:
            P.dma('sp', C.XT[c][:, :, :], C.xd(c))
        phase_h(C, l, 2, chunks)
        wmk = [P.sbuf(es, "wmk%d" % i, [32, 512], BF16) for i in range(3)]
        wbc = [P.sbuf(es, "wbc%d" % i, [128, 512], BF16) for i in range(3)]
        bdn = P.sbuf(es, "bdn", [32, D], F32)
        bgu = P.sbuf(es, "bgu", [128, 32, 16], F32)
        P.dma('sp', bdn[:, :], C.b_down.view(C.b_down.h.ap()[l]))
        P.dma('sp', bgu[:, :, :], C.b_guT.view(C.b_guT.h.ap()[l]))
        bgu1 = P.sbuf(es, "bgu1", [128, 32, 8], F32)
        P.ts(bgu1[:, :, :], bgu[:, :, 8:16], 1.0, None, ALU.add)
        actT = [[P.sbuf(es, "actT%d_%d" % (i, c), [128, 2, CH[c][1]], BF16, slot_axis=1) for c in range(5)]
                for i in range(2)]
        wgu = [P.sbuf(es, "wgu%d" % i, [128, 8, 2, 256], BF16) for i in range(2)]
        wd = [P.sbuf(es, "wd%d" % i, [128, 2, D], BF16) for i in range(3)]
        g1 = [P.sbuf(es, "g1_%d" % i, [128, 512], BF16) for i in range(2)]
        sgm = [P.sbuf(es, "sgm%d" % i, [128, 512], BF16) for i in range(2)]
        u0 = [P.sbuf(es, "u0_%d" % i, [128, 512], BF16) for i in range(2)]
        tq = [P.sbuf(es, "tq%d" % i, [128, 512], BF16) for i in range(4)]
        it = 0
        for c in chunks:
            t0, n, s = CH[c]
            for dc in range(8):
                bk = P.psum[it % 2]
                it += 1
                P.mm(bk[:, 0:n], bdn[0:32, dc * 128:(dc + 1) * 128], C.WT[0:32, t0:t0 + n])
                P.stt(C.XT[c][:, dc, 0:n], bk[:, 0:n], C.modT[:, s, 40 + dc:41 + dc], C.XT[c][:, dc, 0:n],
                      ALU.mult, ALU.add)
        wgu_ap = C.w_gate_up.h.ap()
        wd_ap = C.w_down.h.ap()
        quarters = [(e, qq) for e in range(C.NE) for qq in range(4)]

        def issue_weights(Qi):
            e, qq = quarters[Qi]
            wg = wgu[Qi % 2]
            wdn = wd[Qi % 3]
            c0 = qq * 256
            P.dma('pool', wg[:, :, 0, :], C.w_gate_up.view(
                wgu_ap[l, e][:, c0:c0 + 256].rearrange("(k p) c -> p k c", p=128)))
            P.dma('pool', wg[:, :, 1, :], C.w_gate_up.view(
                wgu_ap[l, e][:, 1024 + c0:1024 + c0 + 256].rearrange("(k p) c -> p k c", p=128)), first=False)
            P.dma('pool', wdn[:, :, :], C.w_down.view(
                wd_ap[l, e][c0:c0 + 256, :].rearrange("(j p) c -> p j c", p=128)))

        st_ = {"u": 0, "d": 0}
        pend = []
        e2done = {}

        def emit_e2():
            Qi, c, j, t_, bW = pend.pop(0)
            t0, n, s = CH[c]
            P.tt(actT[Qi % 2][c][:, j, 0:n], t_[:, 0:n], bW[:, 0:n], ALU.mult)
            e2done[(Qi, c)] = e2done.get((Qi, c), 0) + 1

        def emit_d(Qi, c, dc):
            t0, n, s = CH[c]
            wdn = wd[Qi % 3]
            bD = P.psum[4 + (st_["d"] % 2)]
            st_["d"] += 1
            for j in range(2):
                P.mm(bD[:, 0:n], wdn[:, j, dc * 128:(dc + 1) * 128], actT[Qi % 2][c][:, j, 0:n],
                     start=(j == 0), stop=(j == 1))
            P.stt(C.XT[c][:, dc, 0:n], bD[:, 0:n], C.modT[:, s, 40 + dc:41 + dc], C.XT[c][:, dc, 0:n],
                  ALU.mult, ALU.add)

        issue_weights(0)
        dq = []
        steps = [(Qi, e, qq, c) for Qi, (e, qq) in enumerate(quarters) for c in chunks]

        def emit_wm(si):
            Qi, e, qq, c = steps[si]
            t0, n, s = CH[c]
            P.ts(wmk[si % 3][0:32, 0:n], C.WT[0:32, t0:t0 + n], C.ident[0:32, e:e + 1], None, ALU.mult)
        emit_wm(0)
        for si, (Qi, e, qq, c) in enumerate(steps):
            if c == chunks[0] and Qi + 1 < len(quarters):
                issue_weights(Qi + 1)
            wg = wgu[Qi % 2]
            t0, n, s = CH[c]
            if si + 1 < len(steps):
                emit_wm(si + 1)
            bW = P.psum[6 + (si % 2)]
            P.mm(bW[:, 0:n], C.ones_b[0:32, :], wmk[si % 3][0:32, 0:n])
            wb = wbc[si % 3]
            P.act(wb[:, 0:n], bW[:, 0:n], AF.Identity)
            for j in range(2):
                u = st_["u"]
                st_["u"] += 1
                ffc = qq * 2 + j
                bG = P.psum[(2 * u) % 4]
                bU = P.psum[(2 * u + 1) % 4]

                def try_d():
                    if dq and e2done.get((dq[0][0], dq[0][1]), 0) == 2:
                        emit_d(*dq.pop(0))
                for k in range(8):
                    P.mm(bG[:, 0:n], wg[:, k, 0, j * 128:(j + 1) * 128], C.H2T[c][:, k, 0:n],
                         start=(k == 0), stop=(k == 7))
                    if k == 3 or k == 7:
                        try_d()
                for k in range(8):
                    P.mm(bU[:, 0:n], wg[:, k, 1, j * 128:(j + 1) * 128], C.H2T[c][:, k, 0:n],
                         start=(k == 0), stop=(k == 7))
                    if k == 3 or k == 7:
                        try_d()
                g_ = g1[u % 2]
                s_ = sgm[u % 2]
                u_ = u0[u % 2]
                t_ = tq[u % 4]
                P.ts(g_[:, 0:n], bG[:, 0:n], bgu[:, e, ffc:ffc + 1], 7.0, ALU.add, ALU.min)
                P.act(s_[:, 0:n], g_[:, 0:n], AF.Sigmoid, scale=1.702)
                P.act(u_[:, 0:n], bU[:, 0:n], AF.Identity, bias=bgu1[:, e, ffc:ffc + 1])
                P.ts(u_[:, 0:n], u_[:, 0:n], 8.0, -6.0, ALU.min, ALU.max, eng='pool')
                P.tt(t_[:, 0:n], g_[:, 0:n], s_[:, 0:n], ALU.mult, eng='pool')
                P.tt(t_[:, 0:n], t_[:, 0:n], u_[:, 0:n], ALU.mult, eng='pool')
                pend.append((Qi, c, j, t_, wb))
                if len(pend) > 2:
                    emit_e2()
            if c == chunks[-1]:
                while dq and dq[0][0] < Qi:
                    while e2done.get((dq[0][0], dq[0][1]), 0) < 2:
                        emit_e2()
                    emit_d(*dq.pop(0))
                dq.extend((Qi, c_, dc) for c_ in chunks for dc in range(8))
        while pend:
            emit_e2()
        while dq:
            emit_d(*dq.pop(0))
        for c in chunks:
            P.dma('sp', C.xd(c), C.XT[c][:, :, :])
        P.barrier()
        P.release_dsems(C.XT + C.H2T + [C.WT, bdn, bgu] + actT[0] + actT[1] + wgu + wd)


def phase_final(C):
    P = C.P
    with ExitStack() as es:
        xck = [P.sbuf(es, "fx%d" % i, [128, 8, 512], F32) for i in range(2)]
        sqt = [P.sbuf(es, "fsq%d" % i, [128, 512], F32) for i in range(2)]
        rs = [P.sbuf(es, "frs%d" % i, [128, 512], F32) for i in range(2)]
        for c in range(4):
            t0, n, s = CH[c]
            xc = xck[c % 2]
            P.dma('sp', xc[:, :, :], C.xd(c))
            bank = P.psum[c % 2]
            for k in range(8):
                sq = sqt[k % 2]
                P.tt(sq[:, :], xc[:, k, :], xc[:, k, :], ALU.mult, eng='pool')
                P.mm(bank[:, 0:n], C.ones_f[:, :], sq[:, :], start=(k == 0), stop=(k == 7))
            r = rs[c % 2]
            rstd_from_bank(P, r, bank, n, 1.0 / D)
            for k in range(8):
                P.stt(xc[:, k, :], xc[:, k, :], C.gfin_s[:, k:k + 1], r[:, :], ALU.mult, ALU.mult)
            P.dma('sp', C.outT.view(C.outT.h.ap()[:, t0:t0 + n].rearrange("(k p) t -> p k t", p=128)), xc[:, :, :])
        P.barrier()
        P.release_dsems(xck)


def _rope_tables():
    t = np.arange(NL)
    row = (t // 64).astype(np.float32)
    col = (t % 64).astype(np.float32)
    freqs = (np.float32(10000.0) ** (-np.arange(16, dtype=np.float32) / np.float32(16))).astype(np.float32)
    Ct = np.zeros((128, NL), np.float32)
    St = np.zeros((128, NL), np.float32)
    for p in range(128):
        d = p % 64
        axis = d // 32
        half = (d % 32) // 16
        i = d % 16
        ang = (row if axis == 0 else col) * freqs[i]
        Ct[p] = np.cos(ang)
        St[p] = (-1.0 if half == 0 else 1.0) * np.sin(ang)
    return Ct, St


def _rot_cols():
    idx = np.arange(512)
    d = idx % 64
    return (idx - d) + (d ^ 16)


def _dft(n, scale):
    k = np.arange(n, dtype=np.int64)
    ph = (np.outer(k, k) % n).astype(np.float64) * (2.0 * np.pi / n)
    return (np.cos(ph) * scale).astype(np.float32), (np.sin(ph) * scale).astype(np.float32)


def _na_tables(rpb):
    q = np.arange(64)
    k = np.arange(64)
    cs = np.clip(q - 8, 0, 48)
    mask = (k[None, :] >= cs[:, None]) & (k[None, :] < cs[:, None] + 16)
    dc = np.clip(k[None, :] - q[:, None], -15, 15) + 15
    Bt = rpb[:, :, :, dc]
    Bt = np.where(mask[None, None, None], Bt, np.float32(NEG)).astype(np.float32)
    Bt = Bt.transpose(0, 2, 4, 1, 3)
    Bt = np.ascontiguousarray(Bt[:, :, :, [0, 2, 4, 6, 1, 3, 5, 7], :]).reshape(4, 15, 64, 512)
    M = np.full((4, 64, 512), NEG, np.float32)
    T = np.empty((4, 16, 128, 512), np.float32)
    for i in range(14):
        T[:, i, 0:64] = Bt[:, i]
        T[:, i, 64:128] = Bt[:, i + 1]
    T[:, 14, 0:64] = M
    T[:, 14, 64:128] = Bt[:, 3]
    T[:, 15, 0:64] = Bt[:, 10]
    T[:, 15, 64:128] = M
    return T


def prep_shared(inp):
    f = lambda a: np.ascontiguousarray(np.asarray(a, dtype=np.float32))
    sh = {}
    sh["w_mod"] = f(inp["w_mod"])
    sh["b_modT"] = f(np.asarray(inp["b_mod"]).reshape(4, 48, 128).transpose(0, 2, 1))
    sh["gmix"] = f(np.asarray(inp["norm_mix_g"]).reshape(4, 8, 128).transpose(2, 0, 1))
    sh["gffn"] = f(np.asarray(inp["norm_ffn_g"]).reshape(4, 8, 128).transpose(2, 0, 1))
    sh["gfin"] = f(np.asarray(inp["final_g"]).reshape(8, 128).T)
    w_in = f(inp["w_in"])
    sh["w_in"] = w_in
    rc = _rot_cols()
    sh["w_rot"] = f(np.concatenate([w_in[:, :, 0:512][:, :, rc], w_in[:, :, 512:1024][:, :, rc]], axis=2))
    Ct, St = _rope_tables()
    sh["ropeC"] = Ct
    sh["ropeS"] = St
    sh["da_lam"] = f(np.asarray(inp["da_lambda"]).reshape(4, 256))
    sh["subln"] = f(np.asarray(inp["da_subln_g"]).T)
    sh["naT"] = _na_tables(np.asarray(inp["na_rpb"], dtype=np.float32))
    sh["w_branch"] = f(inp["w_branch"])
    sh["w_out"] = f(inp["w_out"])
    sh["w_router"] = f(inp["w_router"])
    sh["b_router"] = f(inp["b_router"])
    sh["w_gate_up"] = f(inp["w_gate_up"])
    sh["b_guT"] = f(np.asarray(inp["b_gate_up"]).reshape(4, 32, 16, 128).transpose(0, 3, 1, 2))
    sh["w_down"] = f(inp["w_down"])
    sh["b_down"] = f(inp["b_down"])
    c128, s128 = _dft(128, 1.0 / np.sqrt(128.0))
    sh["cs128"] = f(np.concatenate([c128, s128], axis=1))
    cn, sn = _dft(NL, 1.0 / np.sqrt(float(NL)))
    sh["cn"] = cn
    sh["sn"] = f(-sn)
    c256, s256 = _dft(256, 1.0 / 16.0)
    sh["c256"] = c256
    sh["s256"] = f(-s256)
    sh["ident"] = np.eye(128, dtype=np.float32)
    return sh


def prep_core(inp, b):
    x = np.asarray(inp["x"][b], dtype=np.float32)
    ctx = np.asarray(inp["ctx"][b], dtype=np.float32)
    xT = np.ascontiguousarray(np.concatenate([x, ctx], axis=0).T)
    c = np.asarray(inp["c"][b], dtype=np.float32).reshape(8, 128).T
    cctx = np.asarray(inp["c_ctx"], dtype=np.float32).reshape(8, 128).T
    cc = np.ascontiguousarray(np.stack([c, cctx], axis=-1))
    return {"xT": xT, "cc": cc}


_CACHE = {}


def kernel(**inputs):
    if "nc" not in _CACHE:
        _CACHE["nc"] = build(L=4)[0]
    nc = _CACHE["nc"]
    sh = prep_shared(inputs)
    in_maps = []
    for b in range(8):
        m = dict(sh)
        m.update(prep_core(inputs, b))
        in_maps.append(m)
    res = run_bass_kernel_spmd(nc, in_maps, core_ids=list(range(8)))
    out = np.stack([np.ascontiguousarray(res.results[b]["outT"].T) for b in range(8)], axis=0)
    return out.astype(np.float32)
```
